# Optimizing a Trainium2 kernel written in Bass

```python
import jax, jax.numpy as jnp
from jax import lax
import numpy as np

D_MODEL = 1024
BATCH = 32
SEQ = 2048
DEPTH = 4

A_HEADS = 4
A_DK = 128
A_DV = 128
CONV_K = 4
B_HEADS = 4
B_DK = 128
B_DV = 128
CHUNK = 64
C_HEADS = 16
C_KV_HEADS = 4
C_GROUP = C_HEADS // C_KV_HEADS
C_HD = 64
WINDOW = 128
ROPE_THETA = 10000.0
D_FF = 4 * D_MODEL
PLE_DIM = 256
EPS = 1e-6
N_EVEN = (DEPTH + 1) // 2
N_ODD = DEPTH // 2

A_QK = A_HEADS * A_DK
A_V = A_HEADS * A_DV
B_QK = B_HEADS * B_DK
B_V = B_HEADS * B_DV
A_CONV_CH = 2 * A_QK + A_V
A_COLS = 2 * A_QK + A_V + 2 * A_HEADS + A_V
B_COLS = 2 * B_QK + B_V + 2 * B_HEADS + B_V
IN_COLS = A_COLS + B_COLS
MIX_AB = A_V + B_V
C_Q = C_HEADS * C_HD
C_KV = C_KV_HEADS * C_HD
C_COLS = C_Q + 2 * C_KV

kernel_name = "hybrid_deltanet_mlstm_swa_trunk"

F32 = jnp.float32


def rmsnorm(x, g):
    xf = x.astype(F32)
    y = xf * lax.rsqrt(jnp.mean(xf * xf, -1, keepdims=True) + EPS)
    return (y * g.astype(F32)).astype(x.dtype)


def l2norm(x):
    return x * lax.rsqrt(jnp.sum(x * x, -1, keepdims=True) + EPS)


def causal_conv(x, w):
    k_taps = w.shape[0]
    s = x.shape[1]
    xp = jnp.pad(x, ((0, 0), (k_taps - 1, 0), (0, 0)))
    return sum(xp[:, k:k + s] * w[k] for k in range(k_taps))


def to_chunks(x):
    b, s, h = x.shape[:3]
    x = x.reshape((b, s // CHUNK, CHUNK, h) + x.shape[3:])
    return jnp.moveaxis(x, (1, 3), (0, 2))


def from_chunks(x):
    x = jnp.moveaxis(x, (0, 2), (1, 3))
    b, n, c, h, d = x.shape
    return x.reshape(b, n * c, h, d)


def gated_delta_rule(q, k, v, g, beta):
    b, s, h, dk = q.shape
    dv = v.shape[-1]
    qc, kc, vc = to_chunks(q), to_chunks(k), to_chunks(v)
    gcum = jnp.cumsum(to_chunks(g), -1)
    bc = to_chunks(beta)
    causal = jnp.tril(jnp.ones((CHUNK, CHUNK), bool))
    strict = jnp.tril(jnp.ones((CHUNK, CHUNK), bool), -1)
    diff = gcum[..., :, None] - gcum[..., None, :]
    decay = jnp.where(causal, jnp.exp(jnp.where(causal, diff, 0.0)), 0.0)
    kb = kc * bc[..., None]
    vb = vc * bc[..., None]
    lmat = jnp.where(strict, jnp.einsum('nbhcd,nbhed->nbhce', kb, kc) * decay, 0.0)
    eye = jnp.eye(CHUNK, dtype=lmat.dtype)
    rhs = jnp.concatenate([vb, kb * jnp.exp(gcum)[..., None]], -1)
    uw = lax.linalg.triangular_solve(lmat + eye, rhs, left_side=True, lower=True,
                                     unit_diagonal=True)
    u, w = uw[..., :dv], uw[..., dv:]
    attn_intra = jnp.einsum('nbhcd,nbhed->nbhce', qc, kc) * decay
    q_dec = qc * jnp.exp(gcum)[..., None]
    g_last = gcum[..., -1]
    k_dec = kc * jnp.exp(g_last[..., None] - gcum)[..., None]

    def step(state, xs):
        u_, w_, a_, qd, kd, gl = xs
        v_new = u_ - jnp.einsum('bhcd,bhde->bhce', w_, state)
        o = (jnp.einsum('bhcd,bhde->bhce', qd, state)
             + jnp.einsum('bhcs,bhse->bhce', a_, v_new))
        state = (state * jnp.exp(gl)[..., None, None]
                 + jnp.einsum('bhcd,bhce->bhde', kd, v_new))
        return state, o

    s0 = jnp.zeros((b, h, dk, dv), F32)
    _, o = lax.scan(step, s0, (u, w, attn_intra, q_dec, k_dec, g_last))
    return from_chunks(o)


def mlstm_chunkwise(q, k, v, i_log, f_log):
    b, s, h, dk = q.shape
    dv = v.shape[-1]
    qc, kc, vc = to_chunks(q), to_chunks(k), to_chunks(v)
    ic = to_chunks(i_log)
    bcum = jnp.cumsum(to_chunks(f_log), -1)
    causal = jnp.tril(jnp.ones((CHUNK, CHUNK), bool))
    dmat = bcum[..., :, None] - bcum[..., None, :] + ic[..., None, :]
    dmat = jnp.where(causal, dmat, -jnp.inf)
    m_intra = jnp.max(dmat, -1)
    b_last = bcum[..., -1]
    w_state = b_last[..., None] - bcum + ic
    qk = jnp.einsum('nbhtd,nbhsd->nbhts', qc, kc)

    def step(carry, xs):
        c_st, n_st, m = carry
        q_, k_, v_, qk_, d_, mi, bc_, ws, bl = xs
        a = bc_ + m[..., None]
        mt = jnp.maximum(a, mi)
        inter = jnp.exp(a - mt)
        pmat = qk_ * jnp.exp(d_ - mt[..., None])
        num = (inter[..., None] * jnp.einsum('bhcd,bhde->bhce', q_, c_st)
               + jnp.einsum('bhts,bhse->bhte', pmat, v_))
        den = inter * jnp.einsum('bhcd,bhd->bhc', q_, n_st) + jnp.sum(pmat, -1)
        hout = num / jnp.maximum(jnp.abs(den), jnp.exp(-mt))[..., None]
        m_new = jnp.maximum(bl + m, jnp.max(ws, -1))
        sc = jnp.exp(bl + m - m_new)
        kw = k_ * jnp.exp(ws - m_new[..., None])[..., None]
        c_st = sc[..., None, None] * c_st + jnp.einsum('bhcd,bhce->bhde', kw, v_)
        n_st = sc[..., None] * n_st + jnp.sum(kw, -2)
        return (c_st, n_st, m_new), hout

    carry0 = (jnp.zeros((b, h, dk, dv), F32), jnp.zeros((b, h, dk), F32), jnp.zeros((b, h), F32))
    _, o = lax.scan(step, carry0, (qc, kc, vc, qk, dmat, m_intra, bcum, w_state, b_last))
    return from_chunks(o)


def mixer_ab(hn, w_in, conv_w, a_log, dt_bias, norm_a, i_bias, f_bias, norm_b, w_out):
    b, s, _ = hn.shape
    proj = hn @ w_in
    pa, pb = proj[..., :A_COLS], proj[..., A_COLS:]
    qkv = jax.nn.silu(causal_conv(pa[..., :A_CONV_CH], conv_w)).astype(F32)
    qa = l2norm(qkv[..., :A_QK].reshape(b, s, A_HEADS, A_DK)) * (A_DK ** -0.5)
    ka = l2norm(qkv[..., A_QK:2 * A_QK].reshape(b, s, A_HEADS, A_DK))
    va = qkv[..., 2 * A_QK:].reshape(b, s, A_HEADS, A_DV)
    off = A_CONV_CH
    beta = jax.nn.sigmoid(pa[..., off:off + A_HEADS].astype(F32))
    alpha_pre = pa[..., off + A_HEADS:off + 2 * A_HEADS].astype(F32) + dt_bias.astype(F32)
    g = -jnp.exp(a_log.astype(F32)) * jax.nn.softplus(alpha_pre)
    z = pa[..., off + 2 * A_HEADS:].astype(F32).reshape(b, s, A_HEADS, A_DV)
    oa = gated_delta_rule(qa, ka, va, g, beta)
    oa = rmsnorm(oa, norm_a) * jax.nn.silu(z)
    pbf = pb.astype(F32)
    qb = pbf[..., :B_QK].reshape(b, s, B_HEADS, B_DK) * (B_DK ** -0.5)
    kb = pbf[..., B_QK:2 * B_QK].reshape(b, s, B_HEADS, B_DK)
    vb = pbf[..., 2 * B_QK:2 * B_QK + B_V].reshape(b, s, B_HEADS, B_DV)
    off = 2 * B_QK + B_V
    i_log = pbf[..., off:off + B_HEADS] + i_bias.astype(F32)
    f_log = jax.nn.log_sigmoid(pbf[..., off + B_HEADS:off + 2 * B_HEADS] + f_bias.astype(F32))
    o_gate = jax.nn.sigmoid(pbf[..., off + 2 * B_HEADS:]).reshape(b, s, B_HEADS, B_DV)
    ob = mlstm_chunkwise(qb, kb, vb, i_log, f_log)
    ob = rmsnorm(ob, norm_b) * o_gate
    mixed = jnp.concatenate([oa.reshape(b, s, A_V), ob.reshape(b, s, B_V)], -1)
    return mixed.astype(hn.dtype) @ w_out


def rope(x, pos):
    half = x.shape[-1] // 2
    inv = ROPE_THETA ** (-jnp.arange(half, dtype=F32) / half)
    ang = pos.astype(F32)[..., None] * inv
    cos = jnp.cos(ang)[:, :, None, :]
    sin = jnp.sin(ang)[:, :, None, :]
    x1, x2 = x[..., :half], x[..., half:]
    return jnp.concatenate([x1 * cos - x2 * sin, x2 * cos + x1 * sin], -1)


def mixer_c(hn, pos, w_qkv, b_qkv, sinks, w_o, b_o):
    b, s, _ = hn.shape
    nb = s // WINDOW
    proj = (hn @ w_qkv + b_qkv).astype(F32)
    q = rope(proj[..., :C_Q].reshape(b, s, C_HEADS, C_HD), pos)
    q = q.reshape(b, nb, WINDOW, C_KV_HEADS, C_GROUP, C_HD)
    q = jnp.moveaxis(q, 1, 0)
    k = rope(proj[..., C_Q:C_Q + C_KV].reshape(b, s, C_KV_HEADS, C_HD), pos)
    v = proj[..., C_Q + C_KV:].reshape(b, s, C_KV_HEADS, C_HD)
    kp = jnp.pad(k, ((0, 0), (WINDOW, 0), (0, 0), (0, 0)))
    vp = jnp.pad(v, ((0, 0), (WINDOW, 0), (0, 0), (0, 0)))
    t_idx = jnp.arange(WINDOW)[:, None]
    s_idx = jnp.arange(2 * WINDOW)[None, :]
    band = (s_idx > t_idx) & (s_idx <= t_idx + WINDOW)
    sink = sinks.astype(F32).reshape(C_KV_HEADS, C_GROUP)[None, :, :, None]
    scale = C_HD ** -0.5

    def block(args):
        j, qj = args
        kj = lax.dynamic_slice_in_dim(kp, j * WINDOW, 2 * WINDOW, axis=1)
        vj = lax.dynamic_slice_in_dim(vp, j * WINDOW, 2 * WINDOW, axis=1)
        valid = band & (j * WINDOW - WINDOW + s_idx >= 0)
        sc = jnp.einsum('btkgd,bskd->bkgts', qj, kj) * scale
        sc = jnp.where(valid, sc, -jnp.inf)
        m = jnp.maximum(jnp.max(sc, -1), sink)
        pr = jnp.exp(sc - m[..., None])
        den = jnp.sum(pr, -1) + jnp.exp(sink - m)
        pr = pr / den[..., None]
        return jnp.einsum('bkgts,bskd->btkgd', pr, vj)

    o = lax.map(block, (jnp.arange(nb), q))
    o = jnp.moveaxis(o, 0, 1).reshape(b, s, C_Q)
    return o.astype(hn.dtype) @ w_o + b_o


def setup_inputs(seed: int = 0) -> dict:
    key = jax.random.key(seed)
    ks = jax.random.split(key, 24)
    nrm = jax.random.normal
    x = nrm(ks[0], (BATCH, SEQ, D_MODEL), F32)
    p = nrm(ks[1], (DEPTH, BATCH, SEQ, PLE_DIM), F32)
    positions = jnp.broadcast_to(jnp.arange(SEQ, dtype=jnp.int32), (BATCH, SEQ))
    norm_gains = 1.0 + 0.05 * nrm(ks[2], (DEPTH, 4, D_MODEL), F32)
    w_in_ab = nrm(ks[3], (N_EVEN, D_MODEL, IN_COLS), F32) * D_MODEL ** -0.5
    conv_a = nrm(ks[4], (N_EVEN, CONV_K, A_CONV_CH), F32) * CONV_K ** -0.5
    a_log = jnp.log(jax.random.uniform(ks[5], (N_EVEN, A_HEADS), F32, 1.0, 16.0))
    dt = jnp.exp(jax.random.uniform(ks[6], (N_EVEN, A_HEADS), F32, np.log(1e-3), np.log(1e-1)))
    dt_bias = dt + jnp.log(-jnp.expm1(-dt))
    norm_a = 1.0 + 0.05 * nrm(ks[7], (N_EVEN, A_DV), F32)
    i_bias_b = 0.1 * nrm(ks[8], (N_EVEN, B_HEADS), F32)
    f_bias_b = 3.0 + 0.5 * nrm(ks[9], (N_EVEN, B_HEADS), F32)
    norm_b = 1.0 + 0.05 * nrm(ks[10], (N_EVEN, B_HEADS, B_DV), F32)
    w_out_ab = nrm(ks[11], (N_EVEN, MIX_AB, D_MODEL), F32) * MIX_AB ** -0.5
    w_qkv_c = nrm(ks[12], (N_ODD, D_MODEL, C_COLS), F32) * D_MODEL ** -0.5
    b_qkv_c = 0.02 * nrm(ks[13], (N_ODD, C_COLS), F32)
    sinks_c = nrm(ks[14], (N_ODD, C_HEADS), F32)
    w_o_c = nrm(ks[15], (N_ODD, C_Q, D_MODEL), F32) * C_Q ** -0.5
    b_o_c = 0.02 * nrm(ks[16], (N_ODD, D_MODEL), F32)
    w_up = nrm(ks[17], (DEPTH, D_MODEL, D_FF), F32) * D_MODEL ** -0.5
    w_down = nrm(ks[18], (DEPTH, D_FF, D_MODEL), F32) * D_FF ** -0.5
    w_ple = nrm(ks[19], (DEPTH, PLE_DIM, D_MODEL), F32) * PLE_DIM ** -0.5
    w_ple_gate = nrm(ks[20], (DEPTH, D_MODEL, D_MODEL), F32) * D_MODEL ** -0.5
    return {"x": x, "p": p, "positions": positions, "norm_gains": norm_gains,
            "w_in_ab": w_in_ab, "conv_a": conv_a, "a_log": a_log, "dt_bias": dt_bias,
            "norm_a": norm_a, "i_bias_b": i_bias_b, "f_bias_b": f_bias_b, "norm_b": norm_b,
            "w_out_ab": w_out_ab, "w_qkv_c": w_qkv_c, "b_qkv_c": b_qkv_c, "sinks_c": sinks_c,
            "w_o_c": w_o_c, "b_o_c": b_o_c, "w_up": w_up, "w_down": w_down,
            "w_ple": w_ple, "w_ple_gate": w_ple_gate}


def reference(x, p, positions, norm_gains, w_in_ab, conv_a, a_log, dt_bias, norm_a,
              i_bias_b, f_bias_b, norm_b, w_out_ab, w_qkv_c, b_qkv_c, sinks_c, w_o_c, b_o_c,
              w_up, w_down, w_ple, w_ple_gate):
    h = x
    for layer in range(DEPTH):
        gains = norm_gains[layer]
        hn = rmsnorm(h, gains[0])
        if layer % 2 == 0:
            e = layer // 2
            mix = mixer_ab(hn, w_in_ab[e], conv_a[e], a_log[e], dt_bias[e], norm_a[e],
                           i_bias_b[e], f_bias_b[e], norm_b[e], w_out_ab[e])
        else:
            o = layer // 2
            mix = mixer_c(hn, positions, w_qkv_c[o], b_qkv_c[o], sinks_c[o], w_o_c[o], b_o_c[o])
        h = h + rmsnorm(mix, gains[1])
        hn = rmsnorm(h, gains[2])
        ff = jnp.square(jax.nn.relu(hn @ w_up[layer])) @ w_down[layer]
        h = h + rmsnorm(ff, gains[3])
        gate = jax.nn.sigmoid(h @ w_ple_gate[layer])
        h = h + gate * (p[layer] @ w_ple[layer])
    return h
```

```python
import numpy as np
import concourse.bass as bass
import concourse.mybir as mybir
from concourse.bass_utils import run_bass_kernel_spmd
from contextlib import ExitStack

F32 = mybir.dt.float32
BF16 = mybir.dt.bfloat16
I32 = mybir.dt.int32
AF = mybir.ActivationFunctionType
ALU = mybir.AluOpType
AX = mybir.AxisListType

ENGS = ("pe", "act", "dve", "pool", "sp")
SEM_EPOCH = 4000
DMA_K = 8

D_MODEL = 1024
SEQ = 2048
DEPTH = 4
D_FF = 4096
PLE = 256
EPS = 1e-6
NEG = -30000.0
A_COLS = 2056
IN_COLS = 4112
C_ID, C_MLS, C_MUI, C_BO, C_CS0, C_CS1, C_NMB, C_SWA, C_INV, C_ONE, C_TU, C_ML, C_NM, C_END = (
    0, 128, 256, 384, 512, 640, 768, 896, 1152, 1184, 1312, 1440, 1568, 1696)
RA = 656
RC = 1552


class Trk:
    __slots__ = ("name", "w", "rs", "excl")

    def __init__(self, name, excl=False):
        self.name = name
        self.w = None
        self.rs = []
        self.excl = excl


class V:
    __slots__ = ("ap", "trks")

    def __init__(self, ap, trks):
        self.ap = ap
        self.trks = tuple(trks)

    def __getitem__(self, idx):
        return V(self.ap[idx], self.trks)

    def bc(self, dt):
        return V(self.ap.bitcast(dt), self.trks)


class Ins:
    __slots__ = ("eng", "fn", "deps", "dma", "sig", "ev", "fc", "out")

    def __init__(self, eng, fn, deps, dma, out):
        self.eng = eng
        self.fn = fn
        self.deps = deps
        self.dma = dma
        self.sig = False
        self.ev = None
        self.fc = None
        self.out = out


class Sched:
    def __init__(self, nc, ctx):
        self.nc = nc
        self.ctx = ctx
        self.instrs = []
        self.per_eng = {e: [] for e in ENGS}
        self.uid = 0

    def sbuf(self, name, shape, dtype, ctx=None):
        self.uid += 1
        t = (ctx or self.ctx).enter_context(
            self.nc.sbuf_tensor("%s_%d" % (name, self.uid), list(shape), dtype))
        return V(t[tuple(slice(None) for _ in shape)], [Trk(name)])

    def psum(self, name, shape, dtype=F32):
        t = self.ctx.enter_context(self.nc.psum_tensor(name, list(shape), dtype))
        return V(t[tuple(slice(None) for _ in shape)], [Trk(name, excl=True)])

    def add(self, eng, fn, reads=(), writes=(), dma=False, out=False):
        me = len(self.instrs)
        deps = set()
        for r in reads:
            for t in r.trks:
                if t.excl:
                    if t.w is not None:
                        deps.add(t.w)
                    deps.update(t.rs)
                    t.w = me
                    t.rs = []
                else:
                    if t.w is not None:
                        deps.add(t.w)
                    t.rs.append(me)
        for w in writes:
            for t in w.trks:
                if t.w is not None:
                    deps.add(t.w)
                deps.update(t.rs)
                t.w = me
                t.rs = []
        deps.discard(me)
        self.instrs.append(Ins(eng, fn, deps, dma, out))
        self.per_eng[eng].append(me)
        return me

    def barrier(self):
        last = []
        for e in ENGS:
            lst = self.per_eng[e]
            nd = 0
            seen_c = False
            for idx in reversed(lst):
                ins = self.instrs[idx]
                if ins.fn is None:
                    continue
                if ins.dma:
                    if nd < DMA_K:
                        last.append(idx)
                        nd += 1
                elif not seen_c:
                    last.append(idx)
                    seen_c = True
                if nd >= DMA_K and seen_c:
                    break
        for e in ENGS:
            me = len(self.instrs)
            self.instrs.append(Ins(e, None, set(last), False, False))
            self.per_eng[e].append(me)

    def emit(self):
        nc = self.nc
        instrs = self.instrs
        for ins in instrs:
            for d in ins.deps:
                dd = instrs[d]
                if dd.dma:
                    continue
                if dd.eng == "pe" and ins.eng == "pe" and not ins.dma and ins.fn is not None:
                    continue
                dd.sig = True
        cnt = {e: 0 for e in ENGS}
        dcnt = {e: 0 for e in ENGS}
        for ins in instrs:
            if ins.fn is None:
                continue
            if ins.dma:
                j = dcnt[ins.eng]
                dcnt[ins.eng] += 1
                ins.ev = (("d", ins.eng, j % DMA_K), 16 * (j // DMA_K + 1))
                if j >= DMA_K:
                    ins.fc = (("d", ins.eng, j % DMA_K), 16 * (j // DMA_K))
            elif ins.sig:
                n = cnt[ins.eng]
                cnt[ins.eng] += 1
                ins.ev = (("c", ins.eng, n // SEM_EPOCH), n % SEM_EPOCH + 1)
        sems = {}
        for ins in instrs:
            if ins.ev is not None and ins.ev[0] not in sems:
                k = ins.ev[0]
                sems[k] = self.ctx.enter_context(nc.semaphore("s_%s_%s_%d" % k))
        out_events = [ins.ev for ins in instrs if ins.dma and ins.out]
        self.stats = {e: len(self.per_eng[e]) for e in ENGS}

        def run_engine(ename, eng):
            known = {}
            for idx in self.per_eng[ename]:
                ins = instrs[idx]
                best = {}
                for d in ins.deps:
                    dd = instrs[d]
                    if dd.ev is None:
                        continue
                    if (not dd.dma) and dd.eng == "pe" and ename == "pe" and not ins.dma \
                            and ins.fn is not None:
                        continue
                    k, v = dd.ev
                    if known.get(k, 0) < v and best.get(k, 0) < v:
                        best[k] = v
                if ins.fc is not None:
                    k, v = ins.fc
                    if known.get(k, 0) < v and best.get(k, 0) < v:
                        best[k] = v
                for k, v in best.items():
                    eng.wait_ge(sems[k], v)
                    known[k] = v
                if ins.fn is None:
                    continue
                bi = ins.fn(eng)
                if ins.ev is not None:
                    bi.then_inc(sems[ins.ev[0]], 16 if ins.dma else 1)
            if ename == "sp":
                best = {}
                for (k, v) in out_events:
                    if best.get(k, 0) < v:
                        best[k] = v
                for k, v in best.items():
                    if known.get(k, 0) < v:
                        eng.wait_ge(sems[k], v)

        with nc.Block() as block:
            @block.tensor
            def _(e):
                run_engine("pe", e)

            @block.scalar
            def _(e):
                run_engine("act", e)

            @block.vector
            def _(e):
                run_engine("dve", e)

            @block.gpsimd
            def _(e):
                run_engine("pool", e)

            @block.sync
            def _(e):
                run_engine("sp", e)


class KB:
    def __init__(self, nc, ctx, NSEQ, LAYERS, mix=("ab", "c")):
        self.nc = nc
        self.S = Sched(nc, ctx)
        self.NSEQ = NSEQ
        self.LAYERS = LAYERS
        self.mix = mix
        self.evi = 0
        self.psoi = 0
        self.psi = 0
        self.rot = {}

    def ev(self):
        self.evi += 1
        return "act" if self.evi % 2 else "dve"

    def ps(self):
        p = self.PS[self.psi % 8]
        self.psi += 1
        return p

    def ps6(self):
        p = self.PS[self.psi % 6]
        self.psi += 1
        return p

    def ACT(self, out, in_, func, bias=0.0, scale=1.0):
        reads = [in_]
        b, s = bias, scale
        if isinstance(bias, V):
            reads.append(bias)
            b = bias.ap
        if isinstance(scale, V):
            reads.append(scale)
            s = scale.ap
        self.S.add("act", lambda e: e.activation(out=out.ap, in_=in_.ap, func=func, bias=b, scale=s),
                   reads=reads, writes=[out])

    def CP(self, eng, out, in_):
        if eng == "act":
            self.S.add("act", lambda e: e.copy(out=out.ap, in_=in_.ap), reads=[in_], writes=[out])
        else:
            self.S.add(eng, lambda e: e.tensor_copy(out=out.ap, in_=in_.ap), reads=[in_], writes=[out])

    def TT(self, eng, out, a, b, op):
        self.S.add(eng, lambda e: e.tensor_tensor(out=out.ap, in0=a.ap, in1=b.ap, op=op),
                   reads=[a, b], writes=[out])

    def TS(self, eng, out, a, s1, op0, s2=None, op1=None):
        reads = [a]
        x1, x2 = s1, s2
        if isinstance(s1, V):
            reads.append(s1)
            x1 = s1.ap
        if isinstance(s2, V):
            reads.append(s2)
            x2 = s2.ap
        if op1 is None:
            self.S.add(eng, lambda e: e.tensor_scalar(out=out.ap, in0=a.ap, scalar1=x1, scalar2=None, op0=op0),
                       reads=reads, writes=[out])
        else:
            self.S.add(eng, lambda e: e.tensor_scalar(out=out.ap, in0=a.ap, scalar1=x1, scalar2=x2,
                                                      op0=op0, op1=op1), reads=reads, writes=[out])

    def STT(self, eng, out, a, s, b, op0, op1):
        reads = [a, b]
        x = s
        if isinstance(s, V):
            reads.append(s)
            x = s.ap
        self.S.add(eng, lambda e: e.scalar_tensor_tensor(out=out.ap, in0=a.ap, scalar=x, in1=b.ap,
                                                         op0=op0, op1=op1), reads=reads, writes=[out])

    def RED(self, eng, out, in_, op):
        self.S.add(eng, lambda e: e.tensor_reduce(out=out.ap, in_=in_.ap, axis=AX.X, op=op),
                   reads=[in_], writes=[out])

    def RCP(self, out, in_):
        self.S.add("dve", lambda e: e.reciprocal(out=out.ap, in_=in_.ap), reads=[in_], writes=[out])

    def MM(self, ps, lhsT, rhs, start=True, stop=True):
        self.S.add("pe", lambda e: e.matmul(ps.ap, lhsT=lhsT.ap, rhs=rhs.ap, start=start, stop=stop),
                   reads=[lhsT, rhs], writes=[ps])

    def TR(self, ps, in_, ident):
        self.S.add("pe", lambda e: e.transpose(out=ps.ap, in_=in_.ap, identity=ident.ap),
                   reads=[in_, ident], writes=[ps])

    def DMA(self, q, out, in_, is_out=False):
        if is_out:
            self.S.add(q, lambda e: e.dma_start(out=out, in_=in_.ap), reads=[in_], dma=True, out=True)
        else:
            self.S.add(q, lambda e: e.dma_start(out=out.ap, in_=in_), writes=[out], dma=True)

    def dbg(self, name, v, dtype=F32):
        if not getattr(self, "debug", False):
            return
        shp = list(v.ap.shape)
        d = self.nc.dram_tensor(name, shp, dtype, kind="ExternalOutput").ap()
        self.DMA("sp", d, v, is_out=True)

    def R(self, key, n, mk):
        if key not in self.rot:
            self.rot[key] = [[mk(i) for i in range(n)], 0]
        lst = self.rot[key]
        v = lst[0][lst[1] % n]
        lst[1] += 1
        return v

    def build(self, dr):
        S = self.S
        nc = self.nc
        self.dr = dr
        sb = S.sbuf
        self.PS = [S.psum("ps%d" % i, [128, 512], F32) for i in range(8)]
        self.h = [[sb("h", [128, 512], F32) for q in range(4)] for c in range(8)]
        self.hn = [[sb("hn", [128, 512], BF16) for q in range(4)] for c in range(8)]
        self.cst = sb("cst", [128, C_END], F32)
        self.gcol = sb("gcol", [128, 128], F32)
        self.convw = sb("convw", [128, 96], F32)
        self.rows_ab = sb("rows_ab", [128, 2 * RA], F32)
        self.bo = sb("bo", [128, 16], F32)
        self.identb = sb("identb", [128, 128], BF16)
        self.onesb = sb("onesb", [128, 128], BF16)
        self.cos = sb("cos", [128, 16, 32], F32)
        self.sin = sb("sin", [128, 16, 32], F32)
        self.lvlm = sb("lvlm", [128, 1792], BF16)
        self.DMA("pool", self.lvlm, dr["lvlmask"])
        self.DMA("sp", self.cst, dr["consts"])
        self.DMA("sp", self.gcol, dr["gcols"])
        self.DMA("sp", self.convw, dr["convw"])
        self.DMA("sp", self.rows_ab, dr["rows_ab"])
        self.DMA("sp", self.bo, dr["bo_cols"])
        self.CP("dve", self.identb, self.cst[:, C_ID:C_ID + 128])
        self.CP("dve", self.onesb, self.cst[:, C_ONE:C_ONE + 128])
        self.negpi = sb("negpi", [128, 1], F32)
        S.add("pool", lambda e: e.memset(self.negpi.ap, -float(np.pi)), writes=[self.negpi])
        self.identf = self.cst[:, C_ID:C_ID + 128]
        self.onesf = self.cst[:, C_ONE:C_ONE + 128]

        for b in range(self.NSEQ):
            for c in range(8):
                for q in range(4):
                    self.DMA("sp", self.h[c][q], dr["xT"][b, c * 128:(c + 1) * 128, q * 512:(q + 1) * 512])
            if self.LAYERS > 1 and "c" in self.mix:
                self.rope_tables(b)
            for l in range(self.LAYERS):
                self.layer(b, l)
            for c in range(8):
                for q in range(4):
                    self.DMA("sp", dr["outT"][b, c * 128:(c + 1) * 128, q * 512:(q + 1) * 512],
                             self.h[c][q], is_out=True)
        S.emit()

    def gc(self, l, j, c):
        k = (l * 4 + j) * 8 + c
        return self.gcol[:, k:k + 1]

    def rstd_quarter(self, srcs, ctx):
        S = self.S
        ps = self.ps()
        for c in range(8):
            sq = self.R("sq", 2, lambda i: S.sbuf("sq", [128, 512], BF16, ctx))
            if c % 2 == 0:
                self.ACT(sq, srcs[c], AF.Square)
            else:
                self.TT("dve", sq, srcs[c], srcs[c], ALU.mult)
            self.MM(ps, self.onesb, sq, start=(c == 0), stop=(c == 7))
        rstd = self.R("rstd", 2, lambda i: S.sbuf("rstd", [128, 512], F32, ctx))
        self.ACT(rstd, ps, AF.Ln, bias=EPS, scale=1.0 / D_MODEL)
        self.ACT(rstd, rstd, AF.Exp, scale=-0.5)
        return rstd

    def norm_to_hn(self, l, j, ctx, quarters=range(4)):
        for q in quarters:
            rstd = self.rstd_quarter([self.h[c][q] for c in range(8)], ctx)
            for c in range(8):
                self.STT("dve", self.hn[c][q], self.h[c][q], self.gc(l, j, c), rstd, ALU.mult, ALU.mult)

    def add_normed(self, l, j, q, srcs, ctx):
        rstd = self.rstd_quarter(srcs, ctx)
        for c in range(8):
            if c % 4 == 3:
                self.TT("pool", srcs[c], srcs[c], rstd, ALU.mult)
                self.TS("pool", srcs[c], srcs[c], self.gc(l, j, c), ALU.mult)
                self.TT("pool", self.h[c][q], self.h[c][q], srcs[c], ALU.add)
            else:
                self.TT("dve", srcs[c], srcs[c], rstd, ALU.mult)
                self.STT("dve", self.h[c][q], srcs[c], self.gc(l, j, c), self.h[c][q], ALU.mult, ALU.add)

    def layer(self, b, l):
        S = self.S
        with ExitStack() as ctx:
            self.rot = {}
            self.norm_to_hn(l, 0, ctx)
            self.mixT = [[S.sbuf("mixT", [128, 512], BF16, ctx) for q in range(4)] for c in range(8)]
            kind = "ab" if l % 2 == 0 else "c"
            if kind in self.mix:
                if kind == "ab":
                    with ExitStack() as c2:
                        self.mixer_ab(b, l, c2)
                        S.barrier()
                    self.rot = dict((k, v) for k, v in self.rot.items() if k in ("sq", "rstd", "ntmp"))
                else:
                    self.mixer_c(b, l, ctx)
                self.out_proj(b, l, ctx)
            S.barrier()
        with ExitStack() as ctx:
            self.rot = {}
            self.ffn(b, l, ctx)
            S.barrier()
        with ExitStack() as ctx:
            self.rot = {}
            self.ple(b, l, ctx)
            S.barrier()

    def out_proj(self, b, l, ctx):
        S = self.S
        if l == 1:
            for c in range(8):
                self.dbg("d_mixT%d" % c, self.mixT[c][0], BF16)
        kind = "ab" if l % 2 == 0 else "c"
        wsrc = self.dr["w_out_ab"][l // 2] if kind == "ab" else self.dr["w_o_c"][l // 2]
        wv = wsrc.rearrange("(kc p) n -> p kc n", p=128)
        w = [S.sbuf("wout", [128, 8, 256], BF16, ctx) for i in range(4)]
        for i in range(4):
            self.DMA("pool", w[i], wv[:, :, i * 256:(i + 1) * 256])
        for q in range(4):
            srcs = []
            for m in range(8):
                ps = self.ps()
                for kc in range(8):
                    self.MM(ps, w[m // 2][:, kc, (m % 2) * 128:(m % 2) * 128 + 128], self.mixT[kc][q],
                            start=(kc == 0), stop=(kc == 7))
                t = self.R("mo", 8, lambda i: S.sbuf("mo", [128, 512], F32, ctx))
                if kind == "c":
                    k = (l // 2) * 8 + m
                    self.ACT(t, ps, AF.Identity, bias=self.bo[:, k:k + 1])
                else:
                    self.CP(self.ev(), t, ps)
                srcs.append(t)
            self.add_normed(l, 1, q, srcs, ctx)

    def ffn(self, b, l, ctx):
        S = self.S
        wup = self.dr["w_up"][l].rearrange("(kc p) n -> p kc n", p=128)
        wdn = self.dr["w_down"][l].rearrange("(kc p) n -> p kc n", p=128)
        act = [[S.sbuf("act", [128, 512], BF16, ctx) for n in range(2)] for k in range(16)]
        fft = [[S.sbuf("fft", [128, 512], F32, ctx) for n in range(2)] for m in range(8)]
        wslots = [S.sbuf("wffn", [128, 4096], BF16, ctx) for i in range(2)]
        wi = [0]

        def wslot():
            v = wslots[wi[0] % 2]
            wi[0] += 1
            return v

        for half in range(2):
            qs = [2 * half, 2 * half + 1]
            self.norm_to_hn(l, 2, ctx, quarters=qs)
            for fh in range(2):
                for g in range(4):
                    ws = wslot()
                    wu = V(ws.ap.rearrange("p (kc n) -> p kc n", kc=8), ws.trks)
                    c0 = fh * 2048 + g * 512
                    self.DMA("pool", wu, wup[:, :, c0:c0 + 512])
                    for m in range(4):
                        for n in range(2):
                            ps = self.ps()
                            for kc in range(8):
                                self.MM(ps, wu[:, kc, m * 128:(m + 1) * 128], self.hn[kc][qs[n]],
                                        start=(kc == 0), stop=(kc == 7))
                            r = self.R("relu", 2, lambda i: S.sbuf("relu", [128, 512], F32, ctx))
                            self.ACT(r, ps, AF.Relu)
                            self.TT("dve", act[g * 4 + m][n], r, r, ALU.mult)
                for g in range(4):
                    ws = wslot()
                    wd = V(ws.ap.rearrange("p (kc n) -> p kc n", kc=16), ws.trks)
                    self.DMA("pool", wd, wdn[:, fh * 16:(fh + 1) * 16, g * 256:(g + 1) * 256])
                    pss = [[self.ps() for n in range(2)] for m in range(2)]
                    for kc in range(16):
                        for m in range(2):
                            for n in range(2):
                                self.MM(pss[m][n], wd[:, kc, m * 128:(m + 1) * 128], act[kc][n],
                                        start=(kc == 0), stop=(kc == 15))
                    for m in range(2):
                        for n in range(2):
                            dst = fft[g * 2 + m][n]
                            if fh == 0:
                                self.CP(self.ev(), dst, pss[m][n])
                            else:
                                self.TT("dve", dst, dst, pss[m][n], ALU.add)
            for n in range(2):
                self.add_normed(l, 3, qs[n], [fft[m][n] for m in range(8)], ctx)

    def ple(self, b, l, ctx):
        S = self.S
        wg = S.sbuf("wg", [128, 8, 1024], BF16, ctx)
        wp = S.sbuf("wp", [128, 2, 1024], BF16, ctx)
        pT = [S.sbuf("pT", [128, 2, 512], BF16, ctx) for q in range(4)]
        wgv = self.dr["w_ple_gate"][l].rearrange("(kc p) n -> p kc n", p=128)
        for i in range(2):
            self.DMA("pool", wg[:, :, i * 512:(i + 1) * 512], wgv[:, :, i * 512:(i + 1) * 512])
        self.DMA("pool", wp, self.dr["w_ple"][l].rearrange("(kc p) n -> p kc n", p=128))
        pv = self.dr["pT"][l, b].rearrange("(kc p) t -> p kc t", p=128)
        for q in range(4):
            self.DMA("pool", pT[q], pv[:, :, q * 512:(q + 1) * 512])
        for q in range(4):
            for c in range(8):
                self.CP("act" if c % 2 else "pool", self.hn[c][q], self.h[c][q])
            for m in range(8):
                psg = self.ps()
                for kc in range(8):
                    self.MM(psg, wg[:, kc, m * 128:(m + 1) * 128], self.hn[kc][q], start=(kc == 0), stop=(kc == 7))
                psp = self.ps()
                for kc in range(2):
                    self.MM(psp, wp[:, kc, m * 128:(m + 1) * 128], pT[q][:, kc, :], start=(kc == 0), stop=(kc == 1))
                sg = self.R("sg", 3, lambda i: S.sbuf("sg", [128, 512], F32, ctx))
                self.ACT(sg, psg, AF.Sigmoid)
                self.TT("dve", sg, sg, psp, ALU.mult)
                self.TT("pool", self.h[m][q], self.h[m][q], sg, ALU.add)

    def rope_tables(self, b):
        S = self.S
        with ExitStack() as ctx:
            posi = S.sbuf("posi", [128, 16], I32, ctx)
            posf = S.sbuf("posf", [128, 16], F32, ctx)
            y0 = S.sbuf("y0", [128, 16, 32], F32, ctx)
            yy = S.sbuf("yy", [128, 16, 32], F32, ctx)
            ki = S.sbuf("ki", [128, 16, 32], I32, ctx)
            kf = S.sbuf("kf", [128, 16, 32], F32, ctx)
            mk = S.sbuf("mk", [128, 16, 32], F32, ctx)
            self.DMA("sp", posi, self.dr["pos"][b])
            self.CP("dve", posf, posi)
            inv = self.cst[:, C_INV:C_INV + 32]
            invb = V(inv.ap.unsqueeze(1).to_broadcast([128, 16, 32]), inv.trks)
            posb = V(posf.ap.unsqueeze(2).to_broadcast([128, 16, 32]), posf.trks)
            self.TT("dve", y0, invb, posb, ALU.mult)
            for dst, off in ((self.sin, 0.5), (self.cos, 0.75)):
                self.TS("dve", yy, y0, 1.0 / (2.0 * np.pi), ALU.mult, off, ALU.add)
                self.CP("dve", ki, yy)
                self.CP("dve", kf, ki)
                self.TT("dve", yy, yy, kf, ALU.subtract)
                self.TS("dve", mk, yy, 0.0, ALU.is_lt)
                self.TT("dve", yy, yy, mk, ALU.add)
                self.ACT(dst, yy, AF.Sin, bias=self.negpi, scale=2.0 * np.pi)
            S.barrier()

    def mixer_c(self, b, l, ctx0):
        S = self.S
        o = l // 2
        rows = S.sbuf("rowsc", [128, RC], F32, ctx0)
        self.DMA("sp", rows, self.dr["rows_c"][o])
        wv = self.dr["w_qkv_c"][o].rearrange("(kc p) n -> p kc n", p=128)
        for g in range(4):
          with ExitStack() as ctx:
            self.rot = dict((k, v) for k, v in self.rot.items() if k in ("sq", "rstd", "ntmp", "mo"))
            wq = S.sbuf("wq", [128, 8, 384], BF16, ctx)
            self.DMA("pool", wq[:, :, 0:256], wv[:, :, g * 256:(g + 1) * 256])
            self.DMA("pool", wq[:, :, 256:320], wv[:, :, 1024 + g * 64:1024 + (g + 1) * 64])
            self.DMA("pool", wq[:, :, 320:384], wv[:, :, 1280 + g * 64:1280 + (g + 1) * 64])
            brow = S.sbuf("brow", [128, 384], F32, ctx)
            self.CP("pool", brow[:, 0:256], rows[:, g * 256:(g + 1) * 256])
            self.CP("pool", brow[:, 256:320], rows[:, 1024 + g * 64:1024 + (g + 1) * 64])
            self.CP("pool", brow[:, 320:384], rows[:, 1280 + g * 64:1280 + (g + 1) * 64])
            qTall = S.sbuf("qTall", [128, 2, 2048], BF16, ctx)
            kTall = S.sbuf("kTall", [128, 2048], BF16, ctx)
            qTj = [V(qTall.ap[:, :, j * 128:(j + 1) * 128], [Trk("qTj")]) for j in range(16)]
            kTj = [V(kTall.ap[:, j * 128:(j + 1) * 128], [Trk("kTj")]) for j in range(16)]
            vtok = [S.sbuf("vtok", [128, 64], BF16, ctx) for j in range(16)]
            for j in range(16):
                qkv = self.R("qkv", 2, lambda i: S.sbuf("qkv", [128, 384], F32, ctx))
                ps = self.ps6()
                for kc in range(8):
                    self.MM(ps[:, 0:384], self.hn[kc][j // 4][:, (j % 4) * 128:(j % 4) * 128 + 128],
                            wq[:, kc, :], start=(kc == 0), stop=(kc == 7))
                self.TT("dve", qkv, ps[:, 0:384], brow, ALU.add)
                qr = self.R("qr", 2, lambda i: S.sbuf("qr", [128, 256], BF16, ctx))
                kd = self.R("kd", 2, lambda i: S.sbuf("kd", [128, 2, 64], BF16, ctx))
                for (src, dst, nh) in ((qkv[:, 0:256], qr, 4), (qkv[:, 256:320], kd[:, 0, :], 1)):
                    sv = V(src.ap.rearrange("p (h t d) -> p h t d", t=2, d=32), src.trks)
                    dv = V(dst.ap.rearrange("p (h t d) -> p h t d", t=2, d=32), dst.trks)
                    cb = V(self.cos.ap[:, j, :].unsqueeze(1).to_broadcast([128, nh, 32]), self.cos.trks)
                    sb_ = V(self.sin.ap[:, j, :].unsqueeze(1).to_broadcast([128, nh, 32]), self.sin.trks)
                    t1 = self.R("rt1", 2, lambda i: S.sbuf("rt1", [128, 4, 32], F32, ctx))
                    t2 = self.R("rt2", 2, lambda i: S.sbuf("rt2", [128, 4, 32], F32, ctx))
                    t3 = self.R("rt3", 2, lambda i: S.sbuf("rt3", [128, 4, 32], F32, ctx))
                    t4 = self.R("rt4", 2, lambda i: S.sbuf("rt4", [128, 4, 32], F32, ctx))
                    self.TT("dve", t1[:, 0:nh, :], sv[:, :, 0, :], cb, ALU.mult)
                    self.TT("dve", t2[:, 0:nh, :], sv[:, :, 1, :], sb_, ALU.mult)
                    self.TT("dve", dv[:, :, 0, :], t1[:, 0:nh, :], t2[:, 0:nh, :], ALU.subtract)
                    self.TT("pool", t3[:, 0:nh, :], sv[:, :, 1, :], cb, ALU.mult)
                    self.TT("pool", t4[:, 0:nh, :], sv[:, :, 0, :], sb_, ALU.mult)
                    self.TT("pool", dv[:, :, 1, :], t3[:, 0:nh, :], t4[:, 0:nh, :], ALU.add)
                self.CP("act", kd[:, 1, :], kd[:, 0, :])
                self.CP("act", vtok[j], qkv[:, 320:384])
                pst = self.ps6().bc(BF16)
                for i in range(2):
                    self.TR(pst[:, i * 128:(i + 1) * 128], qr[:, i * 128:(i + 1) * 128], self.identb)
                kdf = V(kd.ap.rearrange("p a d -> p (a d)"), kd.trks)
                self.TR(pst[:, 256:384], kdf, self.identb)
                src = V(pst.ap[:, 0:256].rearrange("p (i t) -> p i t", t=128), pst.trks)
                self.CP("act", qTj[j], src)
                self.CP("dve", kTj[j], pst[:, 256:384])
            for j in range(16):
                nk = 1 if j == 0 else 2
                SS = nk * 128
                mask = self.cst[:, C_SWA + 256 - SS:C_SWA + 256]
                pso = self.PS[6 + (self.psoi % 2)]
                self.psoi += 1
                recg = self.R("recg", 2, lambda i: S.sbuf("recg", [128, 4], F32, ctx))
                ktr = [kTj[j - 1], kTj[j]] if j > 0 else [kTj[0]]
                for hl in range(4):
                    hd = 4 * g + hl
                    i = hl // 2
                    hh = hl % 2
                    ps = self.ps6()
                    for kb in range(nk):
                        self.MM(ps[:, kb * 128:(kb + 1) * 128],
                                qTj[j][hh * 64:(hh + 1) * 64, i, :], ktr[kb][hh * 64:(hh + 1) * 64, :])
                    sc = self.R("sc", 3, lambda i_: S.sbuf("sc", [128, 256], F32, ctx))
                    self.STT("dve", sc[:, 0:SS], ps[:, 0:SS], 0.125, mask, ALU.mult, ALU.add)
                    mx = self.R("mx", 3, lambda i_: S.sbuf("mx", [128, 1], F32, ctx))
                    self.RED("dve", mx, sc[:, 0:SS], ALU.max)
                    sk = rows[:, 1536 + hd:1536 + hd + 1]
                    self.TT("dve", mx, mx, sk, ALU.max)
                    nmx = self.R("nmx", 3, lambda i_: S.sbuf("nmx", [128, 1], F32, ctx))
                    self.TS("dve", nmx, mx, -1.0, ALU.mult)
                    pr = self.R("pr", 3, lambda i_: S.sbuf("pr", [128, 256], BF16, ctx))
                    self.ACT(pr[:, 0:SS], sc[:, 0:SS], AF.Exp, bias=nmx)
                    rs = self.R("rs", 3, lambda i_: S.sbuf("rs", [128, 1], F32, ctx))
                    self.RED("dve", rs, pr[:, 0:SS], ALU.add)
                    es = self.R("es", 3, lambda i_: S.sbuf("es", [128, 1], F32, ctx))
                    self.TT("dve", es, sk, mx, ALU.subtract)
                    self.ACT(es, es, AF.Exp)
                    self.TT("dve", rs, rs, es, ALU.add)
                    self.RCP(recg[:, hl:hl + 1], rs)
                    pst = self.ps6().bc(BF16)
                    for kb in range(nk):
                        self.TR(pst[:, kb * 128:(kb + 1) * 128], pr[:, kb * 128:(kb + 1) * 128], self.identb)
                    prT = self.R("prT", 3, lambda i_: S.sbuf("prT", [128, 256], BF16, ctx))
                    self.CP(self.ev(), prT[:, 0:SS], pst[:, 0:SS])
                    for kb in range(nk):
                        jk = j - 1 + kb if j > 0 else 0
                        self.MM(pso[:, hl * 64:hl * 64 + 64], prT[:, kb * 128:(kb + 1) * 128],
                                vtok[jk], start=(kb == 0), stop=(kb == nk - 1))
                otok = self.R("otok", 2, lambda i: S.sbuf("otok", [128, 256], BF16, ctx))
                psov = V(pso.ap[:, 0:256].rearrange("p (h d) -> p h d", d=64), pso.trks)
                recb = V(recg.ap.unsqueeze(2).to_broadcast([128, 4, 64]), recg.trks)
                ov = V(otok.ap.rearrange("p (h d) -> p h d", d=64), otok.trks)
                self.TT("dve", ov, psov, recb, ALU.mult)
                pst = self.ps6().bc(BF16)
                for i in range(2):
                    self.TR(pst[:, i * 128:(i + 1) * 128], otok[:, i * 128:(i + 1) * 128], self.identb)
                for i in range(2):
                    self.CP(self.ev(), self.mixT[2 * g + i][j // 4][:, (j % 4) * 128:(j % 4) * 128 + 128],
                            pst[:, i * 128:(i + 1) * 128])
            S.barrier()

    def mixer_ab(self, b, l, ctx0):
        S = self.S
        e = l // 2
        rab = lambda a, n: self.rows_ab[:, e * RA + a:e * RA + a + n]
        win = self.dr["w_in_ab"][e].rearrange("(kc p) n -> p kc n", p=128)
        sb = lambda name, shape, dt=F32: S.sbuf(name, shape, dt, ctx0)
        identf, onesf, identb = self.identf, self.onesf, self.identb
        triU = self.cst[:, C_TU:C_TU + 128]
        maskL = self.cst[:, C_ML:C_ML + 128]
        negm = self.cst[:, C_NM:C_NM + 128]
        wgate = sb("wgate", [128, 8, 16], BF16)
        self.DMA("pool", wgate[:, :, 0:8], win[:, :, 1536:1544])
        self.DMA("pool", wgate[:, :, 8:16], win[:, :, 3592:3600])
        G = sb("G", [128, 16, 16])
        for j in range(16):
            ps = self.ps()
            for kc in range(8):
                self.MM(ps[:, 0:16], self.hn[kc][j // 4][:, (j % 4) * 128:(j % 4) * 128 + 128], wgate[:, kc, :],
                        start=(kc == 0), stop=(kc == 7))
            self.CP(self.ev(), G[:, j, :], ps[:, 0:16])
        bc4 = lambda v: V(v.ap.unsqueeze(1).to_broadcast([128, 16, 4]), v.trks)
        BETA = sb("BETA", [128, 16, 4])
        self.ACT(BETA, G[:, :, 0:4], AF.Sigmoid)
        X = sb("X", [128, 16, 4])
        T1 = sb("T1", [128, 16, 4])
        T2 = sb("T2", [128, 16, 4])
        SP = sb("SP", [128, 16, 4])

        def softplus(dst, x):
            self.TS("dve", T1, x, -1.0, ALU.mult)
            self.TT("dve", T1, T1, x, ALU.min)
            self.ACT(T1, T1, AF.Exp)
            self.ACT(T1, T1, AF.Ln, bias=1.0)
            self.TS("dve", T2, x, 0.0, ALU.max)
            self.TT("dve", dst, T1, T2, ALU.add)

        GF = sb("GF", [128, 16, 8])
        self.TT("dve", X, G[:, :, 4:8], bc4(rab(0, 4)), ALU.add)
        softplus(SP, X)
        negA = sb("negA", [128, 4])
        self.ACT(negA, rab(4, 4), AF.Exp)
        self.TS("dve", negA, negA, -1.0, ALU.mult)
        self.TT("dve", GF[:, :, 0:4], SP, bc4(negA), ALU.mult)
        ILOG = sb("ILOG", [128, 16, 4])
        self.TT("dve", ILOG, G[:, :, 8:12], bc4(rab(8, 4)), ALU.add)
        self.TT("dve", X, G[:, :, 12:16], bc4(rab(12, 4)), ALU.add)
        self.TS("dve", X, X, -1.0, ALU.mult)
        softplus(SP, X)
        self.TS("dve", GF[:, :, 4:8], SP, -1.0, ALU.mult)
        CUM = sb("CUM", [128, 16, 16])
        for j in range(16):
            ps = self.ps()
            self.MM(ps[:, 0:8], triU, GF[:, j, :])
            self.MM(ps[:, 8:16], onesf, GF[:, j, :])
            self.CP(self.ev(), CUM[:, j, :], ps[:, 0:16])
        gcum, bcum, gtot = CUM[:, :, 0:4], CUM[:, :, 4:8], CUM[:, :, 8:12]
        BG = sb("BG", [128, 16, 4])
        KD = sb("KD", [128, 16, 4])
        EGL = sb("EGL", [128, 16, 4])
        RR = sb("RR", [128, 16, 4])
        self.ACT(BG, gcum, AF.Exp)
        self.TT("dve", BG, BG, BETA, ALU.mult)
        self.TT("dve", KD, gtot, gcum, ALU.subtract)
        self.ACT(KD, KD, AF.Exp)
        self.ACT(EGL, gtot, AF.Exp)
        self.TT("dve", RR, ILOG, bcum, ALU.subtract)
        cols = [i * 128 for i in range(4)]
        vexts = [sb("vext", [128, 129], BF16) for _ in range(2)]
        for v in vexts:
            S.add("pool", lambda e_, v=v: e_.memset(v.ap[:, 128:129], 1.0), writes=[v])
        wh = sb("wh", [128, 8, 1024], BF16)
        Sa = sb("Sa", [128, 128])
        Sab = sb("Sab", [128, 128], BF16)
        Cx = sb("Cx", [128, 129])
        Cb = sb("Cb", [128, 129], BF16)
        mst = sb("mst", [128, 1])
        cbuf = [sb("cbuf", [128, 515]) for _ in range(3)]
        qT = sb("qT", [128, 512], BF16)
        kT = sb("kT", [128, 512], BF16)
        vT = sb("vT", [128, 512], BF16)
        qbT = sb("qbT", [128, 512], BF16)
        kbT = sb("kbT", [128, 512], BF16)
        acc = sb("acc", [128, 512])
        sl = acc
        t128 = lambda name, dt=F32: self.R(name, 1, lambda i_: S.sbuf(name, [128, 128], dt, ctx0))
        c1 = lambda name, n=1: self.R(name, 2, lambda i_: S.sbuf(name, [128, n], F32, ctx0))

        def finish(o, gain, gate, ci, q, tsl):
            tmp = t128("f_tmp")
            self.ACT(tmp, o, AF.Square)
            ss = c1("f_ss")
            self.RED("dve", ss, tmp, ALU.add)
            self.ACT(ss, ss, AF.Ln, bias=EPS, scale=1.0 / 128.0)
            self.ACT(ss, ss, AF.Exp, scale=-0.5)
            y = t128("f_y")
            self.STT("dve", y, o, ss, gain, ALU.mult, ALU.mult)
            yb = t128("f_yb", BF16)
            self.TT("dve", yb, y, gate, ALU.mult)
            pY = self.ps().bc(BF16)
            self.TR(pY[:, 0:128], yb, identb)
            self.CP(self.ev(), self.mixT[ci][q][:, tsl], pY[:, 0:128])

        for i in range(4):
            gcols = [i * 128, 512 + i * 128, 1024 + i * 128, 2056 + i * 128, 2568 + i * 128,
                     3080 + i * 128, 3600 + i * 128, 1544 + i * 128]
            for gi, c0 in enumerate(gcols):
                self.DMA("pool", wh[:, :, gi * 128:(gi + 1) * 128], win[:, :, c0:c0 + 128])
            for t_ in (Sa, Sab, Cx, Cb, mst):
                S.add("pool", lambda e_, t_=t_: e_.memset(t_.ap, 0.0), writes=[t_])
            for g in range(3):
                S.add("pool", lambda e_, t_=cbuf[g]: e_.memset(t_.ap[:, 0:3], 0.0), writes=[cbuf[g]])
            for q in range(4):
                for gi in range(5):
                    ps = self.ps()
                    for kc in range(8):
                        self.MM(ps, wh[:, kc, gi * 128:(gi + 1) * 128], self.hn[kc][q], start=(kc == 0), stop=(kc == 7))
                    if gi < 3:
                        cb_ = cbuf[gi]
                        self.CP(self.ev(), cb_[:, 3:515], ps)
                        wc = lambda k: self.convw[:, ((e * 4 + i) * 3 + gi) * 4 + k:((e * 4 + i) * 3 + gi) * 4 + k + 1]
                        self.TS("dve", acc, cb_[:, 0:512], wc(0), ALU.mult)
                        for k in range(1, 4):
                            self.STT("dve", acc, cb_[:, k:k + 512], wc(k), acc, ALU.mult, ALU.add)
                        self.CP("pool", cb_[:, 0:3], cb_[:, 512:515])
                        self.ACT(sl, acc, AF.Silu)
                        if gi < 2:
                            sqb = self.R("sq", 2, lambda i_: S.sbuf("sq", [128, 512], BF16, ctx0))
                            self.ACT(sqb, sl, AF.Square)
                            p2 = self.ps()
                            self.MM(p2, self.onesb, sqb)
                            rn5 = p2
                            self.ACT(rn5, p2, AF.Ln, bias=EPS)
                            self.ACT(rn5, rn5, AF.Exp, scale=-0.5)
                            if gi == 0:
                                self.STT("dve", qT, sl, 128.0 ** -0.5, rn5, ALU.mult, ALU.mult)
                            else:
                                self.TT("dve", kT, sl, rn5, ALU.mult)
                        else:
                            self.CP("pool", vT, sl)
                    elif gi == 3:
                        self.ACT(qbT, ps, AF.Copy, scale=128.0 ** -0.5)
                    else:
                        self.CP("dve", kbT, ps)
                for jj in range(4):
                    j = 4 * q + jj
                    tsl = slice(jj * 128, (jj + 1) * 128)
                    col = lambda T_, off=0: T_[:, j, off + i:off + i + 1]
                    pt = self.ps()
                    for kc in range(8):
                        self.MM(pt, self.hn[kc][q][:, tsl], wh[:, kc, 512:1024], start=(kc == 0), stop=(kc == 7))
                    kbtok = t128("kbtok")
                    self.CP("act", kbtok, pt[:, 0:128])
                    vext = vexts[j % 2]
                    self.CP("dve", vext[:, 0:128], pt[:, 128:256])
                    og = t128("og")
                    self.ACT(og, pt[:, 256:384], AF.Sigmoid)
                    sz = t128("sz")
                    self.ACT(sz, pt[:, 384:512], AF.Silu)
                    ptr = self.ps().bc(BF16)
                    self.TR(ptr[:, 0:128], kT[:, tsl], identb)
                    self.TR(ptr[:, 128:256], vT[:, tsl], identb)
                    beta, gc_, bg, kdc, egl = col(BETA), col(CUM), col(BG), col(KD), col(EGL)
                    Rm = self.R("Rm", 1, lambda i_: S.sbuf("Rm", [128, 256], F32, ctx0))
                    self.TS("dve", Rm[:, 0:128], ptr[:, 128:256], beta, ALU.mult)
                    self.TS("dve", Rm[:, 128:256], ptr[:, 0:128], bg, ALU.mult)
                    kdec = t128("kdec", BF16)
                    self.TS("dve", kdec, ptr[:, 0:128], kdc, ALU.mult)
                    dg = t128("dg")
                    self.TS("dve", dg, identf, gc_, ALU.mult)
                    pG = self.ps()
                    self.MM(pG[:, 0:128], onesf, dg)
                    D1 = t128("D1")
                    self.STT("dve", D1, pG[:, 0:128], gc_, maskL, ALU.subtract, ALU.mult)
                    DT = t128("DT")
                    self.STT("dve", DT, pG[:, 0:128], gc_, triU, ALU.subtract, ALU.mult)
                    EgR = pG[:, 0:128]
                    self.ACT(EgR, pG[:, 0:128], AF.Exp)
                    self.ACT(D1, D1, AF.Exp, scale=-1.0)
                    self.TT("dve", D1, D1, maskL, ALU.mult)
                    self.ACT(DT, DT, AF.Exp)
                    self.TT("dve", DT, DT, triU, ALU.mult)
                    qdT = t128("qdT", BF16)
                    self.TT("dve", qdT, qT[:, tsl], EgR, ALU.mult)
                    pK = self.ps()
                    self.MM(pK[:, 0:128], kT[:, tsl], kT[:, tsl])
                    self.MM(pK[:, 128:256], kT[:, tsl], qT[:, tsl])
                    Lf = t128("Lf")
                    self.STT("dve", Lf, pK[:, 0:128], beta, D1, ALU.mult, ALU.mult)
                    ATb = t128("ATb", BF16)
                    self.TT("dve", ATb, pK[:, 128:256], DT, ALU.mult)
                    pL = self.ps()
                    self.TR(pL[:, 0:128], Lf, identf)
                    LTf = t128("LTf")
                    self.CP("act", LTf, pL[:, 0:128])
                    Mf = t128("Mf")
                    MTf = t128("MTf")
                    self.CP("pool", Mf, identf)
                    self.CP("pool", MTf, identf)
                    for k in range(7):
                        Ck = t128("Ck")
                        CTk = t128("CTk")
                        self.TT("pool", Ck, Lf, self.lvlm[:, k * 128:(k + 1) * 128], ALU.mult)
                        self.TT("pool", CTk, LTf, self.lvlm[:, 896 + k * 128:896 + (k + 1) * 128], ALU.mult)
                        pa_ = self.ps()
                        self.MM(pa_[:, 0:128], CTk, Mf)
                        self.MM(pa_[:, 128:256], Ck, MTf)
                        T1f = t128("T1f")
                        T3f = t128("T3f")
                        self.CP("act", T1f, pa_[:, 0:128])
                        self.CP("dve", T3f, pa_[:, 128:256])
                        pb_ = self.ps()
                        self.MM(pb_[:, 0:128], MTf, T1f)
                        self.MM(pb_[:, 128:256], Mf, T3f)
                        self.TT("dve", Mf, Mf, pb_[:, 0:128], ALU.subtract)
                        self.TT("dve", MTf, MTf, pb_[:, 128:256], ALU.subtract)
                    pX = self.ps()
                    self.MM(pX[:, 0:256], MTf, Rm)
                    u = t128("u")
                    wb = t128("wb", BF16)
                    self.CP("act", u, pX[:, 0:128])
                    self.CP("dve", wb, pX[:, 128:256])
                    pW = self.ps().bc(BF16)
                    self.TR(pW[:, 0:128], wb, identb)
                    wTn = t128("wTn", BF16)
                    self.ACT(wTn, pW[:, 0:128], AF.Copy, scale=-1.0)
                    pV = self.ps()
                    self.MM(pV[:, 0:128], wTn, Sab)
                    vn = t128("vn", BF16)
                    self.TT("dve", vn, u, pV[:, 0:128], ALU.add)
                    pO = self.ps()
                    self.MM(pO[:, 0:128], qdT, Sab, start=True, stop=False)
                    self.MM(pO[:, 0:128], ATb, vn, start=False, stop=True)
                    oA = t128("oA")
                    self.CP("act", oA, pO[:, 0:128])
                    pS = self.ps()
                    self.MM(pS[:, 0:128], kdec, vn)
                    self.STT("dve", Sa, Sa, egl, pS[:, 0:128], ALU.mult, ALU.add)
                    self.CP("act", Sab, Sa)
                    gA = rab(16, 128)
                    finish(oA, gA, sz, i, q, tsl)
                    rcol, bcc, bl = col(RR), col(CUM, 4), col(CUM, 12)
                    dgr = t128("dg")
                    self.TS("dve", dgr, identf, rcol, ALU.mult)
                    pR = self.ps()
                    self.MM(pR[:, 0:128], onesf, dgr)
                    tmpB = t128("tmpB")
                    self.TT("dve", tmpB, pR[:, 0:128], negm, ALU.add)
                    maxr = c1("maxr")
                    self.RED("dve", maxr, pR[:, 0:128], ALU.max)
                    mr = c1("mr")
                    self.RED("dve", mr, tmpB, ALU.max)
                    nmr = c1("nmr")
                    self.TS("dve", nmr, mr, -1.0, ALU.mult)
                    Eb = t128("Eb")
                    self.ACT(Eb, tmpB, AF.Exp, bias=nmr)
                    pQ = self.ps()
                    self.MM(pQ[:, 0:128], qbT[:, tsl], kbT[:, tsl])
                    P0 = t128("P0", BF16)
                    self.TT("dve", P0, pQ[:, 0:128], Eb, ALU.mult)
                    pP0 = self.ps().bc(BF16)
                    self.TR(pP0[:, 0:128], P0, identb)
                    P0T = t128("P0T", BF16)
                    self.CP("act", P0T, pP0[:, 0:128])
                    ew = c1("ew")
                    self.TT("dve", ew, rcol, maxr, ALU.subtract)
                    self.ACT(ew, ew, AF.Exp)
                    kw0 = t128("kw0", BF16)
                    self.TS("dve", kw0, kbtok, ew, ALU.mult)
                    mi = c1("mi")
                    self.TT("dve", mi, mr, bcc, ALU.add)
                    a_ = c1("a_")
                    self.TT("dve", a_, bcc, mst, ALU.add)
                    mt = c1("mt")
                    self.TT("dve", mt, a_, mi, ALU.max)
                    e3 = c1("e3", 3)
                    self.TT("dve", e3[:, 0:1], a_, mt, ALU.subtract)
                    self.TT("dve", e3[:, 1:2], mi, mt, ALU.subtract)
                    self.TS("dve", e3[:, 2:3], mt, -1.0, ALU.mult)
                    self.ACT(e3, e3, AF.Exp)
                    p1 = self.ps()
                    self.MM(p1[:, 0:129], qbT[:, tsl], Cb)
                    p2 = self.ps()
                    self.MM(p2[:, 0:129], P0T, vext)
                    nd = self.R("nd", 1, lambda i_: S.sbuf("nd", [128, 129], F32, ctx0))
                    self.TS("dve", nd, p1[:, 0:129], e3[:, 0:1], ALU.mult)
                    self.STT("dve", nd, p2[:, 0:129], e3[:, 1:2], nd, ALU.mult, ALU.add)
                    td = c1("td")
                    self.TS("dve", td, nd[:, 128:129], -1.0, ALU.mult)
                    self.TT("dve", td, td, nd[:, 128:129], ALU.max)
                    self.TT("dve", td, td, e3[:, 2:3], ALU.max)
                    self.RCP(td, td)
                    hB = t128("hB")
                    self.TS("dve", hB, nd[:, 0:128], td, ALU.mult)
                    mm = c1("mm")
                    self.TT("dve", mm, mst, maxr, ALU.max)
                    e2 = c1("e2", 2)
                    self.TT("dve", e2[:, 0:1], mst, mm, ALU.subtract)
                    self.TT("dve", e2[:, 1:2], maxr, mm, ALU.subtract)
                    self.ACT(e2, e2, AF.Exp)
                    p3 = self.ps()
                    self.MM(p3[:, 0:129], kw0, vext)
                    self.TS("dve", Cx, Cx, e2[:, 0:1], ALU.mult)
                    self.STT("dve", Cx, p3[:, 0:129], e2[:, 1:2], Cx, ALU.mult, ALU.add)
                    self.CP("act", Cb, Cx)
                    self.TT("dve", mst, bl, mm, ALU.add)
                    gB = rab(144 + i * 128, 128)
                    finish(hB, gB, og, 4 + i, q, tsl)


def make_consts():
    c = np.zeros((128, C_END), np.float32)
    i = np.arange(128)
    P, Fr = i[:, None], i[None, :]
    same = (P // 64) == (Fr // 64)
    c[:, C_ID:C_ID + 128] = np.eye(128)
    c[:, C_MLS:C_MLS + 128] = ((Fr < P) & same)
    c[:, C_MUI:C_MUI + 128] = ((Fr >= P) & same)
    c[:, C_BO:C_BO + 128] = same
    c[:, C_CS0:C_CS0 + 128] = (P < 64)
    c[:, C_CS1:C_CS1 + 128] = (P >= 64)
    c[:, C_NMB:C_NMB + 128] = np.where((Fr <= P) & same, 0.0, NEG)
    s = np.arange(256)[None, :]
    c[:, C_SWA:C_SWA + 256] = np.where((s > P) & (s <= P + 128), 0.0, NEG)
    half = 32
    inv = (10000.0 ** (-np.arange(half, dtype=np.float32) / half)).astype(np.float32)
    c[:, C_INV:C_INV + 32] = inv[None, :]
    c[:, C_ONE:C_ONE + 128] = 1.0
    c[:, C_TU:C_TU + 128] = (Fr >= P)
    c[:, C_ML:C_ML + 128] = (Fr < P)
    c[:, C_NM:C_NM + 128] = np.where(Fr <= P, 0.0, NEG)
    return c


DEBUG = False


def make_lvlmask():
    m = np.zeros((128, 1792), np.float32)
    i = np.arange(128)
    c, e = i[:, None], i[None, :]
    for k in range(7):
        sz = 1 << k
        mk = ((c // (2 * sz)) == (e // (2 * sz))) & ((c % (2 * sz)) >= sz) & ((e % (2 * sz)) < sz)
        m[:, k * 128:(k + 1) * 128] = mk
        m[:, 896 + k * 128:896 + (k + 1) * 128] = mk.T
    return m


def build_nc(NSEQ, LAYERS, mix=("ab", "c")):
    nc = bass.Bass("TRN2", target_bir_lowering=False)

    def di(name, shape, dt=F32):
        return nc.dram_tensor(name, list(shape), dt, kind="ExternalInput").ap()

    dr = {
        "xT": di("xT", [NSEQ, 1024, 2048]),
        "pT": di("pT", [4, NSEQ, 256, 2048]),
        "pos": di("pos", [NSEQ, 128, 16], I32),
        "consts": di("consts", [128, C_END]),
        "lvlmask": di("lvlmask", [128, 1792]),
        "gcols": di("gcols", [128, 128]),
        "convw": di("convw", [128, 96]),
        "rows_ab": di("rows_ab", [128, 2 * RA]),
        "rows_c": di("rows_c", [2, 128, RC]),
        "bo_cols": di("bo_cols", [128, 16]),
        "w_in_ab": di("w_in_ab", [2, 1024, IN_COLS]),
        "w_out_ab": di("w_out_ab", [2, 1024, 1024]),
        "w_qkv_c": di("w_qkv_c", [2, 1024, 1536]),
        "w_o_c": di("w_o_c", [2, 1024, 1024]),
        "w_up": di("w_up", [4, 1024, 4096]),
        "w_down": di("w_down", [4, 4096, 1024]),
        "w_ple": di("w_ple", [4, 256, 1024]),
        "w_ple_gate": di("w_ple_gate", [4, 1024, 1024]),
    }
    dr["outT"] = nc.dram_tensor("outT", [NSEQ, 1024, 2048], F32, kind="ExternalOutput").ap()
    with ExitStack() as ctx:
        kb = KB(nc, ctx, NSEQ, LAYERS, mix)
        kb.debug = DEBUG
        kb.build(dr)
    return nc


def host_params(inp):
    f = lambda a: np.ascontiguousarray(np.asarray(a, dtype=np.float32))
    ng = f(inp["norm_gains"])
    gcols = np.ascontiguousarray(ng.reshape(4, 4, 8, 128).transpose(3, 0, 1, 2).reshape(128, 128))
    cw = f(inp["conv_a"])
    convw = np.ascontiguousarray(cw.reshape(2, 4, 3, 4, 128).transpose(4, 0, 3, 2, 1).reshape(128, 96))
    rows = np.zeros((2, RA), np.float32)
    for e in range(2):
        rows[e, 0:4] = f(inp["dt_bias"])[e]
        rows[e, 4:8] = f(inp["a_log"])[e]
        rows[e, 8:12] = f(inp["i_bias_b"])[e]
        rows[e, 12:16] = f(inp["f_bias_b"])[e]
        rows[e, 16:144] = f(inp["norm_a"])[e]
        rows[e, 144:656] = f(inp["norm_b"])[e].reshape(512)
    rows_ab = np.ascontiguousarray(np.broadcast_to(rows.reshape(1, 2 * RA), (128, 2 * RA)))
    rc = np.zeros((2, RC), np.float32)
    for o in range(2):
        rc[o, 0:1536] = f(inp["b_qkv_c"])[o]
        rc[o, 1536:1552] = f(inp["sinks_c"])[o]
    rows_c = np.ascontiguousarray(np.broadcast_to(rc[:, None, :], (2, 128, RC)))
    bo = f(inp["b_o_c"])
    bo_cols = np.ascontiguousarray(bo.reshape(2, 8, 128).transpose(2, 0, 1).reshape(128, 16))
    return {
        "consts": make_consts(), "lvlmask": make_lvlmask(), "gcols": gcols, "convw": convw, "rows_ab": rows_ab, "rows_c": rows_c,
        "bo_cols": bo_cols,
        "w_in_ab": f(inp["w_in_ab"]), "w_out_ab": f(inp["w_out_ab"]), "w_qkv_c": f(inp["w_qkv_c"]),
        "w_o_c": f(inp["w_o_c"]), "w_up": f(inp["w_up"]), "w_down": f(inp["w_down"]),
        "w_ple": f(inp["w_ple"]), "w_ple_gate": f(inp["w_ple_gate"]),
    }


def run(inp, n_cores=8, LAYERS=DEPTH, mix=("ab", "c"), trace=False):
    x = np.asarray(inp["x"], dtype=np.float32)
    p = np.asarray(inp["p"], dtype=np.float32)
    pos = np.asarray(inp["positions"], dtype=np.int32)
    B = x.shape[0]
    NSEQ = B // n_cores
    shared = host_params(inp)
    nc = build_nc(NSEQ, LAYERS, mix)
    in_maps = []
    for c in range(n_cores):
        sl = slice(c * NSEQ, (c + 1) * NSEQ)
        m = dict(shared)
        m["xT"] = np.ascontiguousarray(x[sl].transpose(0, 2, 1))
        m["pT"] = np.ascontiguousarray(p[:, sl].transpose(0, 1, 3, 2))
        m["pos"] = np.ascontiguousarray(pos[sl].reshape(-1, 16, 128).transpose(0, 2, 1))
        in_maps.append(m)
    res = run_bass_kernel_spmd(nc, in_maps, core_ids=list(range(n_cores)), trace=trace)
    out = np.concatenate([np.asarray(r["outT"]).transpose(0, 2, 1) for r in res.results], axis=0)
    return np.ascontiguousarray(out.astype(np.float32)), res


def kernel(**inputs):
    out, _ = run(inputs)
    return out
```

```python
import numpy as np
import concourse.bass as bass
import concourse.mybir as mybir
from concourse.bass_utils import run_bass_kernel_spmd
from contextlib import ExitStack

F32 = mybir.dt.float32
BF16 = mybir.dt.bfloat16
I32 = mybir.dt.int32
AF = mybir.ActivationFunctionType
ALU = mybir.AluOpType
AX = mybir.AxisListType

ENGS = ("pe", "act", "dve", "pool", "sp")
SEM_EPOCH = 4000
DMA_K = 8

D_MODEL = 1024
SEQ = 2048
DEPTH = 4
D_FF = 4096
PLE = 256
EPS = 1e-6
NEG = -30000.0
A_COLS = 2056
IN_COLS = 4112
C_ID, C_SWA, C_INV, C_ONE, C_TU, C_ML, C_NM, C_END = (0, 128, 384, 416, 544, 672, 800, 928)
RA = 656
RC = 1552


class Trk:
    __slots__ = ("name", "w", "rs", "excl")

    def __init__(self, name, excl=False):
        self.name = name
        self.w = None
        self.rs = []
        self.excl = excl


class V:
    __slots__ = ("ap", "trks")

    def __init__(self, ap, trks):
        self.ap = ap
        self.trks = tuple(trks)

    def __getitem__(self, idx):
        return V(self.ap[idx], self.trks)

    def bc(self, dt):
        return V(self.ap.bitcast(dt), self.trks)


class Ins:
    __slots__ = ("eng", "fn", "deps", "dma", "sig", "ev", "fc", "out")

    def __init__(self, eng, fn, deps, dma, out):
        self.eng = eng
        self.fn = fn
        self.deps = deps
        self.dma = dma
        self.sig = False
        self.ev = None
        self.fc = None
        self.out = out


class Sched:
    def __init__(self, nc, ctx):
        self.nc = nc
        self.ctx = ctx
        self.instrs = []
        self.per_eng = {e: [] for e in ENGS}
        self.uid = 0

    def sbuf(self, name, shape, dtype, ctx=None):
        self.uid += 1
        t = (ctx or self.ctx).enter_context(
            self.nc.sbuf_tensor("%s_%d" % (name, self.uid), list(shape), dtype))
        return V(t[tuple(slice(None) for _ in shape)], [Trk(name)])

    def psum(self, name, shape, dtype=F32):
        t = self.ctx.enter_context(self.nc.psum_tensor(name, list(shape), dtype))
        return V(t[tuple(slice(None) for _ in shape)], [Trk(name, excl=True)])

    def add(self, eng, fn, reads=(), writes=(), dma=False, out=False):
        me = len(self.instrs)
        deps = set()
        for r in reads:
            for t in r.trks:
                if t.excl:
                    if t.w is not None:
                        deps.add(t.w)
                    deps.update(t.rs)
                    t.w = me
                    t.rs = []
                else:
                    if t.w is not None:
                        deps.add(t.w)
                    t.rs.append(me)
        for w in writes:
            for t in w.trks:
                if t.w is not None:
                    deps.add(t.w)
                deps.update(t.rs)
                t.w = me
                t.rs = []
        deps.discard(me)
        self.instrs.append(Ins(eng, fn, deps, dma, out))
        self.per_eng[eng].append(me)
        return me

    def barrier(self):
        last = []
        for e in ENGS:
            lst = self.per_eng[e]
            nd = 0
            seen_c = False
            for idx in reversed(lst):
                ins = self.instrs[idx]
                if ins.fn is None:
                    continue
                if ins.dma:
                    if nd < DMA_K:
                        last.append(idx)
                        nd += 1
                elif not seen_c:
                    last.append(idx)
                    seen_c = True
                if nd >= DMA_K and seen_c:
                    break
        for e in ENGS:
            me = len(self.instrs)
            self.instrs.append(Ins(e, None, set(last), False, False))
            self.per_eng[e].append(me)

    def emit(self):
        nc = self.nc
        instrs = self.instrs
        for ins in instrs:
            for d in ins.deps:
                dd = instrs[d]
                if dd.dma:
                    continue
                if dd.eng == "pe" and ins.eng == "pe" and not ins.dma and ins.fn is not None:
                    continue
                dd.sig = True
        cnt = {e: 0 for e in ENGS}
        dcnt = {e: 0 for e in ENGS}
        for ins in instrs:
            if ins.fn is None:
                continue
            if ins.dma:
                j = dcnt[ins.eng]
                dcnt[ins.eng] += 1
                ins.ev = (("d", ins.eng, j % DMA_K), 16 * (j // DMA_K + 1))
                if j >= DMA_K:
                    ins.fc = (("d", ins.eng, j % DMA_K), 16 * (j // DMA_K))
            elif ins.sig:
                n = cnt[ins.eng]
                cnt[ins.eng] += 1
                ins.ev = (("c", ins.eng, n // SEM_EPOCH), n % SEM_EPOCH + 1)
        sems = {}
        for ins in instrs:
            if ins.ev is not None and ins.ev[0] not in sems:
                k = ins.ev[0]
                sems[k] = self.ctx.enter_context(nc.semaphore("s_%s_%s_%d" % k))
        out_events = [ins.ev for ins in instrs if ins.dma and ins.out]
        self.stats = {e: len(self.per_eng[e]) for e in ENGS}

        def run_engine(ename, eng):
            known = {}
            for idx in self.per_eng[ename]:
                ins = instrs[idx]
                best = {}
                for d in ins.deps:
                    dd = instrs[d]
                    if dd.ev is None:
                        continue
                    if (not dd.dma) and dd.eng == "pe" and ename == "pe" and not ins.dma \
                            and ins.fn is not None:
                        continue
                    k, v = dd.ev
                    if known.get(k, 0) < v and best.get(k, 0) < v:
                        best[k] = v
                if ins.fc is not None:
                    k, v = ins.fc
                    if known.get(k, 0) < v and best.get(k, 0) < v:
                        best[k] = v
                for k, v in best.items():
                    eng.wait_ge(sems[k], v)
                    known[k] = v
                if ins.fn is None:
                    continue
                bi = ins.fn(eng)
                if ins.ev is not None:
                    bi.then_inc(sems[ins.ev[0]], 16 if ins.dma else 1)
            if ename == "sp":
                best = {}
                for (k, v) in out_events:
                    if best.get(k, 0) < v:
                        best[k] = v
                for k, v in best.items():
                    if known.get(k, 0) < v:
                        eng.wait_ge(sems[k], v)

        with nc.Block() as block:
            @block.tensor
            def _(e):
                run_engine("pe", e)

            @block.scalar
            def _(e):
                run_engine("act", e)

            @block.vector
            def _(e):
                run_engine("dve", e)

            @block.gpsimd
            def _(e):
                run_engine("pool", e)

            @block.sync
            def _(e):
                run_engine("sp", e)


class KB:
    def __init__(self, nc, ctx, NSEQ, LAYERS, mix=("ab", "c")):
        self.nc = nc
        self.S = Sched(nc, ctx)
        self.NSEQ = NSEQ
        self.LAYERS = LAYERS
        self.mix = mix
        self.evi = 0
        self.psoi = 0
        self.psrc = {}
        self.psi = 0
        self.rot = {}

    def ev(self):
        self.evi += 1
        return "act" if self.evi % 2 else "dve"

    def ps(self):
        p = self.PS[self.psi % 8]
        self.psi += 1
        return p

    def psr(self, key):
        base = 0 if key == "A" else 4
        n = self.psrc.get(key, 0)
        self.psrc[key] = n + 1
        return self.PS[base + n % 4]

    def ps6(self):
        p = self.PS[self.psi % 6]
        self.psi += 1
        return p

    def ACT(self, out, in_, func, bias=0.0, scale=1.0):
        reads = [in_]
        b, s = bias, scale
        if isinstance(bias, V):
            reads.append(bias)
            b = bias.ap
        if isinstance(scale, V):
            reads.append(scale)
            s = scale.ap
        self.S.add("act", lambda e: e.activation(out=out.ap, in_=in_.ap, func=func, bias=b, scale=s),
                   reads=reads, writes=[out])

    def CP(self, eng, out, in_):
        if eng == "act":
            self.S.add("act", lambda e: e.copy(out=out.ap, in_=in_.ap), reads=[in_], writes=[out])
        else:
            self.S.add(eng, lambda e: e.tensor_copy(out=out.ap, in_=in_.ap), reads=[in_], writes=[out])

    def TT(self, eng, out, a, b, op):
        self.S.add(eng, lambda e: e.tensor_tensor(out=out.ap, in0=a.ap, in1=b.ap, op=op),
                   reads=[a, b], writes=[out])

    def TS(self, eng, out, a, s1, op0, s2=None, op1=None):
        reads = [a]
        x1, x2 = s1, s2
        if isinstance(s1, V):
            reads.append(s1)
            x1 = s1.ap
        if isinstance(s2, V):
            reads.append(s2)
            x2 = s2.ap
        if op1 is None:
            self.S.add(eng, lambda e: e.tensor_scalar(out=out.ap, in0=a.ap, scalar1=x1, scalar2=None, op0=op0),
                       reads=reads, writes=[out])
        else:
            self.S.add(eng, lambda e: e.tensor_scalar(out=out.ap, in0=a.ap, scalar1=x1, scalar2=x2,
                                                      op0=op0, op1=op1), reads=reads, writes=[out])

    def STT(self, eng, out, a, s, b, op0, op1):
        reads = [a, b]
        x = s
        if isinstance(s, V):
            reads.append(s)
            x = s.ap
        self.S.add(eng, lambda e: e.scalar_tensor_tensor(out=out.ap, in0=a.ap, scalar=x, in1=b.ap,
                                                         op0=op0, op1=op1), reads=reads, writes=[out])

    def RED(self, eng, out, in_, op):
        self.S.add(eng, lambda e: e.tensor_reduce(out=out.ap, in_=in_.ap, axis=AX.X, op=op),
                   reads=[in_], writes=[out])

    def RCP(self, out, in_):
        self.S.add("dve", lambda e: e.reciprocal(out=out.ap, in_=in_.ap), reads=[in_], writes=[out])

    def MM(self, ps, lhsT, rhs, start=True, stop=True):
        self.S.add("pe", lambda e: e.matmul(ps.ap, lhsT=lhsT.ap, rhs=rhs.ap, start=start, stop=stop),
                   reads=[lhsT, rhs], writes=[ps])

    def TR(self, ps, in_, ident):
        self.S.add("pe", lambda e: e.transpose(out=ps.ap, in_=in_.ap, identity=ident.ap),
                   reads=[in_, ident], writes=[ps])

    def DMA(self, q, out, in_, is_out=False):
        if is_out:
            self.S.add(q, lambda e: e.dma_start(out=out, in_=in_.ap), reads=[in_], dma=True, out=True)
        else:
            self.S.add(q, lambda e: e.dma_start(out=out.ap, in_=in_), writes=[out], dma=True)

    def dbg(self, name, v, dtype=F32):
        if not getattr(self, "debug", False):
            return
        shp = list(v.ap.shape)
        d = self.nc.dram_tensor(name, shp, dtype, kind="ExternalOutput").ap()
        self.DMA("sp", d, v, is_out=True)

    def lockstep(self, gens):
        gens = list(gens)
        while gens:
            alive = []
            for g in gens:
                try:
                    next(g)
                    alive.append(g)
                except StopIteration:
                    pass
            gens = alive

    def R(self, key, n, mk):
        if key not in self.rot:
            self.rot[key] = [[mk(i) for i in range(n)], 0]
        lst = self.rot[key]
        v = lst[0][lst[1] % n]
        lst[1] += 1
        return v

    def build(self, dr):
        S = self.S
        nc = self.nc
        self.dr = dr
        sb = S.sbuf
        self.PS = [S.psum("ps%d" % i, [128, 512], F32) for i in range(8)]
        self.h = [[sb("h", [128, 512], F32) for q in range(4)] for c in range(8)]
        self.hn = [[sb("hn", [128, 512], BF16) for q in range(4)] for c in range(8)]
        self.cst = sb("cst", [128, C_END], F32)
        self.gcol = sb("gcol", [128, 128], F32)
        self.convw = sb("convw", [128, 96], F32)
        self.rows_ab = sb("rows_ab", [128, 2 * RA], F32)
        self.bo = sb("bo", [128, 16], F32)
        self.identb = sb("identb", [128, 128], BF16)
        self.onesb = sb("onesb", [128, 128], BF16)
        self.cos = sb("cos", [128, 16, 32], F32)
        self.sin = sb("sin", [128, 16, 32], F32)
        self.lvlm = sb("lvlm", [128, 1792], BF16)
        self.DMA("pool", self.lvlm, dr["lvlmask"])
        self.DMA("sp", self.cst, dr["consts"])
        self.DMA("sp", self.gcol, dr["gcols"])
        self.DMA("sp", self.convw, dr["convw"])
        self.DMA("sp", self.rows_ab, dr["rows_ab"])
        self.DMA("sp", self.bo, dr["bo_cols"])
        self.CP("dve", self.identb, self.cst[:, C_ID:C_ID + 128])
        self.CP("dve", self.onesb, self.cst[:, C_ONE:C_ONE + 128])
        self.negpi = sb("negpi", [128, 1], F32)
        S.add("pool", lambda e: e.memset(self.negpi.ap, -float(np.pi)), writes=[self.negpi])
        self.identf = self.cst[:, C_ID:C_ID + 128]
        self.onesf = self.cst[:, C_ONE:C_ONE + 128]

        for b in range(self.NSEQ):
            for c in range(8):
                for q in range(4):
                    self.DMA("sp", self.h[c][q], dr["xT"][b, c * 128:(c + 1) * 128, q * 512:(q + 1) * 512])
            if self.LAYERS > 1 and "c" in self.mix:
                self.rope_tables(b)
            for l in range(self.LAYERS):
                self.layer(b, l)
            for c in range(8):
                for q in range(4):
                    self.DMA("sp", dr["outT"][b, c * 128:(c + 1) * 128, q * 512:(q + 1) * 512],
                             self.h[c][q], is_out=True)
        S.emit()

    def gc(self, l, j, c):
        k = (l * 4 + j) * 8 + c
        return self.gcol[:, k:k + 1]

    def rstd_quarter(self, srcs, ctx):
        S = self.S
        ps = self.ps()
        for c in range(8):
            sq = self.R("sq", 2, lambda i: S.sbuf("sq", [128, 512], BF16, ctx))
            if c % 2 == 0:
                self.ACT(sq, srcs[c], AF.Square)
            else:
                self.TT("dve", sq, srcs[c], srcs[c], ALU.mult)
            self.MM(ps, self.onesb, sq, start=(c == 0), stop=(c == 7))
        rstd = self.R("rstd", 2, lambda i: S.sbuf("rstd", [128, 512], F32, ctx))
        self.ACT(rstd, ps, AF.Ln, bias=EPS, scale=1.0 / D_MODEL)
        self.ACT(rstd, rstd, AF.Exp, scale=-0.5)
        return rstd

    def norm_to_hn(self, l, j, ctx, quarters=range(4)):
        for q in quarters:
            rstd = self.rstd_quarter([self.h[c][q] for c in range(8)], ctx)
            for c in range(8):
                self.STT("dve", self.hn[c][q], self.h[c][q], self.gc(l, j, c), rstd, ALU.mult, ALU.mult)

    def add_normed(self, l, j, q, srcs, ctx):
        rstd = self.rstd_quarter(srcs, ctx)
        for c in range(8):
            if c % 4 == 3:
                self.TT("pool", srcs[c], srcs[c], rstd, ALU.mult)
                self.TS("pool", srcs[c], srcs[c], self.gc(l, j, c), ALU.mult)
                self.TT("pool", self.h[c][q], self.h[c][q], srcs[c], ALU.add)
            else:
                self.TT("dve", srcs[c], srcs[c], rstd, ALU.mult)
                self.STT("dve", self.h[c][q], srcs[c], self.gc(l, j, c), self.h[c][q], ALU.mult, ALU.add)

    def layer(self, b, l):
        S = self.S
        with ExitStack() as ctx:
            self.rot = {}
            self.norm_to_hn(l, 0, ctx)
            self.mixT = [[S.sbuf("mixT", [128, 512], BF16, ctx) for q in range(4)] for c in range(8)]
            kind = "ab" if l % 2 == 0 else "c"
            if kind in self.mix:
                if kind == "ab":
                    with ExitStack() as c2:
                        self.mixer_ab(b, l, c2)
                        S.barrier()
                    self.rot = dict((k, v) for k, v in self.rot.items() if k in ("sq", "rstd", "ntmp"))
                else:
                    self.mixer_c(b, l, ctx)
                self.out_proj(b, l, ctx)
            S.barrier()
        with ExitStack() as ctx:
            self.rot = {}
            self.ffn(b, l, ctx)
            S.barrier()
        with ExitStack() as ctx:
            self.rot = {}
            self.ple(b, l, ctx)
            S.barrier()

    def out_proj(self, b, l, ctx):
        S = self.S
        if l == 1:
            for c in range(8):
                self.dbg("d_mixT%d" % c, self.mixT[c][0], BF16)
        kind = "ab" if l % 2 == 0 else "c"
        wsrc = self.dr["w_out_ab"][l // 2] if kind == "ab" else self.dr["w_o_c"][l // 2]
        wv = wsrc.rearrange("(kc p) n -> p kc n", p=128)
        w = [S.sbuf("wout", [128, 8, 256], BF16, ctx) for i in range(4)]
        for i in range(4):
            self.DMA("pool", w[i], wv[:, :, i * 256:(i + 1) * 256])
        for q in range(4):
            srcs = []
            for m in range(8):
                ps = self.ps()
                for kc in range(8):
                    self.MM(ps, w[m // 2][:, kc, (m % 2) * 128:(m % 2) * 128 + 128], self.mixT[kc][q],
                            start=(kc == 0), stop=(kc == 7))
                t = self.R("mo", 8, lambda i: S.sbuf("mo", [128, 512], F32, ctx))
                if kind == "c":
                    k = (l // 2) * 8 + m
                    self.ACT(t, ps, AF.Identity, bias=self.bo[:, k:k + 1])
                else:
                    self.CP(self.ev(), t, ps)
                srcs.append(t)
            self.add_normed(l, 1, q, srcs, ctx)

    def ffn(self, b, l, ctx):
        S = self.S
        wup = self.dr["w_up"][l].rearrange("(kc p) n -> p kc n", p=128)
        wdn = self.dr["w_down"][l].rearrange("(kc p) n -> p kc n", p=128)
        act = [[S.sbuf("act", [128, 512], BF16, ctx) for n in range(2)] for k in range(16)]
        fft = [[S.sbuf("fft", [128, 512], F32, ctx) for n in range(2)] for m in range(8)]
        wslots = [S.sbuf("wffn", [128, 4096], BF16, ctx) for i in range(2)]
        wi = [0]

        def wslot():
            v = wslots[wi[0] % 2]
            wi[0] += 1
            return v

        for half in range(2):
            qs = [2 * half, 2 * half + 1]
            self.norm_to_hn(l, 2, ctx, quarters=qs)
            for fh in range(2):
                for g in range(4):
                    ws = wslot()
                    wu = V(ws.ap.rearrange("p (kc n) -> p kc n", kc=8), ws.trks)
                    c0 = fh * 2048 + g * 512
                    self.DMA("pool", wu, wup[:, :, c0:c0 + 512])
                    for m in range(4):
                        for n in range(2):
                            ps = self.ps()
                            for kc in range(8):
                                self.MM(ps, wu[:, kc, m * 128:(m + 1) * 128], self.hn[kc][qs[n]],
                                        start=(kc == 0), stop=(kc == 7))
                            r = self.R("relu", 2, lambda i: S.sbuf("relu", [128, 512], F32, ctx))
                            self.ACT(r, ps, AF.Relu)
                            self.TT("dve", act[g * 4 + m][n], r, r, ALU.mult)
                for g in range(4):
                    ws = wslot()
                    wd = V(ws.ap.rearrange("p (kc n) -> p kc n", kc=16), ws.trks)
                    self.DMA("pool", wd, wdn[:, fh * 16:(fh + 1) * 16, g * 256:(g + 1) * 256])
                    pss = [[self.ps() for n in range(2)] for m in range(2)]
                    for kc in range(16):
                        for m in range(2):
                            for n in range(2):
                                self.MM(pss[m][n], wd[:, kc, m * 128:(m + 1) * 128], act[kc][n],
                                        start=(kc == 0), stop=(kc == 15))
                    for m in range(2):
                        for n in range(2):
                            dst = fft[g * 2 + m][n]
                            if fh == 0:
                                self.CP(self.ev(), dst, pss[m][n])
                            else:
                                self.TT("dve", dst, dst, pss[m][n], ALU.add)
            for n in range(2):
                self.add_normed(l, 3, qs[n], [fft[m][n] for m in range(8)], ctx)

    def ple(self, b, l, ctx):
        S = self.S
        wg = S.sbuf("wg", [128, 8, 1024], BF16, ctx)
        wp = S.sbuf("wp", [128, 2, 1024], BF16, ctx)
        pT = [S.sbuf("pT", [128, 2, 512], BF16, ctx) for q in range(4)]
        wgv = self.dr["w_ple_gate"][l].rearrange("(kc p) n -> p kc n", p=128)
        for i in range(2):
            self.DMA("pool", wg[:, :, i * 512:(i + 1) * 512], wgv[:, :, i * 512:(i + 1) * 512])
        self.DMA("pool", wp, self.dr["w_ple"][l].rearrange("(kc p) n -> p kc n", p=128))
        pv = self.dr["pT"][l, b].rearrange("(kc p) t -> p kc t", p=128)
        for q in range(4):
            self.DMA("pool", pT[q], pv[:, :, q * 512:(q + 1) * 512])
        for q in range(4):
            for c in range(8):
                self.CP("act" if c % 2 else "pool", self.hn[c][q], self.h[c][q])
            for m in range(8):
                psg = self.ps()
                for kc in range(8):
                    self.MM(psg, wg[:, kc, m * 128:(m + 1) * 128], self.hn[kc][q], start=(kc == 0), stop=(kc == 7))
                psp = self.ps()
                for kc in range(2):
                    self.MM(psp, wp[:, kc, m * 128:(m + 1) * 128], pT[q][:, kc, :], start=(kc == 0), stop=(kc == 1))
                sg = self.R("sg", 3, lambda i: S.sbuf("sg", [128, 512], F32, ctx))
                self.ACT(sg, psg, AF.Sigmoid)
                self.TT("dve", sg, sg, psp, ALU.mult)
                self.TT("pool", self.h[m][q], self.h[m][q], sg, ALU.add)

    def rope_tables(self, b):
        S = self.S
        with ExitStack() as ctx:
            posi = S.sbuf("posi", [128, 16], I32, ctx)
            posf = S.sbuf("posf", [128, 16], F32, ctx)
            y0 = S.sbuf("y0", [128, 16, 32], F32, ctx)
            yy = S.sbuf("yy", [128, 16, 32], F32, ctx)
            ki = S.sbuf("ki", [128, 16, 32], I32, ctx)
            kf = S.sbuf("kf", [128, 16, 32], F32, ctx)
            mk = S.sbuf("mk", [128, 16, 32], F32, ctx)
            self.DMA("sp", posi, self.dr["pos"][b])
            self.CP("dve", posf, posi)
            inv = self.cst[:, C_INV:C_INV + 32]
            invb = V(inv.ap.unsqueeze(1).to_broadcast([128, 16, 32]), inv.trks)
            posb = V(posf.ap.unsqueeze(2).to_broadcast([128, 16, 32]), posf.trks)
            self.TT("dve", y0, invb, posb, ALU.mult)
            for dst, off in ((self.sin, 0.5), (self.cos, 0.75)):
                self.TS("dve", yy, y0, 1.0 / (2.0 * np.pi), ALU.mult, off, ALU.add)
                self.CP("dve", ki, yy)
                self.CP("dve", kf, ki)
                self.TT("dve", yy, yy, kf, ALU.subtract)
                self.TS("dve", mk, yy, 0.0, ALU.is_lt)
                self.TT("dve", yy, yy, mk, ALU.add)
                self.ACT(dst, yy, AF.Sin, bias=self.negpi, scale=2.0 * np.pi)
            S.barrier()

    def mixer_c(self, b, l, ctx0):
        S = self.S
        o = l // 2
        rows = S.sbuf("rowsc", [128, RC], F32, ctx0)
        self.DMA("sp", rows, self.dr["rows_c"][o])
        wv = self.dr["w_qkv_c"][o].rearrange("(kc p) n -> p kc n", p=128)
        for g in range(4):
          with ExitStack() as ctx:
            self.rot = dict((k, v) for k, v in self.rot.items() if k in ("sq", "rstd", "ntmp", "mo"))
            wq = S.sbuf("wq", [128, 8, 384], BF16, ctx)
            self.DMA("pool", wq[:, :, 0:256], wv[:, :, g * 256:(g + 1) * 256])
            self.DMA("pool", wq[:, :, 256:320], wv[:, :, 1024 + g * 64:1024 + (g + 1) * 64])
            self.DMA("pool", wq[:, :, 320:384], wv[:, :, 1280 + g * 64:1280 + (g + 1) * 64])
            brow = S.sbuf("brow", [128, 384], F32, ctx)
            self.CP("pool", brow[:, 0:256], rows[:, g * 256:(g + 1) * 256])
            self.CP("pool", brow[:, 256:320], rows[:, 1024 + g * 64:1024 + (g + 1) * 64])
            self.CP("pool", brow[:, 320:384], rows[:, 1280 + g * 64:1280 + (g + 1) * 64])
            qTall = S.sbuf("qTall", [128, 2, 2048], BF16, ctx)
            kTall = S.sbuf("kTall", [128, 2048], BF16, ctx)
            qTj = [V(qTall.ap[:, :, j * 128:(j + 1) * 128], [Trk("qTj")]) for j in range(16)]
            kTj = [V(kTall.ap[:, j * 128:(j + 1) * 128], [Trk("kTj")]) for j in range(16)]
            vtok = [S.sbuf("vtok", [128, 64], BF16, ctx) for j in range(16)]
            for j in range(16):
                qkv = self.R("qkv", 2, lambda i: S.sbuf("qkv", [128, 384], F32, ctx))
                ps = self.ps6()
                for kc in range(8):
                    self.MM(ps[:, 0:384], self.hn[kc][j // 4][:, (j % 4) * 128:(j % 4) * 128 + 128],
                            wq[:, kc, :], start=(kc == 0), stop=(kc == 7))
                self.TT("dve", qkv, ps[:, 0:384], brow, ALU.add)
                qr = self.R("qr", 2, lambda i: S.sbuf("qr", [128, 256], BF16, ctx))
                kd = self.R("kd", 2, lambda i: S.sbuf("kd", [128, 2, 64], BF16, ctx))
                for (src, dst, nh) in ((qkv[:, 0:256], qr, 4), (qkv[:, 256:320], kd[:, 0, :], 1)):
                    sv = V(src.ap.rearrange("p (h t d) -> p h t d", t=2, d=32), src.trks)
                    dv = V(dst.ap.rearrange("p (h t d) -> p h t d", t=2, d=32), dst.trks)
                    cb = V(self.cos.ap[:, j, :].unsqueeze(1).to_broadcast([128, nh, 32]), self.cos.trks)
                    sb_ = V(self.sin.ap[:, j, :].unsqueeze(1).to_broadcast([128, nh, 32]), self.sin.trks)
                    t1 = self.R("rt1", 2, lambda i: S.sbuf("rt1", [128, 4, 32], F32, ctx))
                    t2 = self.R("rt2", 2, lambda i: S.sbuf("rt2", [128, 4, 32], F32, ctx))
                    t3 = self.R("rt3", 2, lambda i: S.sbuf("rt3", [128, 4, 32], F32, ctx))
                    t4 = self.R("rt4", 2, lambda i: S.sbuf("rt4", [128, 4, 32], F32, ctx))
                    self.TT("dve", t1[:, 0:nh, :], sv[:, :, 0, :], cb, ALU.mult)
                    self.TT("dve", t2[:, 0:nh, :], sv[:, :, 1, :], sb_, ALU.mult)
                    self.TT("dve", dv[:, :, 0, :], t1[:, 0:nh, :], t2[:, 0:nh, :], ALU.subtract)
                    self.TT("pool", t3[:, 0:nh, :], sv[:, :, 1, :], cb, ALU.mult)
                    self.TT("pool", t4[:, 0:nh, :], sv[:, :, 0, :], sb_, ALU.mult)
                    self.TT("pool", dv[:, :, 1, :], t3[:, 0:nh, :], t4[:, 0:nh, :], ALU.add)
                self.CP("act", kd[:, 1, :], kd[:, 0, :])
                self.CP("act", vtok[j], qkv[:, 320:384])
                pst = self.ps6().bc(BF16)
                for i in range(2):
                    self.TR(pst[:, i * 128:(i + 1) * 128], qr[:, i * 128:(i + 1) * 128], self.identb)
                kdf = V(kd.ap.rearrange("p a d -> p (a d)"), kd.trks)
                self.TR(pst[:, 256:384], kdf, self.identb)
                src = V(pst.ap[:, 0:256].rearrange("p (i t) -> p i t", t=128), pst.trks)
                self.CP("act", qTj[j], src)
                self.CP("dve", kTj[j], pst[:, 256:384])
            for j in range(16):
                nk = 1 if j == 0 else 2
                SS = nk * 128
                mask = self.cst[:, C_SWA + 256 - SS:C_SWA + 256]
                pso = self.PS[6 + (self.psoi % 2)]
                self.psoi += 1
                recg = self.R("recg", 2, lambda i: S.sbuf("recg", [128, 4], F32, ctx))
                ktr = [kTj[j - 1], kTj[j]] if j > 0 else [kTj[0]]
                sc4 = self.R("sc4", 2, lambda i_: S.sbuf("sc4", [128, 4, 256], F32, ctx))
                for hl in range(4):
                    i = hl // 2
                    hh = hl % 2
                    ps = self.ps6()
                    for kb in range(nk):
                        self.MM(ps[:, kb * 128:(kb + 1) * 128],
                                qTj[j][hh * 64:(hh + 1) * 64, i, :], ktr[kb][hh * 64:(hh + 1) * 64, :])
                    self.STT("dve", sc4[:, hl, 0:SS], ps[:, 0:SS], 0.125, mask, ALU.mult, ALU.add)
                sk4 = rows[:, 1536 + 4 * g:1536 + 4 * g + 4]
                mx4 = self.R("mx4", 2, lambda i_: S.sbuf("mx4", [128, 4], F32, ctx))
                self.RED("dve", mx4, sc4[:, :, 0:SS], ALU.max)
                self.TT("dve", mx4, mx4, sk4, ALU.max)
                nmx4 = self.R("nmx4", 2, lambda i_: S.sbuf("nmx4", [128, 4], F32, ctx))
                self.TS("dve", nmx4, mx4, -1.0, ALU.mult)
                pr4 = self.R("pr4", 2, lambda i_: S.sbuf("pr4", [128, 4, 256], BF16, ctx))
                for hl in range(4):
                    self.ACT(pr4[:, hl, 0:SS], sc4[:, hl, 0:SS], AF.Exp, bias=nmx4[:, hl:hl + 1])
                rs4 = self.R("rs4", 2, lambda i_: S.sbuf("rs4", [128, 4], F32, ctx))
                self.RED("dve", rs4, pr4[:, :, 0:SS], ALU.add)
                es4 = self.R("es4", 2, lambda i_: S.sbuf("es4", [128, 4], F32, ctx))
                self.TT("dve", es4, sk4, mx4, ALU.subtract)
                self.ACT(es4, es4, AF.Exp)
                self.TT("dve", rs4, rs4, es4, ALU.add)
                self.RCP(recg, rs4)
                for hl in range(4):
                    pst = self.ps6().bc(BF16)
                    for kb in range(nk):
                        self.TR(pst[:, kb * 128:(kb + 1) * 128], pr4[:, hl, kb * 128:(kb + 1) * 128], self.identb)
                    prT = self.R("prT", 4, lambda i_: S.sbuf("prT", [128, 256], BF16, ctx))
                    self.CP(self.ev(), prT[:, 0:SS], pst[:, 0:SS])
                    for kb in range(nk):
                        jk = j - 1 + kb if j > 0 else 0
                        self.MM(pso[:, hl * 64:hl * 64 + 64], prT[:, kb * 128:(kb + 1) * 128],
                                vtok[jk], start=(kb == 0), stop=(kb == nk - 1))
                otok = self.R("otok", 2, lambda i: S.sbuf("otok", [128, 256], BF16, ctx))
                psov = V(pso.ap[:, 0:256].rearrange("p (h d) -> p h d", d=64), pso.trks)
                recb = V(recg.ap.unsqueeze(2).to_broadcast([128, 4, 64]), recg.trks)
                ov = V(otok.ap.rearrange("p (h d) -> p h d", d=64), otok.trks)
                self.TT("dve", ov, psov, recb, ALU.mult)
                pst = self.ps6().bc(BF16)
                for i in range(2):
                    self.TR(pst[:, i * 128:(i + 1) * 128], otok[:, i * 128:(i + 1) * 128], self.identb)
                for i in range(2):
                    self.CP(self.ev(), self.mixT[2 * g + i][j // 4][:, (j % 4) * 128:(j % 4) * 128 + 128],
                            pst[:, i * 128:(i + 1) * 128])
            S.barrier()

    def mixer_ab(self, b, l, ctx0):
        S = self.S
        e = l // 2
        rab = lambda a, n: self.rows_ab[:, e * RA + a:e * RA + a + n]
        win = self.dr["w_in_ab"][e].rearrange("(kc p) n -> p kc n", p=128)
        sb = lambda name, shape, dt=F32: S.sbuf(name, shape, dt, ctx0)
        identf, onesf, identb = self.identf, self.onesf, self.identb
        triU = self.cst[:, C_TU:C_TU + 128]
        maskL = self.cst[:, C_ML:C_ML + 128]
        negm = self.cst[:, C_NM:C_NM + 128]
        wgate = sb("wgate", [128, 8, 16], BF16)
        self.DMA("pool", wgate[:, :, 0:8], win[:, :, 1536:1544])
        self.DMA("pool", wgate[:, :, 8:16], win[:, :, 3592:3600])
        G = sb("G", [128, 16, 16])
        for j in range(16):
            ps = self.ps()
            for kc in range(8):
                self.MM(ps[:, 0:16], self.hn[kc][j // 4][:, (j % 4) * 128:(j % 4) * 128 + 128], wgate[:, kc, :],
                        start=(kc == 0), stop=(kc == 7))
            self.CP(self.ev(), G[:, j, :], ps[:, 0:16])
        bc4 = lambda v: V(v.ap.unsqueeze(1).to_broadcast([128, 16, 4]), v.trks)
        BETA = sb("BETA", [128, 16, 4])
        self.ACT(BETA, G[:, :, 0:4], AF.Sigmoid)
        X = sb("X", [128, 16, 4])
        T1 = sb("T1", [128, 16, 4])
        T2 = sb("T2", [128, 16, 4])
        SP = sb("SP", [128, 16, 4])

        def softplus(dst, x):
            self.TS("dve", T1, x, -1.0, ALU.mult)
            self.TT("dve", T1, T1, x, ALU.min)
            self.ACT(T1, T1, AF.Exp)
            self.ACT(T1, T1, AF.Ln, bias=1.0)
            self.TS("dve", T2, x, 0.0, ALU.max)
            self.TT("dve", dst, T1, T2, ALU.add)

        GF = sb("GF", [128, 16, 8])
        self.TT("dve", X, G[:, :, 4:8], bc4(rab(0, 4)), ALU.add)
        softplus(SP, X)
        negA = sb("negA", [128, 4])
        self.ACT(negA, rab(4, 4), AF.Exp)
        self.TS("dve", negA, negA, -1.0, ALU.mult)
        self.TT("dve", GF[:, :, 0:4], SP, bc4(negA), ALU.mult)
        ILOG = sb("ILOG", [128, 16, 4])
        self.TT("dve", ILOG, G[:, :, 8:12], bc4(rab(8, 4)), ALU.add)
        self.TT("dve", X, G[:, :, 12:16], bc4(rab(12, 4)), ALU.add)
        self.TS("dve", X, X, -1.0, ALU.mult)
        softplus(SP, X)
        self.TS("dve", GF[:, :, 4:8], SP, -1.0, ALU.mult)
        CUM = sb("CUM", [128, 16, 16])
        for j in range(16):
            ps = self.ps()
            self.MM(ps[:, 0:8], triU, GF[:, j, :])
            self.MM(ps[:, 8:16], onesf, GF[:, j, :])
            self.CP(self.ev(), CUM[:, j, :], ps[:, 0:16])
        gcum, bcum, gtot = CUM[:, :, 0:4], CUM[:, :, 4:8], CUM[:, :, 8:12]
        BG = sb("BG", [128, 16, 4])
        KD = sb("KD", [128, 16, 4])
        EGL = sb("EGL", [128, 16, 4])
        RR = sb("RR", [128, 16, 4])
        self.ACT(BG, gcum, AF.Exp)
        self.TT("dve", BG, BG, BETA, ALU.mult)
        self.TT("dve", KD, gtot, gcum, ALU.subtract)
        self.ACT(KD, KD, AF.Exp)
        self.ACT(EGL, gtot, AF.Exp)
        self.TT("dve", RR, ILOG, bcum, ALU.subtract)
        cols = [i * 128 for i in range(4)]
        vexts = [sb("vext", [128, 129], BF16) for _ in range(2)]
        for v in vexts:
            S.add("pool", lambda e_, v=v: e_.memset(v.ap[:, 128:129], 1.0), writes=[v])
        wh = sb("wh", [128, 8, 1024], BF16)
        Sa = sb("Sa", [128, 128])
        Sab = sb("Sab", [128, 128], BF16)
        Cx = sb("Cx", [128, 129])
        Cb = sb("Cb", [128, 129], BF16)
        mst = sb("mst", [128, 1])
        cwork = sb("cwork", [128, 515])
        ctail = [sb("ctail", [128, 4]) for _ in range(3)]
        qT = sb("qT", [128, 512], BF16)
        kT = sb("kT", [128, 512], BF16)
        vT = sb("vT", [128, 512], BF16)
        qbT = sb("qbT", [128, 512], BF16)
        kbT = sb("kbT", [128, 512], BF16)
        acc = sb("acc", [128, 512])
        sl = acc
        t128 = lambda name, dt=F32: self.R(name, 1, lambda i_: S.sbuf(name, [128, 128], dt, ctx0))
        c1 = lambda name, n=1: self.R(name, 2, lambda i_: S.sbuf(name, [128, n], F32, ctx0))

        def finish(key, pf, o, gain, gate, ci, q, tsl):
            tmp = t128(pf + "f_tmp")
            self.ACT(tmp, o, AF.Square)
            yield
            ss = c1(pf + "f_ss")
            self.RED("dve", ss, tmp, ALU.add)
            yield
            self.ACT(ss, ss, AF.Ln, bias=EPS, scale=1.0 / 128.0)
            self.ACT(ss, ss, AF.Exp, scale=-0.5)
            yield
            y = t128(pf + "f_y")
            self.STT("dve", y, o, ss, gain, ALU.mult, ALU.mult)
            yb = t128(pf + "f_yb", BF16)
            self.TT("dve", yb, y, gate, ALU.mult)
            yield
            pY = self.psr(key).bc(BF16)
            self.TR(pY[:, 0:128], yb, identb)
            self.CP(self.ev(), self.mixT[ci][q][:, tsl], pY[:, 0:128])
            yield

        for i in range(4):
            gcols = [i * 128, 512 + i * 128, 1024 + i * 128, 2056 + i * 128, 2568 + i * 128,
                     3080 + i * 128, 3600 + i * 128, 1544 + i * 128]
            for gi, c0 in enumerate(gcols):
                self.DMA("pool", wh[:, :, gi * 128:(gi + 1) * 128], win[:, :, c0:c0 + 128])
            for t_ in (Sa, Sab, Cx, Cb, mst):
                S.add("pool", lambda e_, t_=t_: e_.memset(t_.ap, 0.0), writes=[t_])
            for g in range(3):
                S.add("pool", lambda e_, t_=ctail[g]: e_.memset(t_.ap, 0.0), writes=[ctail[g]])
            for q in range(4):
                for gi in range(5):
                    ps = self.ps()
                    for kc in range(8):
                        self.MM(ps, wh[:, kc, gi * 128:(gi + 1) * 128], self.hn[kc][q], start=(kc == 0), stop=(kc == 7))
                    if gi < 3:
                        cb_ = cwork
                        self.CP("pool", cb_[:, 0:3], ctail[gi][:, 0:3])
                        self.CP(self.ev(), cb_[:, 3:515], ps)
                        wc = lambda k: self.convw[:, ((e * 4 + i) * 3 + gi) * 4 + k:((e * 4 + i) * 3 + gi) * 4 + k + 1]
                        self.TS("dve", acc, cb_[:, 0:512], wc(0), ALU.mult)
                        for k in range(1, 4):
                            self.STT("dve", acc, cb_[:, k:k + 512], wc(k), acc, ALU.mult, ALU.add)
                        self.CP("pool", ctail[gi][:, 0:3], cb_[:, 512:515])
                        self.ACT(sl, acc, AF.Silu)
                        if gi < 2:
                            sqb = self.R("sq", 2, lambda i_: S.sbuf("sq", [128, 512], BF16, ctx0))
                            self.ACT(sqb, sl, AF.Square)
                            p2 = self.ps()
                            self.MM(p2, self.onesb, sqb)
                            rn5 = p2
                            self.ACT(rn5, p2, AF.Ln, bias=EPS)
                            self.ACT(rn5, rn5, AF.Exp, scale=-0.5)
                            if gi == 0:
                                self.STT("dve", qT, sl, 128.0 ** -0.5, rn5, ALU.mult, ALU.mult)
                            else:
                                self.TT("dve", kT, sl, rn5, ALU.mult)
                        else:
                            self.CP("pool", vT, sl)
                    elif gi == 3:
                        self.ACT(qbT, ps, AF.Copy, scale=128.0 ** -0.5)
                    else:
                        self.CP("dve", kbT, ps)
                for jj in range(4):
                    j = 4 * q + jj
                    tsl = slice(jj * 128, (jj + 1) * 128)
                    col = lambda T_, off=0: T_[:, j, off + i:off + i + 1]
                    pt = self.ps()
                    for kc in range(8):
                        self.MM(pt, self.hn[kc][q][:, tsl], wh[:, kc, 512:1024], start=(kc == 0), stop=(kc == 7))
                    kbtok = t128("kbtok")
                    self.CP("act", kbtok, pt[:, 0:128])
                    vext = vexts[j % 2]
                    self.CP("dve", vext[:, 0:128], pt[:, 128:256])
                    og = t128("og")
                    self.ACT(og, pt[:, 256:384], AF.Sigmoid)
                    sz = t128("sz")
                    self.ACT(sz, pt[:, 384:512], AF.Silu)
                    def chainA():
                        ptr = self.psr("A").bc(BF16)
                        self.TR(ptr[:, 0:128], kT[:, tsl], identb)
                        self.TR(ptr[:, 128:256], vT[:, tsl], identb)
                        beta, gc_, bg, kdc, egl = col(BETA), col(CUM), col(BG), col(KD), col(EGL)
                        Rm = self.R("Rm", 1, lambda i_: S.sbuf("Rm", [128, 256], F32, ctx0))
                        self.TS("dve", Rm[:, 0:128], ptr[:, 128:256], beta, ALU.mult)
                        yield
                        self.TS("dve", Rm[:, 128:256], ptr[:, 0:128], bg, ALU.mult)
                        yield
                        kdec = t128("kdec", BF16)
                        self.TS("dve", kdec, ptr[:, 0:128], kdc, ALU.mult)
                        yield
                        dg = t128("dg")
                        self.TS("dve", dg, identf, gc_, ALU.mult)
                        yield
                        pG = self.psr("A")
                        self.MM(pG[:, 0:128], onesf, dg)
                        D1 = t128("D1")
                        self.STT("dve", D1, pG[:, 0:128], gc_, maskL, ALU.subtract, ALU.mult)
                        yield
                        DT = t128("DT")
                        self.STT("dve", DT, pG[:, 0:128], gc_, triU, ALU.subtract, ALU.mult)
                        yield
                        EgR = pG[:, 0:128]
                        self.ACT(EgR, pG[:, 0:128], AF.Exp)
                        yield
                        self.ACT(D1, D1, AF.Exp, scale=-1.0)
                        yield
                        self.TT("dve", D1, D1, maskL, ALU.mult)
                        yield
                        self.ACT(DT, DT, AF.Exp)
                        yield
                        self.TT("dve", DT, DT, triU, ALU.mult)
                        yield
                        qdT = t128("qdT", BF16)
                        self.TT("dve", qdT, qT[:, tsl], EgR, ALU.mult)
                        yield
                        pK = self.psr("A")
                        self.MM(pK[:, 0:128], kT[:, tsl], kT[:, tsl])
                        self.MM(pK[:, 128:256], kT[:, tsl], qT[:, tsl])
                        Lf = t128("Lf")
                        self.STT("dve", Lf, pK[:, 0:128], beta, D1, ALU.mult, ALU.mult)
                        yield
                        ATb = t128("ATb", BF16)
                        self.TT("dve", ATb, pK[:, 128:256], DT, ALU.mult)
                        yield
                        pL = self.psr("A")
                        self.TR(pL[:, 0:128], Lf, identf)
                        LTf = t128("LTf")
                        self.CP("act", LTf, pL[:, 0:128])
                        yield
                        Mf = t128("Mf")
                        MTf = t128("MTf")
                        t128b = lambda name: self.R(name, 2, lambda i_: S.sbuf(name, [128, 128], F32, ctx0))
                        Ck = t128b("Ck")
                        CTk = t128b("CTk")
                        self.TT("pool", Ck, Lf, self.lvlm[:, 0:128], ALU.mult)
                        self.TT("pool", CTk, LTf, self.lvlm[:, 896:1024], ALU.mult)
                        self.TT("pool", Mf, identf, Ck, ALU.subtract)
                        self.TT("pool", MTf, identf, CTk, ALU.subtract)
                        yield
                        for k in range(1, 7):
                            Ck = t128b("Ck")
                            CTk = t128b("CTk")
                            self.TT("pool", Ck, Lf, self.lvlm[:, k * 128:(k + 1) * 128], ALU.mult)
                            self.TT("pool", CTk, LTf, self.lvlm[:, 896 + k * 128:896 + (k + 1) * 128], ALU.mult)
                            pa_ = self.psr("A")
                            self.MM(pa_[:, 0:128], CTk, Mf)
                            self.MM(pa_[:, 128:256], Ck, MTf)
                            T1f = t128("T1f")
                            T3f = t128("T3f")
                            self.CP("act", T1f, pa_[:, 0:128])
                            self.CP("dve", T3f, pa_[:, 128:256])
                            yield
                            pb_ = self.psr("A")
                            self.MM(pb_[:, 0:128], MTf, T1f)
                            self.MM(pb_[:, 128:256], Mf, T3f)
                            self.TT("dve", Mf, Mf, pb_[:, 0:128], ALU.subtract)
                            self.TT("dve", MTf, MTf, pb_[:, 128:256], ALU.subtract)
                            yield
                        pX = self.psr("A")
                        self.MM(pX[:, 0:256], MTf, Rm)
                        u = t128("u")
                        wb = t128("wb", BF16)
                        self.CP("act", u, pX[:, 0:128])
                        yield
                        self.CP("dve", wb, pX[:, 128:256])
                        yield
                        pW = self.psr("A").bc(BF16)
                        self.TR(pW[:, 0:128], wb, identb)
                        wTn = t128("wTn", BF16)
                        self.ACT(wTn, pW[:, 0:128], AF.Copy, scale=-1.0)
                        yield
                        pV = self.psr("A")
                        self.MM(pV[:, 0:128], wTn, Sab)
                        vn = t128("vn", BF16)
                        self.TT("dve", vn, u, pV[:, 0:128], ALU.add)
                        yield
                        pO = self.psr("A")
                        self.MM(pO[:, 0:128], qdT, Sab, start=True, stop=False)
                        self.MM(pO[:, 0:128], ATb, vn, start=False, stop=True)
                        oA = t128("oA")
                        self.CP("act", oA, pO[:, 0:128])
                        yield
                        pS = self.psr("A")
                        self.MM(pS[:, 0:128], kdec, vn)
                        self.STT("dve", Sa, Sa, egl, pS[:, 0:128], ALU.mult, ALU.add)
                        yield
                        self.CP("act", Sab, Sa)
                        yield
                        gA = rab(16, 128)
                        yield from finish("A", "a", oA, gA, sz, i, q, tsl)
                    def chainB():
                        rcol, bcc, bl = col(RR), col(CUM, 4), col(CUM, 12)
                        dgr = t128("dgr")
                        self.TS("dve", dgr, identf, rcol, ALU.mult)
                        yield
                        pR = self.psr("B")
                        self.MM(pR[:, 0:128], onesf, dgr)
                        tmpB = t128("tmpB")
                        self.TT("dve", tmpB, pR[:, 0:128], negm, ALU.add)
                        yield
                        maxr = c1("maxr")
                        self.RED("dve", maxr, pR[:, 0:128], ALU.max)
                        yield
                        mr = c1("mr")
                        self.RED("dve", mr, tmpB, ALU.max)
                        yield
                        nmr = c1("nmr")
                        self.TS("dve", nmr, mr, -1.0, ALU.mult)
                        yield
                        Eb = t128("Eb")
                        self.ACT(Eb, tmpB, AF.Exp, bias=nmr)
                        yield
                        pQ = self.psr("B")
                        self.MM(pQ[:, 0:128], qbT[:, tsl], kbT[:, tsl])
                        P0 = t128("P0", BF16)
                        self.TT("dve", P0, pQ[:, 0:128], Eb, ALU.mult)
                        yield
                        pP0 = self.psr("B").bc(BF16)
                        self.TR(pP0[:, 0:128], P0, identb)
                        P0T = t128("P0T", BF16)
                        self.CP("act", P0T, pP0[:, 0:128])
                        yield
                        ew = c1("ew")
                        self.TT("dve", ew, rcol, maxr, ALU.subtract)
                        yield
                        self.ACT(ew, ew, AF.Exp)
                        yield
                        kw0 = t128("kw0", BF16)
                        self.TS("dve", kw0, kbtok, ew, ALU.mult)
                        yield
                        mi = c1("mi")
                        self.TT("dve", mi, mr, bcc, ALU.add)
                        yield
                        a_ = c1("a_")
                        self.TT("dve", a_, bcc, mst, ALU.add)
                        yield
                        mt = c1("mt")
                        self.TT("dve", mt, a_, mi, ALU.max)
                        yield
                        e3 = c1("e3", 3)
                        self.TT("dve", e3[:, 0:1], a_, mt, ALU.subtract)
                        yield
                        self.TT("dve", e3[:, 1:2], mi, mt, ALU.subtract)
                        yield
                        self.TS("dve", e3[:, 2:3], mt, -1.0, ALU.mult)
                        yield
                        self.ACT(e3, e3, AF.Exp)
                        yield
                        p1 = self.psr("B")
                        self.MM(p1[:, 0:129], qbT[:, tsl], Cb)
                        p2 = self.psr("B")
                        self.MM(p2[:, 0:129], P0T, vext)
                        nd = self.R("nd", 1, lambda i_: S.sbuf("nd", [128, 129], F32, ctx0))
                        self.TS("dve", nd, p1[:, 0:129], e3[:, 0:1], ALU.mult)
                        yield
                        self.STT("dve", nd, p2[:, 0:129], e3[:, 1:2], nd, ALU.mult, ALU.add)
                        yield
                        td = c1("td")
                        self.TS("dve", td, nd[:, 128:129], -1.0, ALU.mult)
                        yield
                        self.TT("dve", td, td, nd[:, 128:129], ALU.max)
                        yield
                        self.TT("dve", td, td, e3[:, 2:3], ALU.max)
                        yield
                        self.RCP(td, td)
                        yield
                        hB = t128("hB")
                        self.TS("dve", hB, nd[:, 0:128], td, ALU.mult)
                        yield
                        mm = c1("mm")
                        self.TT("dve", mm, mst, maxr, ALU.max)
                        yield
                        e2 = c1("e2", 2)
                        self.TT("dve", e2[:, 0:1], mst, mm, ALU.subtract)
                        yield
                        self.TT("dve", e2[:, 1:2], maxr, mm, ALU.subtract)
                        yield
                        self.ACT(e2, e2, AF.Exp)
                        yield
                        p3 = self.psr("B")
                        self.MM(p3[:, 0:129], kw0, vext)
                        self.TS("dve", Cx, Cx, e2[:, 0:1], ALU.mult)
                        yield
                        self.STT("dve", Cx, p3[:, 0:129], e2[:, 1:2], Cx, ALU.mult, ALU.add)
                        yield
                        self.CP("act", Cb, Cx)
                        yield
                        self.TT("dve", mst, bl, mm, ALU.add)
                        yield
                        gB = rab(144 + i * 128, 128)
                        yield from finish("B", "b", hB, gB, og, 4 + i, q, tsl)
                    self.lockstep([chainA(), chainB()])


def make_consts():
    c = np.zeros((128, C_END), np.float32)
    i = np.arange(128)
    P, Fr = i[:, None], i[None, :]
    same = (P // 64) == (Fr // 64)
    c[:, C_ID:C_ID + 128] = np.eye(128)
    s = np.arange(256)[None, :]
    c[:, C_SWA:C_SWA + 256] = np.where((s > P) & (s <= P + 128), 0.0, NEG)
    half = 32
    inv = (10000.0 ** (-np.arange(half, dtype=np.float32) / half)).astype(np.float32)
    c[:, C_INV:C_INV + 32] = inv[None, :]
    c[:, C_ONE:C_ONE + 128] = 1.0
    c[:, C_TU:C_TU + 128] = (Fr >= P)
    c[:, C_ML:C_ML + 128] = (Fr < P)
    c[:, C_NM:C_NM + 128] = np.where(Fr <= P, 0.0, NEG)
    return c


DEBUG = False


def make_lvlmask():
    m = np.zeros((128, 1792), np.float32)
    i = np.arange(128)
    c, e = i[:, None], i[None, :]
    for k in range(7):
        sz = 1 << k
        mk = ((c // (2 * sz)) == (e // (2 * sz))) & ((c % (2 * sz)) >= sz) & ((e % (2 * sz)) < sz)
        m[:, k * 128:(k + 1) * 128] = mk
        m[:, 896 + k * 128:896 + (k + 1) * 128] = mk.T
    return m


def build_nc(NSEQ, LAYERS, mix=("ab", "c")):
    nc = bass.Bass("TRN2", target_bir_lowering=False)

    def di(name, shape, dt=F32):
        return nc.dram_tensor(name, list(shape), dt, kind="ExternalInput").ap()

    dr = {
        "xT": di("xT", [NSEQ, 1024, 2048]),
        "pT": di("pT", [4, NSEQ, 256, 2048]),
        "pos": di("pos", [NSEQ, 128, 16], I32),
        "consts": di("consts", [128, C_END]),
        "lvlmask": di("lvlmask", [128, 1792]),
        "gcols": di("gcols", [128, 128]),
        "convw": di("convw", [128, 96]),
        "rows_ab": di("rows_ab", [128, 2 * RA]),
        "rows_c": di("rows_c", [2, 128, RC]),
        "bo_cols": di("bo_cols", [128, 16]),
        "w_in_ab": di("w_in_ab", [2, 1024, IN_COLS]),
        "w_out_ab": di("w_out_ab", [2, 1024, 1024]),
        "w_qkv_c": di("w_qkv_c", [2, 1024, 1536]),
        "w_o_c": di("w_o_c", [2, 1024, 1024]),
        "w_up": di("w_up", [4, 1024, 4096]),
        "w_down": di("w_down", [4, 4096, 1024]),
        "w_ple": di("w_ple", [4, 256, 1024]),
        "w_ple_gate": di("w_ple_gate", [4, 1024, 1024]),
    }
    dr["outT"] = nc.dram_tensor("outT", [NSEQ, 1024, 2048], F32, kind="ExternalOutput").ap()
    with ExitStack() as ctx:
        kb = KB(nc, ctx, NSEQ, LAYERS, mix)
        kb.debug = DEBUG
        kb.build(dr)
    return nc


def host_params(inp):
    f = lambda a: np.ascontiguousarray(np.asarray(a, dtype=np.float32))
    ng = f(inp["norm_gains"])
    gcols = np.ascontiguousarray(ng.reshape(4, 4, 8, 128).transpose(3, 0, 1, 2).reshape(128, 128))
    cw = f(inp["conv_a"])
    convw = np.ascontiguousarray(cw.reshape(2, 4, 3, 4, 128).transpose(4, 0, 3, 2, 1).reshape(128, 96))
    rows = np.zeros((2, RA), np.float32)
    for e in range(2):
        rows[e, 0:4] = f(inp["dt_bias"])[e]
        rows[e, 4:8] = f(inp["a_log"])[e]
        rows[e, 8:12] = f(inp["i_bias_b"])[e]
        rows[e, 12:16] = f(inp["f_bias_b"])[e]
        rows[e, 16:144] = f(inp["norm_a"])[e]
        rows[e, 144:656] = f(inp["norm_b"])[e].reshape(512)
    rows_ab = np.ascontiguousarray(np.broadcast_to(rows.reshape(1, 2 * RA), (128, 2 * RA)))
    rc = np.zeros((2, RC), np.float32)
    for o in range(2):
        rc[o, 0:1536] = f(inp["b_qkv_c"])[o]
        rc[o, 1536:1552] = f(inp["sinks_c"])[o]
    rows_c = np.ascontiguousarray(np.broadcast_to(rc[:, None, :], (2, 128, RC)))
    bo = f(inp["b_o_c"])
    bo_cols = np.ascontiguousarray(bo.reshape(2, 8, 128).transpose(2, 0, 1).reshape(128, 16))
    return {
        "consts": make_consts(), "lvlmask": make_lvlmask(), "gcols": gcols, "convw": convw, "rows_ab": rows_ab, "rows_c": rows_c,
        "bo_cols": bo_cols,
        "w_in_ab": f(inp["w_in_ab"]), "w_out_ab": f(inp["w_out_ab"]), "w_qkv_c": f(inp["w_qkv_c"]),
        "w_o_c": f(inp["w_o_c"]), "w_up": f(inp["w_up"]), "w_down": f(inp["w_down"]),
        "w_ple": f(inp["w_ple"]), "w_ple_gate": f(inp["w_ple_gate"]),
    }


def run(inp, n_cores=8, LAYERS=DEPTH, mix=("ab", "c"), trace=False):
    x = np.asarray(inp["x"], dtype=np.float32)
    p = np.asarray(inp["p"], dtype=np.float32)
    pos = np.asarray(inp["positions"], dtype=np.int32)
    B = x.shape[0]
    NSEQ = B // n_cores
    shared = host_params(inp)
    nc = build_nc(NSEQ, LAYERS, mix)
    in_maps = []
    for c in range(n_cores):
        sl = slice(c * NSEQ, (c + 1) * NSEQ)
        m = dict(shared)
        m["xT"] = np.ascontiguousarray(x[sl].transpose(0, 2, 1))
        m["pT"] = np.ascontiguousarray(p[:, sl].transpose(0, 1, 3, 2))
        m["pos"] = np.ascontiguousarray(pos[sl].reshape(-1, 16, 128).transpose(0, 2, 1))
        in_maps.append(m)
    res = run_bass_kernel_spmd(nc, in_maps, core_ids=list(range(n_cores)), trace=trace)
    out = np.concatenate([np.asarray(r["outT"]).transpose(0, 2, 1) for r in res.results], axis=0)
    return np.ascontiguousarray(out.astype(np.float32)), res


def kernel(**inputs):
    out, _ = run(inputs)
    return out
```

```python
import numpy as np
import concourse.bass as bass
import concourse.mybir as mybir
from concourse.bass_utils import run_bass_kernel_spmd
from contextlib import ExitStack

F32 = mybir.dt.float32
BF16 = mybir.dt.bfloat16
I32 = mybir.dt.int32
AF = mybir.ActivationFunctionType
ALU = mybir.AluOpType
AX = mybir.AxisListType

ENGS = ("pe", "act", "dve", "pool", "sp")
SEM_EPOCH = 4000
DMA_K = 8

D_MODEL = 1024
SEQ = 2048
DEPTH = 4
D_FF = 4096
PLE = 256
EPS = 1e-6
NEG = -30000.0
A_COLS = 2056
IN_COLS = 4112
C_ID, C_SWA, C_INV, C_ONE, C_TU, C_ML, C_NM, C_END = (0, 128, 384, 416, 544, 672, 800, 928)
RA = 656
RC = 1552


class Trk:
    __slots__ = ("name", "w", "rs", "excl")

    def __init__(self, name, excl=False):
        self.name = name
        self.w = None
        self.rs = []
        self.excl = excl


class V:
    __slots__ = ("ap", "trks")

    def __init__(self, ap, trks):
        self.ap = ap
        self.trks = tuple(trks)

    def __getitem__(self, idx):
        return V(self.ap[idx], self.trks)

    def bc(self, dt):
        return V(self.ap.bitcast(dt), self.trks)


class Ins:
    __slots__ = ("eng", "fn", "deps", "dma", "sig", "ev", "fc", "out")

    def __init__(self, eng, fn, deps, dma, out):
        self.eng = eng
        self.fn = fn
        self.deps = deps
        self.dma = dma
        self.sig = False
        self.ev = None
        self.fc = None
        self.out = out


class Sched:
    def __init__(self, nc, ctx):
        self.nc = nc
        self.ctx = ctx
        self.instrs = []
        self.per_eng = {e: [] for e in ENGS}
        self.uid = 0

    def sbuf(self, name, shape, dtype, ctx=None):
        self.uid += 1
        t = (ctx or self.ctx).enter_context(
            self.nc.sbuf_tensor("%s_%d" % (name, self.uid), list(shape), dtype))
        return V(t[tuple(slice(None) for _ in shape)], [Trk(name)])

    def psum(self, name, shape, dtype=F32):
        t = self.ctx.enter_context(self.nc.psum_tensor(name, list(shape), dtype))
        return V(t[tuple(slice(None) for _ in shape)], [Trk(name, excl=True)])

    def add(self, eng, fn, reads=(), writes=(), dma=False, out=False):
        me = len(self.instrs)
        deps = set()
        for r in reads:
            for t in r.trks:
                if t.excl:
                    if t.w is not None:
                        deps.add(t.w)
                    deps.update(t.rs)
                    t.w = me
                    t.rs = []
                else:
                    if t.w is not None:
                        deps.add(t.w)
                    t.rs.append(me)
        for w in writes:
            for t in w.trks:
                if t.w is not None:
                    deps.add(t.w)
                deps.update(t.rs)
                t.w = me
                t.rs = []
        deps.discard(me)
        self.instrs.append(Ins(eng, fn, deps, dma, out))
        self.per_eng[eng].append(me)
        return me

    def barrier(self):
        last = []
        for e in ENGS:
            lst = self.per_eng[e]
            nd = 0
            seen_c = False
            for idx in reversed(lst):
                ins = self.instrs[idx]
                if ins.fn is None:
                    continue
                if ins.dma:
                    if nd < DMA_K:
                        last.append(idx)
                        nd += 1
                elif not seen_c:
                    last.append(idx)
                    seen_c = True
                if nd >= DMA_K and seen_c:
                    break
        for e in ENGS:
            me = len(self.instrs)
            self.instrs.append(Ins(e, None, set(last), False, False))
            self.per_eng[e].append(me)

    def emit(self):
        nc = self.nc
        instrs = self.instrs
        for ins in instrs:
            for d in ins.deps:
                dd = instrs[d]
                if dd.dma:
                    continue
                if dd.eng == "pe" and ins.eng == "pe" and not ins.dma and ins.fn is not None:
                    continue
                dd.sig = True
        cnt = {e: 0 for e in ENGS}
        dcnt = {e: 0 for e in ENGS}
        for ins in instrs:
            if ins.fn is None:
                continue
            if ins.dma:
                j = dcnt[ins.eng]
                dcnt[ins.eng] += 1
                ins.ev = (("d", ins.eng, j % DMA_K), 16 * (j // DMA_K + 1))
                if j >= DMA_K:
                    ins.fc = (("d", ins.eng, j % DMA_K), 16 * (j // DMA_K))
            elif ins.sig:
                n = cnt[ins.eng]
                cnt[ins.eng] += 1
                ins.ev = (("c", ins.eng, n // SEM_EPOCH), n % SEM_EPOCH + 1)
        sems = {}
        for ins in instrs:
            if ins.ev is not None and ins.ev[0] not in sems:
                k = ins.ev[0]
                sems[k] = self.ctx.enter_context(nc.semaphore("s_%s_%s_%d" % k))
        out_events = [ins.ev for ins in instrs if ins.dma and ins.out]
        self.stats = {e: len(self.per_eng[e]) for e in ENGS}

        def run_engine(ename, eng):
            known = {}
            for idx in self.per_eng[ename]:
                ins = instrs[idx]
                best = {}
                for d in ins.deps:
                    dd = instrs[d]
                    if dd.ev is None:
                        continue
                    if (not dd.dma) and dd.eng == "pe" and ename == "pe" and not ins.dma \
                            and ins.fn is not None:
                        continue
                    k, v = dd.ev
                    if known.get(k, 0) < v and best.get(k, 0) < v:
                        best[k] = v
                if ins.fc is not None:
                    k, v = ins.fc
                    if known.get(k, 0) < v and best.get(k, 0) < v:
                        best[k] = v
                for k, v in best.items():
                    eng.wait_ge(sems[k], v)
                    known[k] = v
                if ins.fn is None:
                    continue
                bi = ins.fn(eng)
                if ins.ev is not None:
                    bi.then_inc(sems[ins.ev[0]], 16 if ins.dma else 1)
            if ename == "sp":
                best = {}
                for (k, v) in out_events:
                    if best.get(k, 0) < v:
                        best[k] = v
                for k, v in best.items():
                    if known.get(k, 0) < v:
                        eng.wait_ge(sems[k], v)

        with nc.Block() as block:
            @block.tensor
            def _(e):
                run_engine("pe", e)

            @block.scalar
            def _(e):
                run_engine("act", e)

            @block.vector
            def _(e):
                run_engine("dve", e)

            @block.gpsimd
            def _(e):
                run_engine("pool", e)

            @block.sync
            def _(e):
                run_engine("sp", e)


class KB:
    def __init__(self, nc, ctx, NSEQ, LAYERS, mix=("ab", "c")):
        self.nc = nc
        self.S = Sched(nc, ctx)
        self.NSEQ = NSEQ
        self.LAYERS = LAYERS
        self.mix = mix
        self.evi = 0
        self.psoi = 0
        self.psrc = {}
        self.psi = 0
        self.rot = {}

    def ev(self):
        self.evi += 1
        return "act" if self.evi % 2 else "dve"

    def ps(self):
        p = self.PS[self.psi % 8]
        self.psi += 1
        return p

    PSR_BANKS = {"A0": (0,), "A1": (1,), "B0": (2,), "B1": (3,), "SA": (4, 5), "SB": (6, 7)}

    def psr(self, key):
        banks = self.PSR_BANKS[key]
        n = self.psrc.get(key, 0)
        self.psrc[key] = n + 1
        return self.PS[banks[n % len(banks)]]

    def ps6(self):
        p = self.PS[self.psi % 6]
        self.psi += 1
        return p

    def ACT(self, out, in_, func, bias=0.0, scale=1.0):
        reads = [in_]
        b, s = bias, scale
        if isinstance(bias, V):
            reads.append(bias)
            b = bias.ap
        if isinstance(scale, V):
            reads.append(scale)
            s = scale.ap
        self.S.add("act", lambda e: e.activation(out=out.ap, in_=in_.ap, func=func, bias=b, scale=s),
                   reads=reads, writes=[out])

    def CP(self, eng, out, in_):
        if eng == "act":
            self.S.add("act", lambda e: e.copy(out=out.ap, in_=in_.ap), reads=[in_], writes=[out])
        else:
            self.S.add(eng, lambda e: e.tensor_copy(out=out.ap, in_=in_.ap), reads=[in_], writes=[out])

    def TT(self, eng, out, a, b, op):
        self.S.add(eng, lambda e: e.tensor_tensor(out=out.ap, in0=a.ap, in1=b.ap, op=op),
                   reads=[a, b], writes=[out])

    def TS(self, eng, out, a, s1, op0, s2=None, op1=None):
        reads = [a]
        x1, x2 = s1, s2
        if isinstance(s1, V):
            reads.append(s1)
            x1 = s1.ap
        if isinstance(s2, V):
            reads.append(s2)
            x2 = s2.ap
        if op1 is None:
            self.S.add(eng, lambda e: e.tensor_scalar(out=out.ap, in0=a.ap, scalar1=x1, scalar2=None, op0=op0),
                       reads=reads, writes=[out])
        else:
            self.S.add(eng, lambda e: e.tensor_scalar(out=out.ap, in0=a.ap, scalar1=x1, scalar2=x2,
                                                      op0=op0, op1=op1), reads=reads, writes=[out])

    def STT(self, eng, out, a, s, b, op0, op1):
        reads = [a, b]
        x = s
        if isinstance(s, V):
            reads.append(s)
            x = s.ap
        self.S.add(eng, lambda e: e.scalar_tensor_tensor(out=out.ap, in0=a.ap, scalar=x, in1=b.ap,
                                                         op0=op0, op1=op1), reads=reads, writes=[out])

    def RED(self, eng, out, in_, op):
        self.S.add(eng, lambda e: e.tensor_reduce(out=out.ap, in_=in_.ap, axis=AX.X, op=op),
                   reads=[in_], writes=[out])

    def RCP(self, out, in_):
        self.S.add("dve", lambda e: e.reciprocal(out=out.ap, in_=in_.ap), reads=[in_], writes=[out])

    def MM(self, ps, lhsT, rhs, start=True, stop=True):
        self.S.add("pe", lambda e: e.matmul(ps.ap, lhsT=lhsT.ap, rhs=rhs.ap, start=start, stop=stop),
                   reads=[lhsT, rhs], writes=[ps])

    def TR(self, ps, in_, ident):
        self.S.add("pe", lambda e: e.transpose(out=ps.ap, in_=in_.ap, identity=ident.ap),
                   reads=[in_, ident], writes=[ps])

    def DMA(self, q, out, in_, is_out=False):
        if is_out:
            self.S.add(q, lambda e: e.dma_start(out=out, in_=in_.ap), reads=[in_], dma=True, out=True)
        else:
            self.S.add(q, lambda e: e.dma_start(out=out.ap, in_=in_), writes=[out], dma=True)

    def dbg(self, name, v, dtype=F32):
        if not getattr(self, "debug", False):
            return
        shp = list(v.ap.shape)
        d = self.nc.dram_tensor(name, shp, dtype, kind="ExternalOutput").ap()
        self.DMA("sp", d, v, is_out=True)

    def lockstep(self, gens):
        gens = list(gens)
        while gens:
            alive = []
            for g in gens:
                try:
                    next(g)
                    alive.append(g)
                except StopIteration:
                    pass
            gens = alive

    def R(self, key, n, mk):
        if key not in self.rot:
            self.rot[key] = [[mk(i) for i in range(n)], 0]
        lst = self.rot[key]
        v = lst[0][lst[1] % n]
        lst[1] += 1
        return v

    def build(self, dr):
        S = self.S
        nc = self.nc
        self.dr = dr
        sb = S.sbuf
        self.PS = [S.psum("ps%d" % i, [128, 512], F32) for i in range(8)]
        self.h = [[sb("h", [128, 512], F32) for q in range(4)] for c in range(8)]
        self.cst = sb("cst", [128, C_END], F32)
        self.gcol = sb("gcol", [128, 128], F32)
        self.convw = sb("convw", [128, 96], F32)
        self.rows_ab = sb("rows_ab", [128, 2 * RA], F32)
        self.bo = sb("bo", [128, 16], F32)
        self.identb = sb("identb", [128, 128], BF16)
        self.onesb = sb("onesb", [128, 128], BF16)
        self.cos = sb("cos", [128, 16, 32], F32)
        self.sin = sb("sin", [128, 16, 32], F32)
        self.lvlm = sb("lvlm", [128, 1792], BF16)
        self.DMA("pool", self.lvlm, dr["lvlmask"])
        self.DMA("sp", self.cst, dr["consts"])
        self.DMA("sp", self.gcol, dr["gcols"])
        self.DMA("sp", self.convw, dr["convw"])
        self.DMA("sp", self.rows_ab, dr["rows_ab"])
        self.DMA("sp", self.bo, dr["bo_cols"])
        self.CP("dve", self.identb, self.cst[:, C_ID:C_ID + 128])
        self.CP("dve", self.onesb, self.cst[:, C_ONE:C_ONE + 128])
        self.negpi = sb("negpi", [128, 1], F32)
        S.add("pool", lambda e: e.memset(self.negpi.ap, -float(np.pi)), writes=[self.negpi])
        self.identf = self.cst[:, C_ID:C_ID + 128]
        self.onesf = self.cst[:, C_ONE:C_ONE + 128]

        for b in range(self.NSEQ):
            for c in range(8):
                for q in range(4):
                    self.DMA("sp", self.h[c][q], dr["xT"][b, c * 128:(c + 1) * 128, q * 512:(q + 1) * 512])
            if self.LAYERS > 1 and "c" in self.mix:
                self.rope_tables(b)
            for l in range(self.LAYERS):
                self.layer(b, l)
            for c in range(8):
                for q in range(4):
                    self.DMA("sp", dr["outT"][b, c * 128:(c + 1) * 128, q * 512:(q + 1) * 512],
                             self.h[c][q], is_out=True)
        S.emit()

    def alloc_hn(self, ctx, nq):
        self.hn = [[self.S.sbuf("hn", [128, 512], BF16, ctx) for q in range(nq)] for c in range(8)]

    def hq(self, c, q):
        row = self.hn[c]
        return row[q % len(row)]

    def gc(self, l, j, c):
        k = (l * 4 + j) * 8 + c
        return self.gcol[:, k:k + 1]

    def rstd_quarter(self, srcs, ctx):
        S = self.S
        ps = self.ps()
        for c in range(8):
            sq = self.R("sq", 2, lambda i: S.sbuf("sq", [128, 512], BF16, ctx))
            if c % 2 == 0:
                self.ACT(sq, srcs[c], AF.Square)
            else:
                self.TT("dve", sq, srcs[c], srcs[c], ALU.mult)
            self.MM(ps, self.onesb, sq, start=(c == 0), stop=(c == 7))
        rstd = self.R("rstd", 2, lambda i: S.sbuf("rstd", [128, 512], F32, ctx))
        self.ACT(rstd, ps, AF.Ln, bias=EPS, scale=1.0 / D_MODEL)
        self.ACT(rstd, rstd, AF.Exp, scale=-0.5)
        return rstd

    def norm_to_hn(self, l, j, ctx, quarters=range(4)):
        for q in quarters:
            rstd = self.rstd_quarter([self.h[c][q] for c in range(8)], ctx)
            for c in range(8):
                self.STT("dve", self.hq(c, q), self.h[c][q], self.gc(l, j, c), rstd, ALU.mult, ALU.mult)

    def add_normed(self, l, j, q, srcs, ctx):
        rstd = self.rstd_quarter(srcs, ctx)
        for c in range(8):
            if c % 4 == 3:
                self.TT("pool", srcs[c], srcs[c], rstd, ALU.mult)
                self.TS("pool", srcs[c], srcs[c], self.gc(l, j, c), ALU.mult)
                self.TT("pool", self.h[c][q], self.h[c][q], srcs[c], ALU.add)
            else:
                self.TT("dve", srcs[c], srcs[c], rstd, ALU.mult)
                self.STT("dve", self.h[c][q], srcs[c], self.gc(l, j, c), self.h[c][q], ALU.mult, ALU.add)

    def layer(self, b, l):
        S = self.S
        with ExitStack() as ctx:
            self.rot = {}
            kind0 = "ab" if l % 2 == 0 else "c"
            if kind0 == "c" and kind0 in self.mix:
                self.alloc_hn(ctx, 4)
                self.norm_to_hn(l, 0, ctx)
            self.mixT = [[S.sbuf("mixT", [128, 512], BF16, ctx) for q in range(4)] for c in range(8)]
            kind = "ab" if l % 2 == 0 else "c"
            if kind in self.mix:
                if kind == "ab":
                    with ExitStack() as c2:
                        self.alloc_hn(c2, 1)
                        self.mixer_ab(b, l, c2)
                        S.barrier()
                    self.rot = {}
                else:
                    self.mixer_c(b, l, ctx)
                self.out_proj(b, l, ctx)
            S.barrier()
        with ExitStack() as ctx:
            self.rot = {}
            self.ffn(b, l, ctx)
            S.barrier()
        with ExitStack() as ctx:
            self.rot = {}
            self.ple(b, l, ctx)
            S.barrier()

    def out_proj(self, b, l, ctx):
        S = self.S
        if l == 1:
            for c in range(8):
                self.dbg("d_mixT%d" % c, self.mixT[c][0], BF16)
        kind = "ab" if l % 2 == 0 else "c"
        wsrc = self.dr["w_out_ab"][l // 2] if kind == "ab" else self.dr["w_o_c"][l // 2]
        wv = wsrc.rearrange("(kc p) n -> p kc n", p=128)
        w = [S.sbuf("wout", [128, 8, 256], BF16, ctx) for i in range(4)]
        for i in range(4):
            self.DMA("pool", w[i], wv[:, :, i * 256:(i + 1) * 256])
        for q in range(4):
            srcs = []
            for m in range(8):
                ps = self.ps()
                for kc in range(8):
                    self.MM(ps, w[m // 2][:, kc, (m % 2) * 128:(m % 2) * 128 + 128], self.mixT[kc][q],
                            start=(kc == 0), stop=(kc == 7))
                t = self.R("mo", 8, lambda i: S.sbuf("mo", [128, 512], F32, ctx))
                if kind == "c":
                    k = (l // 2) * 8 + m
                    self.ACT(t, ps, AF.Identity, bias=self.bo[:, k:k + 1])
                else:
                    self.CP(self.ev(), t, ps)
                srcs.append(t)
            self.add_normed(l, 1, q, srcs, ctx)

    def ffn(self, b, l, ctx):
        S = self.S
        wup = self.dr["w_up"][l].rearrange("(kc p) n -> p kc n", p=128)
        wdn = self.dr["w_down"][l].rearrange("(kc p) n -> p kc n", p=128)
        act = [[S.sbuf("act", [128, 512], BF16, ctx) for n in range(2)] for k in range(16)]
        fft = [[S.sbuf("fft", [128, 512], F32, ctx) for n in range(2)] for m in range(8)]
        wslots = [S.sbuf("wffn", [128, 4096], BF16, ctx) for i in range(2)]
        self.alloc_hn(ctx, 2)
        wi = [0]

        def wslot():
            v = wslots[wi[0] % 2]
            wi[0] += 1
            return v

        for half in range(2):
            qs = [2 * half, 2 * half + 1]
            self.norm_to_hn(l, 2, ctx, quarters=qs)
            for fh in range(2):
                for g in range(4):
                    ws = wslot()
                    wu = V(ws.ap.rearrange("p (kc n) -> p kc n", kc=8), ws.trks)
                    c0 = fh * 2048 + g * 512
                    self.DMA("pool", wu, wup[:, :, c0:c0 + 512])
                    for m in range(4):
                        for n in range(2):
                            ps = self.ps()
                            for kc in range(8):
                                self.MM(ps, wu[:, kc, m * 128:(m + 1) * 128], self.hq(kc, qs[n]),
                                        start=(kc == 0), stop=(kc == 7))
                            r = self.R("relu", 2, lambda i: S.sbuf("relu", [128, 512], F32, ctx))
                            self.ACT(r, ps, AF.Relu)
                            self.TT("dve", act[g * 4 + m][n], r, r, ALU.mult)
                for g in range(4):
                    ws = wslot()
                    wd = V(ws.ap.rearrange("p (kc n) -> p kc n", kc=16), ws.trks)
                    self.DMA("pool", wd, wdn[:, fh * 16:(fh + 1) * 16, g * 256:(g + 1) * 256])
                    pss = [[self.ps() for n in range(2)] for m in range(2)]
                    for kc in range(16):
                        for m in range(2):
                            for n in range(2):
                                self.MM(pss[m][n], wd[:, kc, m * 128:(m + 1) * 128], act[kc][n],
                                        start=(kc == 0), stop=(kc == 15))
                    for m in range(2):
                        for n in range(2):
                            dst = fft[g * 2 + m][n]
                            if fh == 0:
                                self.CP(self.ev(), dst, pss[m][n])
                            else:
                                self.TT("dve", dst, dst, pss[m][n], ALU.add)
            for n in range(2):
                self.add_normed(l, 3, qs[n], [fft[m][n] for m in range(8)], ctx)

    def ple(self, b, l, ctx):
        S = self.S
        self.alloc_hn(ctx, 1)
        wg = S.sbuf("wg", [128, 8, 1024], BF16, ctx)
        wp = S.sbuf("wp", [128, 2, 1024], BF16, ctx)
        pT = [S.sbuf("pT", [128, 2, 512], BF16, ctx) for q in range(4)]
        wgv = self.dr["w_ple_gate"][l].rearrange("(kc p) n -> p kc n", p=128)
        for i in range(2):
            self.DMA("pool", wg[:, :, i * 512:(i + 1) * 512], wgv[:, :, i * 512:(i + 1) * 512])
        self.DMA("pool", wp, self.dr["w_ple"][l].rearrange("(kc p) n -> p kc n", p=128))
        pv = self.dr["pT"][l, b].rearrange("(kc p) t -> p kc t", p=128)
        for q in range(4):
            self.DMA("pool", pT[q], pv[:, :, q * 512:(q + 1) * 512])
        for q in range(4):
            for c in range(8):
                self.CP("act" if c % 2 else "pool", self.hq(c, q), self.h[c][q])
            for m in range(8):
                psg = self.ps()
                for kc in range(8):
                    self.MM(psg, wg[:, kc, m * 128:(m + 1) * 128], self.hq(kc, q), start=(kc == 0), stop=(kc == 7))
                psp = self.ps()
                for kc in range(2):
                    self.MM(psp, wp[:, kc, m * 128:(m + 1) * 128], pT[q][:, kc, :], start=(kc == 0), stop=(kc == 1))
                sg = self.R("sg", 3, lambda i: S.sbuf("sg", [128, 512], F32, ctx))
                self.ACT(sg, psg, AF.Sigmoid)
                self.TT("dve", sg, sg, psp, ALU.mult)
                self.TT("pool", self.h[m][q], self.h[m][q], sg, ALU.add)

    def rope_tables(self, b):
        S = self.S
        with ExitStack() as ctx:
            posi = S.sbuf("posi", [128, 16], I32, ctx)
            posf = S.sbuf("posf", [128, 16], F32, ctx)
            y0 = S.sbuf("y0", [128, 16, 32], F32, ctx)
            yy = S.sbuf("yy", [128, 16, 32], F32, ctx)
            ki = S.sbuf("ki", [128, 16, 32], I32, ctx)
            kf = S.sbuf("kf", [128, 16, 32], F32, ctx)
            mk = S.sbuf("mk", [128, 16, 32], F32, ctx)
            self.DMA("sp", posi, self.dr["pos"][b])
            self.CP("dve", posf, posi)
            inv = self.cst[:, C_INV:C_INV + 32]
            invb = V(inv.ap.unsqueeze(1).to_broadcast([128, 16, 32]), inv.trks)
            posb = V(posf.ap.unsqueeze(2).to_broadcast([128, 16, 32]), posf.trks)
            self.TT("dve", y0, invb, posb, ALU.mult)
            for dst, off in ((self.sin, 0.5), (self.cos, 0.75)):
                self.TS("dve", yy, y0, 1.0 / (2.0 * np.pi), ALU.mult, off, ALU.add)
                self.CP("dve", ki, yy)
                self.CP("dve", kf, ki)
                self.TT("dve", yy, yy, kf, ALU.subtract)
                self.TS("dve", mk, yy, 0.0, ALU.is_lt)
                self.TT("dve", yy, yy, mk, ALU.add)
                self.ACT(dst, yy, AF.Sin, bias=self.negpi, scale=2.0 * np.pi)
            S.barrier()

    def mixer_c(self, b, l, ctx0):
        S = self.S
        o = l // 2
        rows = S.sbuf("rowsc", [128, RC], F32, ctx0)
        self.DMA("sp", rows, self.dr["rows_c"][o])
        wv = self.dr["w_qkv_c"][o].rearrange("(kc p) n -> p kc n", p=128)
        for g in range(4):
          with ExitStack() as ctx:
            self.rot = dict((k, v) for k, v in self.rot.items() if k in ("sq", "rstd", "ntmp", "mo"))
            wq = S.sbuf("wq", [128, 8, 384], BF16, ctx)
            self.DMA("pool", wq[:, :, 0:256], wv[:, :, g * 256:(g + 1) * 256])
            self.DMA("pool", wq[:, :, 256:320], wv[:, :, 1024 + g * 64:1024 + (g + 1) * 64])
            self.DMA("pool", wq[:, :, 320:384], wv[:, :, 1280 + g * 64:1280 + (g + 1) * 64])
            brow = S.sbuf("brow", [128, 384], F32, ctx)
            self.CP("pool", brow[:, 0:256], rows[:, g * 256:(g + 1) * 256])
            self.CP("pool", brow[:, 256:320], rows[:, 1024 + g * 64:1024 + (g + 1) * 64])
            self.CP("pool", brow[:, 320:384], rows[:, 1280 + g * 64:1280 + (g + 1) * 64])
            qTall = S.sbuf("qTall", [128, 2, 2048], BF16, ctx)
            kTall = S.sbuf("kTall", [128, 2048], BF16, ctx)
            qTj = [V(qTall.ap[:, :, j * 128:(j + 1) * 128], [Trk("qTj")]) for j in range(16)]
            kTj = [V(kTall.ap[:, j * 128:(j + 1) * 128], [Trk("kTj")]) for j in range(16)]
            vtok = [S.sbuf("vtok", [128, 64], BF16, ctx) for j in range(16)]
            for j in range(16):
                qkv = self.R("qkv", 2, lambda i: S.sbuf("qkv", [128, 384], F32, ctx))
                ps = self.ps6()
                for kc in range(8):
                    self.MM(ps[:, 0:384], self.hq(kc, j // 4)[:, (j % 4) * 128:(j % 4) * 128 + 128],
                            wq[:, kc, :], start=(kc == 0), stop=(kc == 7))
                self.TT("dve", qkv, ps[:, 0:384], brow, ALU.add)
                qr = self.R("qr", 2, lambda i: S.sbuf("qr", [128, 256], BF16, ctx))
                kd = self.R("kd", 2, lambda i: S.sbuf("kd", [128, 2, 64], BF16, ctx))
                for (src, dst, nh) in ((qkv[:, 0:256], qr, 4), (qkv[:, 256:320], kd[:, 0, :], 1)):
                    sv = V(src.ap.rearrange("p (h t d) -> p h t d", t=2, d=32), src.trks)
                    dv = V(dst.ap.rearrange("p (h t d) -> p h t d", t=2, d=32), dst.trks)
                    cb = V(self.cos.ap[:, j, :].unsqueeze(1).to_broadcast([128, nh, 32]), self.cos.trks)
                    sb_ = V(self.sin.ap[:, j, :].unsqueeze(1).to_broadcast([128, nh, 32]), self.sin.trks)
                    t1 = self.R("rt1", 2, lambda i: S.sbuf("rt1", [128, 4, 32], F32, ctx))
                    t2 = self.R("rt2", 2, lambda i: S.sbuf("rt2", [128, 4, 32], F32, ctx))
                    t3 = self.R("rt3", 2, lambda i: S.sbuf("rt3", [128, 4, 32], F32, ctx))
                    t4 = self.R("rt4", 2, lambda i: S.sbuf("rt4", [128, 4, 32], F32, ctx))
                    self.TT("dve", t1[:, 0:nh, :], sv[:, :, 0, :], cb, ALU.mult)
                    self.TT("dve", t2[:, 0:nh, :], sv[:, :, 1, :], sb_, ALU.mult)
                    self.TT("dve", dv[:, :, 0, :], t1[:, 0:nh, :], t2[:, 0:nh, :], ALU.subtract)
                    self.TT("pool", t3[:, 0:nh, :], sv[:, :, 1, :], cb, ALU.mult)
                    self.TT("pool", t4[:, 0:nh, :], sv[:, :, 0, :], sb_, ALU.mult)
                    self.TT("pool", dv[:, :, 1, :], t3[:, 0:nh, :], t4[:, 0:nh, :], ALU.add)
                self.CP("act", kd[:, 1, :], kd[:, 0, :])
                self.CP("act", vtok[j], qkv[:, 320:384])
                pst = self.ps6().bc(BF16)
                for i in range(2):
                    self.TR(pst[:, i * 128:(i + 1) * 128], qr[:, i * 128:(i + 1) * 128], self.identb)
                kdf = V(kd.ap.rearrange("p a d -> p (a d)"), kd.trks)
                self.TR(pst[:, 256:384], kdf, self.identb)
                src = V(pst.ap[:, 0:256].rearrange("p (i t) -> p i t", t=128), pst.trks)
                self.CP("act", qTj[j], src)
                self.CP("dve", kTj[j], pst[:, 256:384])
            for j in range(16):
                nk = 1 if j == 0 else 2
                SS = nk * 128
                mask = self.cst[:, C_SWA + 256 - SS:C_SWA + 256]
                pso = self.PS[6 + (self.psoi % 2)]
                self.psoi += 1
                recg = self.R("recg", 2, lambda i: S.sbuf("recg", [128, 4], F32, ctx))
                ktr = [kTj[j - 1], kTj[j]] if j > 0 else [kTj[0]]
                sc4 = self.R("sc4", 2, lambda i_: S.sbuf("sc4", [128, 4, 256], F32, ctx))
                for hl in range(4):
                    i = hl // 2
                    hh = hl % 2
                    ps = self.ps6()
                    for kb in range(nk):
                        self.MM(ps[:, kb * 128:(kb + 1) * 128],
                                qTj[j][hh * 64:(hh + 1) * 64, i, :], ktr[kb][hh * 64:(hh + 1) * 64, :])
                    self.STT("dve", sc4[:, hl, 0:SS], ps[:, 0:SS], 0.125, mask, ALU.mult, ALU.add)
                sk4 = rows[:, 1536 + 4 * g:1536 + 4 * g + 4]
                mx4 = self.R("mx4", 2, lambda i_: S.sbuf("mx4", [128, 4], F32, ctx))
                self.RED("dve", mx4, sc4[:, :, 0:SS], ALU.max)
                self.TT("dve", mx4, mx4, sk4, ALU.max)
                nmx4 = self.R("nmx4", 2, lambda i_: S.sbuf("nmx4", [128, 4], F32, ctx))
                self.TS("dve", nmx4, mx4, -1.0, ALU.mult)
                pr4 = self.R("pr4", 2, lambda i_: S.sbuf("pr4", [128, 4, 256], BF16, ctx))
                for hl in range(4):
                    self.ACT(pr4[:, hl, 0:SS], sc4[:, hl, 0:SS], AF.Exp, bias=nmx4[:, hl:hl + 1])
                rs4 = self.R("rs4", 2, lambda i_: S.sbuf("rs4", [128, 4], F32, ctx))
                self.RED("dve", rs4, pr4[:, :, 0:SS], ALU.add)
                es4 = self.R("es4", 2, lambda i_: S.sbuf("es4", [128, 4], F32, ctx))
                self.TT("dve", es4, sk4, mx4, ALU.subtract)
                self.ACT(es4, es4, AF.Exp)
                self.TT("dve", rs4, rs4, es4, ALU.add)
                self.RCP(recg, rs4)
                for hl in range(4):
                    pst = self.ps6().bc(BF16)
                    for kb in range(nk):
                        self.TR(pst[:, kb * 128:(kb + 1) * 128], pr4[:, hl, kb * 128:(kb + 1) * 128], self.identb)
                    prT = self.R("prT", 4, lambda i_: S.sbuf("prT", [128, 256], BF16, ctx))
                    self.CP(self.ev(), prT[:, 0:SS], pst[:, 0:SS])
                    for kb in range(nk):
                        jk = j - 1 + kb if j > 0 else 0
                        self.MM(pso[:, hl * 64:hl * 64 + 64], prT[:, kb * 128:(kb + 1) * 128],
                                vtok[jk], start=(kb == 0), stop=(kb == nk - 1))
                otok = self.R("otok", 2, lambda i: S.sbuf("otok", [128, 256], BF16, ctx))
                psov = V(pso.ap[:, 0:256].rearrange("p (h d) -> p h d", d=64), pso.trks)
                recb = V(recg.ap.unsqueeze(2).to_broadcast([128, 4, 64]), recg.trks)
                ov = V(otok.ap.rearrange("p (h d) -> p h d", d=64), otok.trks)
                self.TT("dve", ov, psov, recb, ALU.mult)
                pst = self.ps6().bc(BF16)
                for i in range(2):
                    self.TR(pst[:, i * 128:(i + 1) * 128], otok[:, i * 128:(i + 1) * 128], self.identb)
                for i in range(2):
                    self.CP(self.ev(), self.mixT[2 * g + i][j // 4][:, (j % 4) * 128:(j % 4) * 128 + 128],
                            pst[:, i * 128:(i + 1) * 128])
            S.barrier()

    def mixer_ab(self, b, l, ctx0):
        S = self.S
        e = l // 2
        rab = lambda a, n: self.rows_ab[:, e * RA + a:e * RA + a + n]
        win = self.dr["w_in_ab"][e].rearrange("(kc p) n -> p kc n", p=128)
        sb = lambda name, shape, dt=F32: S.sbuf(name, shape, dt, ctx0)
        identf, onesf, identb = self.identf, self.onesf, self.identb
        triU = self.cst[:, C_TU:C_TU + 128]
        maskL = self.cst[:, C_ML:C_ML + 128]
        negm = self.cst[:, C_NM:C_NM + 128]
        wgate = sb("wgate", [128, 8, 16], BF16)
        self.DMA("pool", wgate[:, :, 0:8], win[:, :, 1536:1544])
        self.DMA("pool", wgate[:, :, 8:16], win[:, :, 3592:3600])
        G = sb("G", [128, 16, 16])
        for j in range(16):
            if j % 4 == 0:
                self.norm_to_hn(l, 0, ctx0, quarters=[j // 4])
            ps = self.ps()
            for kc in range(8):
                self.MM(ps[:, 0:16], self.hq(kc, j // 4)[:, (j % 4) * 128:(j % 4) * 128 + 128], wgate[:, kc, :],
                        start=(kc == 0), stop=(kc == 7))
            self.CP(self.ev(), G[:, j, :], ps[:, 0:16])
        bc4 = lambda v: V(v.ap.unsqueeze(1).to_broadcast([128, 16, 4]), v.trks)
        BETA = sb("BETA", [128, 16, 4])
        self.ACT(BETA, G[:, :, 0:4], AF.Sigmoid)
        X = sb("X", [128, 16, 4])
        T1 = sb("T1", [128, 16, 4])
        T2 = sb("T2", [128, 16, 4])
        SP = sb("SP", [128, 16, 4])

        def softplus(dst, x):
            self.TS("dve", T1, x, -1.0, ALU.mult)
            self.TT("dve", T1, T1, x, ALU.min)
            self.ACT(T1, T1, AF.Exp)
            self.ACT(T1, T1, AF.Ln, bias=1.0)
            self.TS("dve", T2, x, 0.0, ALU.max)
            self.TT("dve", dst, T1, T2, ALU.add)

        GF = sb("GF", [128, 16, 8])
        self.TT("dve", X, G[:, :, 4:8], bc4(rab(0, 4)), ALU.add)
        softplus(SP, X)
        negA = sb("negA", [128, 4])
        self.ACT(negA, rab(4, 4), AF.Exp)
        self.TS("dve", negA, negA, -1.0, ALU.mult)
        self.TT("dve", GF[:, :, 0:4], SP, bc4(negA), ALU.mult)
        ILOG = sb("ILOG", [128, 16, 4])
        self.TT("dve", ILOG, G[:, :, 8:12], bc4(rab(8, 4)), ALU.add)
        self.TT("dve", X, G[:, :, 12:16], bc4(rab(12, 4)), ALU.add)
        self.TS("dve", X, X, -1.0, ALU.mult)
        softplus(SP, X)
        self.TS("dve", GF[:, :, 4:8], SP, -1.0, ALU.mult)
        CUM = sb("CUM", [128, 16, 16])
        for j in range(16):
            ps = self.ps()
            self.MM(ps[:, 0:8], triU, GF[:, j, :])
            self.MM(ps[:, 8:16], onesf, GF[:, j, :])
            self.CP(self.ev(), CUM[:, j, :], ps[:, 0:16])
        gcum, bcum, gtot = CUM[:, :, 0:4], CUM[:, :, 4:8], CUM[:, :, 8:12]
        BG = sb("BG", [128, 16, 4])
        KD = sb("KD", [128, 16, 4])
        EGL = sb("EGL", [128, 16, 4])
        RR = sb("RR", [128, 16, 4])
        self.ACT(BG, gcum, AF.Exp)
        self.TT("dve", BG, BG, BETA, ALU.mult)
        self.TT("dve", KD, gtot, gcum, ALU.subtract)
        self.ACT(KD, KD, AF.Exp)
        self.ACT(EGL, gtot, AF.Exp)
        self.TT("dve", RR, ILOG, bcum, ALU.subtract)
        cols = [i * 128 for i in range(4)]
        vexts = [sb("vext", [128, 129], BF16) for _ in range(4)]
        for v in vexts:
            S.add("pool", lambda e_, v=v: e_.memset(v.ap[:, 128:129], 1.0), writes=[v])
        wh = sb("wh", [128, 8, 1024], BF16)
        Sa = sb("Sa", [128, 128])
        Sab = sb("Sab", [128, 128], BF16)
        Cx = sb("Cx", [128, 129])
        Cb = sb("Cb", [128, 129], BF16)
        mst = sb("mst", [128, 1])
        cwork = sb("cwork", [128, 515])
        ctail = [sb("ctail", [128, 4]) for _ in range(3)]
        qT = sb("qT", [128, 512], BF16)
        kT = sb("kT", [128, 512], BF16)
        vT = sb("vT", [128, 512], BF16)
        qbT = sb("qbT", [128, 512], BF16)
        kbT = sb("kbT", [128, 512], BF16)
        acc = sb("acc", [128, 512])
        sl = acc
        t128 = lambda name, dt=F32: self.R(name, 2, lambda i_: S.sbuf(name, [128, 128], dt, ctx0))
        c1 = lambda name, n=1: self.R(name, 2, lambda i_: S.sbuf(name, [128, n], F32, ctx0))

        def finish(key, pf, o, gain, gate, ci, q, tsl):
            tmp = t128(pf + "f_tmp")
            self.ACT(tmp, o, AF.Square)
            yield
            ss = c1(pf + "f_ss")
            self.RED("dve", ss, tmp, ALU.add)
            yield
            self.ACT(ss, ss, AF.Ln, bias=EPS, scale=1.0 / 128.0)
            self.ACT(ss, ss, AF.Exp, scale=-0.5)
            yield
            y = t128(pf + "f_y")
            self.STT("dve", y, o, ss, gain, ALU.mult, ALU.mult)
            yb = t128(pf + "f_yb", BF16)
            self.TT("dve", yb, y, gate, ALU.mult)
            yield
            pY = self.psr(key).bc(BF16)
            self.TR(pY[:, 0:128], yb, identb)
            self.CP(self.ev(), self.mixT[ci][q][:, tsl], pY[:, 0:128])
            yield

        for i in range(4):
            gcols = [i * 128, 512 + i * 128, 1024 + i * 128, 2056 + i * 128, 2568 + i * 128,
                     3080 + i * 128, 3600 + i * 128, 1544 + i * 128]
            for gi, c0 in enumerate(gcols):
                self.DMA("pool", wh[:, :, gi * 128:(gi + 1) * 128], win[:, :, c0:c0 + 128])
            for t_ in (Sa, Sab, Cx, Cb, mst):
                S.add("pool", lambda e_, t_=t_: e_.memset(t_.ap, 0.0), writes=[t_])
            for g in range(3):
                S.add("pool", lambda e_, t_=ctail[g]: e_.memset(t_.ap, 0.0), writes=[ctail[g]])
            for q in range(4):
                self.norm_to_hn(l, 0, ctx0, quarters=[q])
                for gi in range(5):
                    ps = self.ps()
                    for kc in range(8):
                        self.MM(ps, wh[:, kc, gi * 128:(gi + 1) * 128], self.hq(kc, q), start=(kc == 0), stop=(kc == 7))
                    if gi < 3:
                        cb_ = cwork
                        self.CP("pool", cb_[:, 0:3], ctail[gi][:, 0:3])
                        self.CP(self.ev(), cb_[:, 3:515], ps)
                        wc = lambda k: self.convw[:, ((e * 4 + i) * 3 + gi) * 4 + k:((e * 4 + i) * 3 + gi) * 4 + k + 1]
                        self.TS("dve", acc, cb_[:, 0:512], wc(0), ALU.mult)
                        for k in range(1, 4):
                            self.STT("dve", acc, cb_[:, k:k + 512], wc(k), acc, ALU.mult, ALU.add)
                        self.CP("pool", ctail[gi][:, 0:3], cb_[:, 512:515])
                        self.ACT(sl, acc, AF.Silu)
                        if gi < 2:
                            sqb = self.R("sq", 2, lambda i_: S.sbuf("sq", [128, 512], BF16, ctx0))
                            self.ACT(sqb, sl, AF.Square)
                            p2 = self.ps()
                            self.MM(p2, self.onesb, sqb)
                            rn5 = p2
                            self.ACT(rn5, p2, AF.Ln, bias=EPS)
                            self.ACT(rn5, rn5, AF.Exp, scale=-0.5)
                            if gi == 0:
                                self.STT("dve", qT, sl, 128.0 ** -0.5, rn5, ALU.mult, ALU.mult)
                            else:
                                self.TT("dve", kT, sl, rn5, ALU.mult)
                        else:
                            self.CP("pool", vT, sl)
                    elif gi == 3:
                        self.ACT(qbT, ps, AF.Copy, scale=128.0 ** -0.5)
                    else:
                        self.CP("dve", kbT, ps)
                def mk_tile(jj):
                    j = 4 * q + jj
                    tsl = slice(jj * 128, (jj + 1) * 128)
                    col = lambda T_, off=0: T_[:, j, off + i:off + i + 1]
                    pt = self.ps()
                    for kc in range(8):
                        self.MM(pt, self.hq(kc, q)[:, tsl], wh[:, kc, 512:1024], start=(kc == 0), stop=(kc == 7))
                    h4 = lambda name, dt=F32: self.R(name, 4, lambda i_: S.sbuf(name, [128, 128], dt, ctx0))
                    kbtok = h4("kbtok")
                    self.CP("act", kbtok, pt[:, 0:128])
                    vext = vexts[j % 4]
                    self.CP("dve", vext[:, 0:128], pt[:, 128:256])
                    og = h4("og")
                    self.ACT(og, pt[:, 256:384], AF.Sigmoid)
                    sz = h4("sz")
                    self.ACT(sz, pt[:, 384:512], AF.Silu)
                    beta, gc_, bg, kdc, egl = col(BETA), col(CUM), col(BG), col(KD), col(EGL)
                    rcol, bcc, bl = col(RR), col(CUM, 4), col(CUM, 12)
                    gA = rab(16, 128)
                    gB = rab(144 + i * 128, 128)
                    h4 = lambda name, dt=F32: self.R(name, 4, lambda i_: S.sbuf(name, [128, 128], dt, ctx0))
                    kdec, qdT, ATb, wTn = h4("kdec", BF16), h4("qdT", BF16), h4("ATb", BF16), h4("wTn", BF16)
                    u, P0T, kw0 = h4("u"), h4("P0T", BF16), h4("kw0", BF16)
                    mi = self.R("mi", 4, lambda i_: S.sbuf("mi", [128, 1], F32, ctx0))
                    maxr = self.R("maxr", 4, lambda i_: S.sbuf("maxr", [128, 1], F32, ctx0))
                    kA = "A%d" % (jj % 2)
                    kB = "B%d" % (jj % 2)
                    def bulkA():
                        ptr = self.psr(kA).bc(BF16)
                        self.TR(ptr[:, 0:128], kT[:, tsl], identb)
                        self.TR(ptr[:, 128:256], vT[:, tsl], identb)
                        Rm = self.R("Rm", 2, lambda i_: S.sbuf("Rm", [128, 256], F32, ctx0))
                        self.TS("dve", Rm[:, 0:128], ptr[:, 128:256], beta, ALU.mult)
                        yield
                        self.TS("dve", Rm[:, 128:256], ptr[:, 0:128], bg, ALU.mult)
                        yield
                        self.TS("dve", kdec, ptr[:, 0:128], kdc, ALU.mult)
                        yield
                        dg = t128("dg")
                        self.TS("dve", dg, identf, gc_, ALU.mult)
                        yield
                        pG = self.psr(kA)
                        self.MM(pG[:, 0:128], onesf, dg)
                        D1 = t128("D1")
                        self.STT("dve", D1, pG[:, 0:128], gc_, maskL, ALU.subtract, ALU.mult)
                        yield
                        DT = t128("DT")
                        self.STT("dve", DT, pG[:, 0:128], gc_, triU, ALU.subtract, ALU.mult)
                        yield
                        EgR = pG[:, 0:128]
                        self.ACT(EgR, pG[:, 0:128], AF.Exp)
                        yield
                        self.ACT(D1, D1, AF.Exp, scale=-1.0)
                        yield
                        self.TT("dve", D1, D1, maskL, ALU.mult)
                        yield
                        self.ACT(DT, DT, AF.Exp)
                        yield
                        self.TT("dve", DT, DT, triU, ALU.mult)
                        yield
                        self.TT("dve", qdT, qT[:, tsl], EgR, ALU.mult)
                        yield
                        pK = self.psr(kA)
                        self.MM(pK[:, 0:128], kT[:, tsl], kT[:, tsl])
                        self.MM(pK[:, 128:256], kT[:, tsl], qT[:, tsl])
                        Lf = t128("Lf")
                        self.STT("dve", Lf, pK[:, 0:128], beta, D1, ALU.mult, ALU.mult)
                        yield
                        self.TT("dve", ATb, pK[:, 128:256], DT, ALU.mult)
                        yield
                        pL = self.psr(kA)
                        self.TR(pL[:, 0:128], Lf, identf)
                        LTf = t128("LTf")
                        self.CP("act", LTf, pL[:, 0:128])
                        yield
                        Mf = t128("Mf")
                        MTf = t128("MTf")
                        t128b = lambda name: self.R(name, 2, lambda i_: S.sbuf(name, [128, 128], F32, ctx0))
                        Ck = t128b("Ck")
                        CTk = t128b("CTk")
                        self.TT("pool", Ck, Lf, self.lvlm[:, 0:128], ALU.mult)
                        self.TT("pool", CTk, LTf, self.lvlm[:, 896:1024], ALU.mult)
                        self.TT("pool", Mf, identf, Ck, ALU.subtract)
                        self.TT("pool", MTf, identf, CTk, ALU.subtract)
                        yield
                        for k in range(1, 7):
                            Ck = t128b("Ck")
                            CTk = t128b("CTk")
                            self.TT("pool", Ck, Lf, self.lvlm[:, k * 128:(k + 1) * 128], ALU.mult)
                            self.TT("pool", CTk, LTf, self.lvlm[:, 896 + k * 128:896 + (k + 1) * 128], ALU.mult)
                            pa_ = self.psr(kA)
                            self.MM(pa_[:, 0:128], CTk, Mf)
                            self.MM(pa_[:, 128:256], Ck, MTf)
                            T1f = t128("T1f")
                            T3f = t128("T3f")
                            self.CP("act", T1f, pa_[:, 0:128])
                            self.CP("dve", T3f, pa_[:, 128:256])
                            yield
                            pb_ = self.psr(kA)
                            self.MM(pb_[:, 0:128], MTf, T1f)
                            self.MM(pb_[:, 128:256], Mf, T3f)
                            self.TT("dve", Mf, Mf, pb_[:, 0:128], ALU.subtract)
                            self.TT("dve", MTf, MTf, pb_[:, 128:256], ALU.subtract)
                            yield
                        pX = self.psr(kA)
                        self.MM(pX[:, 0:256], MTf, Rm)
                        wb = t128("wb", BF16)
                        self.CP("act", u, pX[:, 0:128])
                        yield
                        self.CP("dve", wb, pX[:, 128:256])
                        yield
                        pW = self.psr(kA).bc(BF16)
                        self.TR(pW[:, 0:128], wb, identb)
                        self.ACT(wTn, pW[:, 0:128], AF.Copy, scale=-1.0)
                        yield
                    def scanA():
                        pV = self.psr("SA")
                        self.MM(pV[:, 0:128], wTn, Sab)
                        vn = t128("vn", BF16)
                        self.TT("dve", vn, u, pV[:, 0:128], ALU.add)
                        yield
                        pO = self.psr("SA")
                        self.MM(pO[:, 0:128], qdT, Sab, start=True, stop=False)
                        self.MM(pO[:, 0:128], ATb, vn, start=False, stop=True)
                        oA = t128("oA")
                        self.CP("act", oA, pO[:, 0:128])
                        yield
                        pS = self.psr("SA")
                        self.MM(pS[:, 0:128], kdec, vn)
                        self.STT("dve", Sa, Sa, egl, pS[:, 0:128], ALU.mult, ALU.add)
                        yield
                        self.CP("act", Sab, Sa)
                        yield
                        yield from finish("SA", "a", oA, gA, sz, i, q, tsl)
                    def bulkB():
                        dgr = t128("dgr")
                        self.TS("dve", dgr, identf, rcol, ALU.mult)
                        yield
                        pR = self.psr(kB)
                        self.MM(pR[:, 0:128], onesf, dgr)
                        tmpB = t128("tmpB")
                        self.TT("dve", tmpB, pR[:, 0:128], negm, ALU.add)
                        yield
                        self.RED("dve", maxr, pR[:, 0:128], ALU.max)
                        yield
                        mr = c1("mr")
                        self.RED("dve", mr, tmpB, ALU.max)
                        yield
                        nmr = c1("nmr")
                        self.TS("dve", nmr, mr, -1.0, ALU.mult)
                        yield
                        Eb = t128("Eb")
                        self.ACT(Eb, tmpB, AF.Exp, bias=nmr)
                        yield
                        pQ = self.psr(kB)
                        self.MM(pQ[:, 0:128], qbT[:, tsl], kbT[:, tsl])
                        P0 = t128("P0", BF16)
                        self.TT("dve", P0, pQ[:, 0:128], Eb, ALU.mult)
                        yield
                        pP0 = self.psr(kB).bc(BF16)
                        self.TR(pP0[:, 0:128], P0, identb)
                        self.CP("act", P0T, pP0[:, 0:128])
                        yield
                        ew = c1("ew")
                        self.TT("dve", ew, rcol, maxr, ALU.subtract)
                        yield
                        self.ACT(ew, ew, AF.Exp)
                        yield
                        self.TS("dve", kw0, kbtok, ew, ALU.mult)
                        yield
                        self.TT("dve", mi, mr, bcc, ALU.add)
                        yield
                    def scanB():
                        a_ = c1("a_")
                        self.TT("dve", a_, bcc, mst, ALU.add)
                        yield
                        mt = c1("mt")
                        self.TT("dve", mt, a_, mi, ALU.max)
                        yield
                        e3 = c1("e3", 3)
                        self.TT("dve", e3[:, 0:1], a_, mt, ALU.subtract)
                        yield
                        self.TT("dve", e3[:, 1:2], mi, mt, ALU.subtract)
                        yield
                        self.TS("dve", e3[:, 2:3], mt, -1.0, ALU.mult)
                        yield
                        self.ACT(e3, e3, AF.Exp)
                        yield
                        p1 = self.psr("SB")
                        self.MM(p1[:, 0:129], qbT[:, tsl], Cb)
                        p2 = self.psr("SB")
                        self.MM(p2[:, 0:129], P0T, vext)
                        nd = self.R("nd", 2, lambda i_: S.sbuf("nd", [128, 129], F32, ctx0))
                        self.TS("dve", nd, p1[:, 0:129], e3[:, 0:1], ALU.mult)
                        yield
                        self.STT("dve", nd, p2[:, 0:129], e3[:, 1:2], nd, ALU.mult, ALU.add)
                        yield
                        td = c1("td")
                        self.TS("dve", td, nd[:, 128:129], -1.0, ALU.mult)
                        yield
                        self.TT("dve", td, td, nd[:, 128:129], ALU.max)
                        yield
                        self.TT("dve", td, td, e3[:, 2:3], ALU.max)
                        yield
                        self.RCP(td, td)
                        yield
                        hB = t128("hB")
                        self.TS("dve", hB, nd[:, 0:128], td, ALU.mult)
                        yield
                        mm = c1("mm")
                        self.TT("dve", mm, mst, maxr, ALU.max)
                        yield
                        e2 = c1("e2", 2)
                        self.TT("dve", e2[:, 0:1], mst, mm, ALU.subtract)
                        yield
                        self.TT("dve", e2[:, 1:2], maxr, mm, ALU.subtract)
                        yield
                        self.ACT(e2, e2, AF.Exp)
                        yield
                        p3 = self.psr("SB")
                        self.MM(p3[:, 0:129], kw0, vext)
                        self.TS("dve", Cx, Cx, e2[:, 0:1], ALU.mult)
                        yield
                        self.STT("dve", Cx, p3[:, 0:129], e2[:, 1:2], Cx, ALU.mult, ALU.add)
                        yield
                        self.CP("act", Cb, Cx)
                        yield
                        self.TT("dve", mst, bl, mm, ALU.add)
                        yield
                        yield from finish("SB", "b", hB, gB, og, 4 + i, q, tsl)
                    return bulkA, scanA, bulkB, scanB
                TL = [mk_tile(jj) for jj in range(4)]
                def seq(*gs):
                    for g_ in gs:
                        yield from g_
                def par(*gs):
                    gs = list(gs)
                    while gs:
                        al = []
                        for g_ in gs:
                            try:
                                next(g_)
                                al.append(g_)
                            except StopIteration:
                                pass
                        gs = al
                        yield
                self.lockstep([TL[0][0](), TL[0][2](), TL[1][0](), TL[1][2]()])
                self.lockstep([TL[2][0](), TL[2][2](), TL[3][0](), TL[3][2](),
                               seq(par(TL[0][1](), TL[0][3]()), par(TL[1][1](), TL[1][3]()))])
                self.lockstep([seq(par(TL[2][1](), TL[2][3]()), par(TL[3][1](), TL[3][3]()))])


def make_consts():
    c = np.zeros((128, C_END), np.float32)
    i = np.arange(128)
    P, Fr = i[:, None], i[None, :]
    same = (P // 64) == (Fr // 64)
    c[:, C_ID:C_ID + 128] = np.eye(128)
    s = np.arange(256)[None, :]
    c[:, C_SWA:C_SWA + 256] = np.where((s > P) & (s <= P + 128), 0.0, NEG)
    half = 32
    inv = (10000.0 ** (-np.arange(half, dtype=np.float32) / half)).astype(np.float32)
    c[:, C_INV:C_INV + 32] = inv[None, :]
    c[:, C_ONE:C_ONE + 128] = 1.0
    c[:, C_TU:C_TU + 128] = (Fr >= P)
    c[:, C_ML:C_ML + 128] = (Fr < P)
    c[:, C_NM:C_NM + 128] = np.where(Fr <= P, 0.0, NEG)
    return c


DEBUG = False


def make_lvlmask():
    m = np.zeros((128, 1792), np.float32)
    i = np.arange(128)
    c, e = i[:, None], i[None, :]
    for k in range(7):
        sz = 1 << k
        mk = ((c // (2 * sz)) == (e // (2 * sz))) & ((c % (2 * sz)) >= sz) & ((e % (2 * sz)) < sz)
        m[:, k * 128:(k + 1) * 128] = mk
        m[:, 896 + k * 128:896 + (k + 1) * 128] = mk.T
    return m


def build_nc(NSEQ, LAYERS, mix=("ab", "c")):
    nc = bass.Bass("TRN2", target_bir_lowering=False)

    def di(name, shape, dt=F32):
        return nc.dram_tensor(name, list(shape), dt, kind="ExternalInput").ap()

    dr = {
        "xT": di("xT", [NSEQ, 1024, 2048]),
        "pT": di("pT", [4, NSEQ, 256, 2048]),
        "pos": di("pos", [NSEQ, 128, 16], I32),
        "consts": di("consts", [128, C_END]),
        "lvlmask": di("lvlmask", [128, 1792]),
        "gcols": di("gcols", [128, 128]),
        "convw": di("convw", [128, 96]),
        "rows_ab": di("rows_ab", [128, 2 * RA]),
        "rows_c": di("rows_c", [2, 128, RC]),
        "bo_cols": di("bo_cols", [128, 16]),
        "w_in_ab": di("w_in_ab", [2, 1024, IN_COLS]),
        "w_out_ab": di("w_out_ab", [2, 1024, 1024]),
        "w_qkv_c": di("w_qkv_c", [2, 1024, 1536]),
        "w_o_c": di("w_o_c", [2, 1024, 1024]),
        "w_up": di("w_up", [4, 1024, 4096]),
        "w_down": di("w_down", [4, 4096, 1024]),
        "w_ple": di("w_ple", [4, 256, 1024]),
        "w_ple_gate": di("w_ple_gate", [4, 1024, 1024]),
    }
    dr["outT"] = nc.dram_tensor("outT", [NSEQ, 1024, 2048], F32, kind="ExternalOutput").ap()
    with ExitStack() as ctx:
        kb = KB(nc, ctx, NSEQ, LAYERS, mix)
        kb.debug = DEBUG
        kb.build(dr)
    return nc


def host_params(inp):
    f = lambda a: np.ascontiguousarray(np.asarray(a, dtype=np.float32))
    ng = f(inp["norm_gains"])
    gcols = np.ascontiguousarray(ng.reshape(4, 4, 8, 128).transpose(3, 0, 1, 2).reshape(128, 128))
    cw = f(inp["conv_a"])
    convw = np.ascontiguousarray(cw.reshape(2, 4, 3, 4, 128).transpose(4, 0, 3, 2, 1).reshape(128, 96))
    rows = np.zeros((2, RA), np.float32)
    for e in range(2):
        rows[e, 0:4] = f(inp["dt_bias"])[e]
        rows[e, 4:8] = f(inp["a_log"])[e]
        rows[e, 8:12] = f(inp["i_bias_b"])[e]
        rows[e, 12:16] = f(inp["f_bias_b"])[e]
        rows[e, 16:144] = f(inp["norm_a"])[e]
        rows[e, 144:656] = f(inp["norm_b"])[e].reshape(512)
    rows_ab = np.ascontiguousarray(np.broadcast_to(rows.reshape(1, 2 * RA), (128, 2 * RA)))
    rc = np.zeros((2, RC), np.float32)
    for o in range(2):
        rc[o, 0:1536] = f(inp["b_qkv_c"])[o]
        rc[o, 1536:1552] = f(inp["sinks_c"])[o]
    rows_c = np.ascontiguousarray(np.broadcast_to(rc[:, None, :], (2, 128, RC)))
    bo = f(inp["b_o_c"])
    bo_cols = np.ascontiguousarray(bo.reshape(2, 8, 128).transpose(2, 0, 1).reshape(128, 16))
    return {
        "consts": make_consts(), "lvlmask": make_lvlmask(), "gcols": gcols, "convw": convw, "rows_ab": rows_ab, "rows_c": rows_c,
        "bo_cols": bo_cols,
        "w_in_ab": f(inp["w_in_ab"]), "w_out_ab": f(inp["w_out_ab"]), "w_qkv_c": f(inp["w_qkv_c"]),
        "w_o_c": f(inp["w_o_c"]), "w_up": f(inp["w_up"]), "w_down": f(inp["w_down"]),
        "w_ple": f(inp["w_ple"]), "w_ple_gate": f(inp["w_ple_gate"]),
    }


def run(inp, n_cores=8, LAYERS=DEPTH, mix=("ab", "c"), trace=False):
    x = np.asarray(inp["x"], dtype=np.float32)
    p = np.asarray(inp["p"], dtype=np.float32)
    pos = np.asarray(inp["positions"], dtype=np.int32)
    B = x.shape[0]
    NSEQ = B // n_cores
    shared = host_params(inp)
    nc = build_nc(NSEQ, LAYERS, mix)
    in_maps = []
    for c in range(n_cores):
        sl = slice(c * NSEQ, (c + 1) * NSEQ)
        m = dict(shared)
        m["xT"] = np.ascontiguousarray(x[sl].transpose(0, 2, 1))
        m["pT"] = np.ascontiguousarray(p[:, sl].transpose(0, 1, 3, 2))
        m["pos"] = np.ascontiguousarray(pos[sl].reshape(-1, 16, 128).transpose(0, 2, 1))
        in_maps.append(m)
    res = run_bass_kernel_spmd(nc, in_maps, core_ids=list(range(n_cores)), trace=trace)
    out = np.concatenate([np.asarray(r["outT"]).transpose(0, 2, 1) for r in res.results], axis=0)
    return np.ascontiguousarray(out.astype(np.float32)), res


def kernel(**inputs):
    out, _ = run(inputs)
    return out
```

```python
import numpy as np
import concourse.bass as bass
import concourse.mybir as mybir
from concourse.bass_utils import run_bass_kernel_spmd
from contextlib import ExitStack

F32 = mybir.dt.float32
BF16 = mybir.dt.bfloat16
I32 = mybir.dt.int32
AF = mybir.ActivationFunctionType
ALU = mybir.AluOpType
AX = mybir.AxisListType

ENGS = ("pe", "act", "dve", "pool", "sp")
SEM_EPOCH = 4000
DMA_K = 8

D_MODEL = 1024
SEQ = 2048
DEPTH = 4
D_FF = 4096
PLE = 256
EPS = 1e-6
NEG = -30000.0
A_COLS = 2056
IN_COLS = 4112
C_ID, C_SWA, C_INV, C_ONE, C_TU, C_ML, C_NM, C_END = (0, 128, 384, 416, 544, 672, 800, 928)
RA = 656
RC = 1552


class Trk:
    __slots__ = ("name", "w", "rs", "excl")

    def __init__(self, name, excl=False):
        self.name = name
        self.w = None
        self.rs = []
        self.excl = excl


class V:
    __slots__ = ("ap", "trks")

    def __init__(self, ap, trks):
        self.ap = ap
        self.trks = tuple(trks)

    def __getitem__(self, idx):
        return V(self.ap[idx], self.trks)

    def bc(self, dt):
        return V(self.ap.bitcast(dt), self.trks)


class Ins:
    __slots__ = ("eng", "fn", "deps", "dma", "sig", "ev", "fc", "out")

    def __init__(self, eng, fn, deps, dma, out):
        self.eng = eng
        self.fn = fn
        self.deps = deps
        self.dma = dma
        self.sig = False
        self.ev = None
        self.fc = None
        self.out = out


class Sched:
    def __init__(self, nc, ctx):
        self.nc = nc
        self.ctx = ctx
        self.instrs = []
        self.per_eng = {e: [] for e in ENGS}
        self.uid = 0

    def sbuf(self, name, shape, dtype, ctx=None):
        self.uid += 1
        t = (ctx or self.ctx).enter_context(
            self.nc.sbuf_tensor("%s_%d" % (name, self.uid), list(shape), dtype))
        return V(t[tuple(slice(None) for _ in shape)], [Trk(name)])

    def psum(self, name, shape, dtype=F32):
        t = self.ctx.enter_context(self.nc.psum_tensor(name, list(shape), dtype))
        return V(t[tuple(slice(None) for _ in shape)], [Trk(name, excl=True)])

    def add(self, eng, fn, reads=(), writes=(), dma=False, out=False):
        me = len(self.instrs)
        deps = set()
        for r in reads:
            for t in r.trks:
                if t.excl:
                    if t.w is not None:
                        deps.add(t.w)
                    deps.update(t.rs)
                    t.w = me
                    t.rs = []
                else:
                    if t.w is not None:
                        deps.add(t.w)
                    t.rs.append(me)
        for w in writes:
            for t in w.trks:
                if t.w is not None:
                    deps.add(t.w)
                deps.update(t.rs)
                t.w = me
                t.rs = []
        deps.discard(me)
        self.instrs.append(Ins(eng, fn, deps, dma, out))
        self.per_eng[eng].append(me)
        return me

    def barrier(self):
        last = []
        for e in ENGS:
            lst = self.per_eng[e]
            nd = 0
            seen_c = False
            for idx in reversed(lst):
                ins = self.instrs[idx]
                if ins.fn is None:
                    continue
                if ins.dma:
                    if nd < DMA_K:
                        last.append(idx)
                        nd += 1
                elif not seen_c:
                    last.append(idx)
                    seen_c = True
                if nd >= DMA_K and seen_c:
                    break
        for e in ENGS:
            me = len(self.instrs)
            self.instrs.append(Ins(e, None, set(last), False, False))
            self.per_eng[e].append(me)

    def emit(self):
        nc = self.nc
        instrs = self.instrs
        for ins in instrs:
            for d in ins.deps:
                dd = instrs[d]
                if dd.dma:
                    continue
                if dd.eng == "pe" and ins.eng == "pe" and not ins.dma and ins.fn is not None:
                    continue
                dd.sig = True
        cnt = {e: 0 for e in ENGS}
        dcnt = {e: 0 for e in ENGS}
        for ins in instrs:
            if ins.fn is None:
                continue
            if ins.dma:
                j = dcnt[ins.eng]
                dcnt[ins.eng] += 1
                ins.ev = (("d", ins.eng, j % DMA_K), 16 * (j // DMA_K + 1))
                if j >= DMA_K:
                    ins.fc = (("d", ins.eng, j % DMA_K), 16 * (j // DMA_K))
            elif ins.sig:
                n = cnt[ins.eng]
                cnt[ins.eng] += 1
                ins.ev = (("c", ins.eng, n // SEM_EPOCH), n % SEM_EPOCH + 1)
        sems = {}
        for ins in instrs:
            if ins.ev is not None and ins.ev[0] not in sems:
                k = ins.ev[0]
                sems[k] = self.ctx.enter_context(nc.semaphore("s_%s_%s_%d" % k))
        out_events = [ins.ev for ins in instrs if ins.dma and ins.out]
        self.stats = {e: len(self.per_eng[e]) for e in ENGS}

        def run_engine(ename, eng):
            known = {}
            for idx in self.per_eng[ename]:
                ins = instrs[idx]
                best = {}
                for d in ins.deps:
                    dd = instrs[d]
                    if dd.ev is None:
                        continue
                    if (not dd.dma) and dd.eng == "pe" and ename == "pe" and not ins.dma \
                            and ins.fn is not None:
                        continue
                    k, v = dd.ev
                    if known.get(k, 0) < v and best.get(k, 0) < v:
                        best[k] = v
                if ins.fc is not None:
                    k, v = ins.fc
                    if known.get(k, 0) < v and best.get(k, 0) < v:
                        best[k] = v
                for k, v in best.items():
                    eng.wait_ge(sems[k], v)
                    known[k] = v
                if ins.fn is None:
                    continue
                bi = ins.fn(eng)
                if ins.ev is not None:
                    bi.then_inc(sems[ins.ev[0]], 16 if ins.dma else 1)
            if ename == "sp":
                best = {}
                for (k, v) in out_events:
                    if best.get(k, 0) < v:
                        best[k] = v
                for k, v in best.items():
                    if known.get(k, 0) < v:
                        eng.wait_ge(sems[k], v)

        with nc.Block() as block:
            @block.tensor
            def _(e):
                run_engine("pe", e)

            @block.scalar
            def _(e):
                run_engine("act", e)

            @block.vector
            def _(e):
                run_engine("dve", e)

            @block.gpsimd
            def _(e):
                run_engine("pool", e)

            @block.sync
            def _(e):
                run_engine("sp", e)


class KB:
    def __init__(self, nc, ctx, NSEQ, LAYERS, mix=("ab", "c")):
        self.nc = nc
        self.S = Sched(nc, ctx)
        self.NSEQ = NSEQ
        self.LAYERS = LAYERS
        self.mix = mix
        self.evi = 0
        self.psoi = 0
        self.psrc = {}
        self.psi = 0
        self.rot = {}

    def ev(self):
        self.evi += 1
        return "act" if self.evi % 2 else "dve"

    def ps(self):
        p = self.PS[self.psi % 8]
        self.psi += 1
        return p

    PSR_BANKS = {"A0": (0,), "A1": (1,), "B0": (2,), "B1": (3,), "SA": (4, 5), "SB": (6, 7), "P": (0, 1, 2, 3)}

    def psr(self, key):
        banks = self.PSR_BANKS[key]
        n = self.psrc.get(key, 0)
        self.psrc[key] = n + 1
        return self.PS[banks[n % len(banks)]]

    def ps6(self):
        p = self.PS[self.psi % 6]
        self.psi += 1
        return p

    def ACT(self, out, in_, func, bias=0.0, scale=1.0):
        reads = [in_]
        b, s = bias, scale
        if isinstance(bias, V):
            reads.append(bias)
            b = bias.ap
        if isinstance(scale, V):
            reads.append(scale)
            s = scale.ap
        self.S.add("act", lambda e: e.activation(out=out.ap, in_=in_.ap, func=func, bias=b, scale=s),
                   reads=reads, writes=[out])

    def CP(self, eng, out, in_):
        if eng == "act":
            self.S.add("act", lambda e: e.copy(out=out.ap, in_=in_.ap), reads=[in_], writes=[out])
        else:
            self.S.add(eng, lambda e: e.tensor_copy(out=out.ap, in_=in_.ap), reads=[in_], writes=[out])

    def TT(self, eng, out, a, b, op):
        self.S.add(eng, lambda e: e.tensor_tensor(out=out.ap, in0=a.ap, in1=b.ap, op=op),
                   reads=[a, b], writes=[out])

    def TS(self, eng, out, a, s1, op0, s2=None, op1=None):
        reads = [a]
        x1, x2 = s1, s2
        if isinstance(s1, V):
            reads.append(s1)
            x1 = s1.ap
        if isinstance(s2, V):
            reads.append(s2)
            x2 = s2.ap
        if op1 is None:
            self.S.add(eng, lambda e: e.tensor_scalar(out=out.ap, in0=a.ap, scalar1=x1, scalar2=None, op0=op0),
                       reads=reads, writes=[out])
        else:
            self.S.add(eng, lambda e: e.tensor_scalar(out=out.ap, in0=a.ap, scalar1=x1, scalar2=x2,
                                                      op0=op0, op1=op1), reads=reads, writes=[out])

    def STT(self, eng, out, a, s, b, op0, op1):
        reads = [a, b]
        x = s
        if isinstance(s, V):
            reads.append(s)
            x = s.ap
        self.S.add(eng, lambda e: e.scalar_tensor_tensor(out=out.ap, in0=a.ap, scalar=x, in1=b.ap,
                                                         op0=op0, op1=op1), reads=reads, writes=[out])

    def RED(self, eng, out, in_, op):
        self.S.add(eng, lambda e: e.tensor_reduce(out=out.ap, in_=in_.ap, axis=AX.X, op=op),
                   reads=[in_], writes=[out])

    def RCP(self, out, in_):
        self.S.add("dve", lambda e: e.reciprocal(out=out.ap, in_=in_.ap), reads=[in_], writes=[out])

    def MM(self, ps, lhsT, rhs, start=True, stop=True):
        self.S.add("pe", lambda e: e.matmul(ps.ap, lhsT=lhsT.ap, rhs=rhs.ap, start=start, stop=stop),
                   reads=[lhsT, rhs], writes=[ps])

    def TR(self, ps, in_, ident):
        self.S.add("pe", lambda e: e.transpose(out=ps.ap, in_=in_.ap, identity=ident.ap),
                   reads=[in_, ident], writes=[ps])

    def DMA(self, q, out, in_, is_out=False):
        if is_out:
            self.S.add(q, lambda e: e.dma_start(out=out, in_=in_.ap), reads=[in_], dma=True, out=True)
        else:
            self.S.add(q, lambda e: e.dma_start(out=out.ap, in_=in_), writes=[out], dma=True)

    def dbg(self, name, v, dtype=F32):
        if not getattr(self, "debug", False):
            return
        shp = list(v.ap.shape)
        d = self.nc.dram_tensor(name, shp, dtype, kind="ExternalOutput").ap()
        self.DMA("sp", d, v, is_out=True)

    def lockstep(self, gens):
        gens = list(gens)
        while gens:
            alive = []
            for g in gens:
                try:
                    next(g)
                    alive.append(g)
                except StopIteration:
                    pass
            gens = alive

    def R(self, key, n, mk):
        if key not in self.rot:
            self.rot[key] = [[mk(i) for i in range(n)], 0]
        lst = self.rot[key]
        v = lst[0][lst[1] % n]
        lst[1] += 1
        return v

    def build(self, dr):
        S = self.S
        nc = self.nc
        self.dr = dr
        sb = S.sbuf
        self.PS = [S.psum("ps%d" % i, [128, 512], F32) for i in range(8)]
        self.h = [[sb("h", [128, 512], F32) for q in range(4)] for c in range(8)]
        self.cst = sb("cst", [128, C_END], F32)
        self.gcol = sb("gcol", [128, 128], F32)
        self.convw = sb("convw", [128, 96], F32)
        self.rows_ab = sb("rows_ab", [128, 2 * RA], F32)
        self.bo = sb("bo", [128, 16], F32)
        self.identb = sb("identb", [128, 128], BF16)
        self.onesb = sb("onesb", [128, 128], BF16)
        self.cos = sb("cos", [128, 16, 32], F32)
        self.sin = sb("sin", [128, 16, 32], F32)
        self.lvlm = sb("lvlm", [128, 1792], BF16)
        self.DMA("pool", self.lvlm, dr["lvlmask"])
        self.DMA("sp", self.cst, dr["consts"])
        self.DMA("sp", self.gcol, dr["gcols"])
        self.DMA("sp", self.convw, dr["convw"])
        self.DMA("sp", self.rows_ab, dr["rows_ab"])
        self.DMA("sp", self.bo, dr["bo_cols"])
        self.CP("dve", self.identb, self.cst[:, C_ID:C_ID + 128])
        self.CP("dve", self.onesb, self.cst[:, C_ONE:C_ONE + 128])
        self.negpi = sb("negpi", [128, 1], F32)
        S.add("pool", lambda e: e.memset(self.negpi.ap, -float(np.pi)), writes=[self.negpi])
        self.identf = self.cst[:, C_ID:C_ID + 128]
        self.onesf = self.cst[:, C_ONE:C_ONE + 128]

        for b in range(self.NSEQ):
            for c in range(8):
                for q in range(4):
                    self.DMA("sp", self.h[c][q], dr["xT"][b, c * 128:(c + 1) * 128, q * 512:(q + 1) * 512])
            if self.LAYERS > 1 and "c" in self.mix:
                self.rope_tables(b)
            for l in range(self.LAYERS):
                self.layer(b, l)
            for c in range(8):
                for q in range(4):
                    self.DMA("sp", dr["outT"][b, c * 128:(c + 1) * 128, q * 512:(q + 1) * 512],
                             self.h[c][q], is_out=True)
        S.emit()

    def alloc_hn(self, ctx, nq):
        self.hn = [[self.S.sbuf("hn", [128, 512], BF16, ctx) for q in range(nq)] for c in range(8)]

    def hq(self, c, q):
        row = self.hn[c]
        return row[q % len(row)]

    def gc(self, l, j, c):
        k = (l * 4 + j) * 8 + c
        return self.gcol[:, k:k + 1]

    def rstd_quarter(self, srcs, ctx):
        S = self.S
        ps = self.ps()
        for c in range(8):
            sq = self.R("sq", 2, lambda i: S.sbuf("sq", [128, 512], BF16, ctx))
            if c % 2 == 0:
                self.ACT(sq, srcs[c], AF.Square)
            else:
                self.TT("dve", sq, srcs[c], srcs[c], ALU.mult)
            self.MM(ps, self.onesb, sq, start=(c == 0), stop=(c == 7))
        rstd = self.R("rstd", 2, lambda i: S.sbuf("rstd", [128, 512], F32, ctx))
        self.ACT(rstd, ps, AF.Ln, bias=EPS, scale=1.0 / D_MODEL)
        self.ACT(rstd, rstd, AF.Exp, scale=-0.5)
        return rstd

    def norm_to_hn(self, l, j, ctx, quarters=range(4)):
        for q in quarters:
            rstd = self.rstd_quarter([self.h[c][q] for c in range(8)], ctx)
            for c in range(8):
                self.STT("dve", self.hq(c, q), self.h[c][q], self.gc(l, j, c), rstd, ALU.mult, ALU.mult)

    def add_normed(self, l, j, q, srcs, ctx):
        rstd = self.rstd_quarter(srcs, ctx)
        for c in range(8):
            if c % 4 == 3:
                self.TT("pool", srcs[c], srcs[c], rstd, ALU.mult)
                self.TS("pool", srcs[c], srcs[c], self.gc(l, j, c), ALU.mult)
                self.TT("pool", self.h[c][q], self.h[c][q], srcs[c], ALU.add)
            else:
                self.TT("dve", srcs[c], srcs[c], rstd, ALU.mult)
                self.STT("dve", self.h[c][q], srcs[c], self.gc(l, j, c), self.h[c][q], ALU.mult, ALU.add)

    def layer(self, b, l):
        S = self.S
        with ExitStack() as ctx:
            self.rot = {}
            kind0 = "ab" if l % 2 == 0 else "c"
            if kind0 == "c" and kind0 in self.mix:
                self.alloc_hn(ctx, 4)
                self.norm_to_hn(l, 0, ctx)
            self.mixT = [[S.sbuf("mixT", [128, 512], BF16, ctx) for q in range(4)] for c in range(8)]
            kind = "ab" if l % 2 == 0 else "c"
            if kind in self.mix:
                if kind == "ab":
                    with ExitStack() as c2:
                        self.alloc_hn(c2, 1)
                        self.mixer_ab(b, l, c2)
                        S.barrier()
                    self.rot = {}
                else:
                    self.mixer_c(b, l, ctx)
                self.out_proj(b, l, ctx)
            S.barrier()
        with ExitStack() as ctx:
            self.rot = {}
            self.ffn(b, l, ctx)
            S.barrier()
        with ExitStack() as ctx:
            self.rot = {}
            self.ple(b, l, ctx)
            S.barrier()

    def out_proj(self, b, l, ctx):
        S = self.S
        if l == 1:
            for c in range(8):
                self.dbg("d_mixT%d" % c, self.mixT[c][0], BF16)
        kind = "ab" if l % 2 == 0 else "c"
        wsrc = self.dr["w_out_ab"][l // 2] if kind == "ab" else self.dr["w_o_c"][l // 2]
        wv = wsrc.rearrange("(kc p) n -> p kc n", p=128)
        w = [S.sbuf("wout", [128, 8, 256], BF16, ctx) for i in range(4)]
        for i in range(4):
            self.DMA("pool", w[i], wv[:, :, i * 256:(i + 1) * 256])
        for q in range(4):
            srcs = []
            for m in range(8):
                ps = self.ps()
                for kc in range(8):
                    self.MM(ps, w[m // 2][:, kc, (m % 2) * 128:(m % 2) * 128 + 128], self.mixT[kc][q],
                            start=(kc == 0), stop=(kc == 7))
                t = self.R("mo", 8, lambda i: S.sbuf("mo", [128, 512], F32, ctx))
                if kind == "c":
                    k = (l // 2) * 8 + m
                    self.ACT(t, ps, AF.Identity, bias=self.bo[:, k:k + 1])
                else:
                    self.CP(self.ev(), t, ps)
                srcs.append(t)
            self.add_normed(l, 1, q, srcs, ctx)

    def ffn(self, b, l, ctx):
        S = self.S
        wup = self.dr["w_up"][l].rearrange("(kc p) n -> p kc n", p=128)
        wdn = self.dr["w_down"][l].rearrange("(kc p) n -> p kc n", p=128)
        act = [[S.sbuf("act", [128, 512], BF16, ctx) for n in range(2)] for k in range(16)]
        fft = [[S.sbuf("fft", [128, 512], F32, ctx) for n in range(2)] for m in range(8)]
        wslots = [S.sbuf("wffn", [128, 4096], BF16, ctx) for i in range(3)]
        self.alloc_hn(ctx, 2)
        wi = [0]

        def wslot():
            v = wslots[wi[0] % 3]
            wi[0] += 1
            return v

        for half in range(2):
            qs = [2 * half, 2 * half + 1]
            self.norm_to_hn(l, 2, ctx, quarters=qs)
            for fh in range(2):
                for g in range(4):
                    ws = wslot()
                    wu = V(ws.ap.rearrange("p (kc n) -> p kc n", kc=8), ws.trks)
                    c0 = fh * 2048 + g * 512
                    self.DMA("pool", wu, wup[:, :, c0:c0 + 512])
                    for m in range(4):
                        for n in range(2):
                            ps = self.ps()
                            for kc in range(8):
                                self.MM(ps, wu[:, kc, m * 128:(m + 1) * 128], self.hq(kc, qs[n]),
                                        start=(kc == 0), stop=(kc == 7))
                            r = self.R("relu", 2, lambda i: S.sbuf("relu", [128, 512], F32, ctx))
                            self.ACT(r, ps, AF.Relu)
                            self.TT("dve", act[g * 4 + m][n], r, r, ALU.mult)
                for g in range(4):
                    ws = wslot()
                    wd = V(ws.ap.rearrange("p (kc n) -> p kc n", kc=16), ws.trks)
                    self.DMA("pool", wd, wdn[:, fh * 16:(fh + 1) * 16, g * 256:(g + 1) * 256])
                    pss = [[self.ps() for n in range(2)] for m in range(2)]
                    for kc in range(16):
                        for m in range(2):
                            for n in range(2):
                                self.MM(pss[m][n], wd[:, kc, m * 128:(m + 1) * 128], act[kc][n],
                                        start=(kc == 0), stop=(kc == 15))
                    for m in range(2):
                        for n in range(2):
                            dst = fft[g * 2 + m][n]
                            if fh == 0:
                                self.CP(self.ev(), dst, pss[m][n])
                            else:
                                self.TT("dve", dst, dst, pss[m][n], ALU.add)
            for n in range(2):
                self.add_normed(l, 3, qs[n], [fft[m][n] for m in range(8)], ctx)

    def ple(self, b, l, ctx):
        S = self.S
        self.alloc_hn(ctx, 1)
        wg = S.sbuf("wg", [128, 8, 1024], BF16, ctx)
        wp = S.sbuf("wp", [128, 2, 1024], BF16, ctx)
        pT = [S.sbuf("pT", [128, 2, 512], BF16, ctx) for q in range(4)]
        wgv = self.dr["w_ple_gate"][l].rearrange("(kc p) n -> p kc n", p=128)
        for i in range(2):
            self.DMA("pool", wg[:, :, i * 512:(i + 1) * 512], wgv[:, :, i * 512:(i + 1) * 512])
        self.DMA("pool", wp, self.dr["w_ple"][l].rearrange("(kc p) n -> p kc n", p=128))
        pv = self.dr["pT"][l, b].rearrange("(kc p) t -> p kc t", p=128)
        for q in range(4):
            self.DMA("pool", pT[q], pv[:, :, q * 512:(q + 1) * 512])
        for q in range(4):
            for c in range(8):
                self.CP("act" if c % 2 else "pool", self.hq(c, q), self.h[c][q])
            for m in range(8):
                psg = self.ps()
                for kc in range(8):
                    self.MM(psg, wg[:, kc, m * 128:(m + 1) * 128], self.hq(kc, q), start=(kc == 0), stop=(kc == 7))
                psp = self.ps()
                for kc in range(2):
                    self.MM(psp, wp[:, kc, m * 128:(m + 1) * 128], pT[q][:, kc, :], start=(kc == 0), stop=(kc == 1))
                sg = self.R("sg", 3, lambda i: S.sbuf("sg", [128, 512], F32, ctx))
                self.ACT(sg, psg, AF.Sigmoid)
                self.TT("dve", sg, sg, psp, ALU.mult)
                self.TT("pool", self.h[m][q], self.h[m][q], sg, ALU.add)

    def rope_tables(self, b):
        S = self.S
        with ExitStack() as ctx:
            posi = S.sbuf("posi", [128, 16], I32, ctx)
            posf = S.sbuf("posf", [128, 16], F32, ctx)
            y0 = S.sbuf("y0", [128, 16, 32], F32, ctx)
            yy = S.sbuf("yy", [128, 16, 32], F32, ctx)
            ki = S.sbuf("ki", [128, 16, 32], I32, ctx)
            kf = S.sbuf("kf", [128, 16, 32], F32, ctx)
            mk = S.sbuf("mk", [128, 16, 32], F32, ctx)
            self.DMA("sp", posi, self.dr["pos"][b])
            self.CP("dve", posf, posi)
            inv = self.cst[:, C_INV:C_INV + 32]
            invb = V(inv.ap.unsqueeze(1).to_broadcast([128, 16, 32]), inv.trks)
            posb = V(posf.ap.unsqueeze(2).to_broadcast([128, 16, 32]), posf.trks)
            self.TT("dve", y0, invb, posb, ALU.mult)
            for dst, off in ((self.sin, 0.5), (self.cos, 0.75)):
                self.TS("dve", yy, y0, 1.0 / (2.0 * np.pi), ALU.mult, off, ALU.add)
                self.CP("dve", ki, yy)
                self.CP("dve", kf, ki)
                self.TT("dve", yy, yy, kf, ALU.subtract)
                self.TS("dve", mk, yy, 0.0, ALU.is_lt)
                self.TT("dve", yy, yy, mk, ALU.add)
                self.ACT(dst, yy, AF.Sin, bias=self.negpi, scale=2.0 * np.pi)
            S.barrier()

    def mixer_c(self, b, l, ctx0):
        S = self.S
        o = l // 2
        rows = S.sbuf("rowsc", [128, RC], F32, ctx0)
        self.DMA("sp", rows, self.dr["rows_c"][o])
        wv = self.dr["w_qkv_c"][o].rearrange("(kc p) n -> p kc n", p=128)
        for g in range(4):
          with ExitStack() as ctx:
            self.rot = dict((k, v) for k, v in self.rot.items() if k in ("sq", "rstd", "ntmp", "mo"))
            wq = S.sbuf("wq", [128, 8, 384], BF16, ctx)
            self.DMA("pool", wq[:, :, 0:256], wv[:, :, g * 256:(g + 1) * 256])
            self.DMA("pool", wq[:, :, 256:320], wv[:, :, 1024 + g * 64:1024 + (g + 1) * 64])
            self.DMA("pool", wq[:, :, 320:384], wv[:, :, 1280 + g * 64:1280 + (g + 1) * 64])
            brow = S.sbuf("brow", [128, 384], F32, ctx)
            self.CP("pool", brow[:, 0:256], rows[:, g * 256:(g + 1) * 256])
            self.CP("pool", brow[:, 256:320], rows[:, 1024 + g * 64:1024 + (g + 1) * 64])
            self.CP("pool", brow[:, 320:384], rows[:, 1280 + g * 64:1280 + (g + 1) * 64])
            qTall = S.sbuf("qTall", [128, 2, 2048], BF16, ctx)
            kTall = S.sbuf("kTall", [128, 2048], BF16, ctx)
            qTj = [V(qTall.ap[:, :, j * 128:(j + 1) * 128], [Trk("qTj")]) for j in range(16)]
            kTj = [V(kTall.ap[:, j * 128:(j + 1) * 128], [Trk("kTj")]) for j in range(16)]
            vtok = [S.sbuf("vtok", [128, 64], BF16, ctx) for j in range(16)]
            def tile1(j):
                    qkv = self.R("qkv", 2, lambda i: S.sbuf("qkv", [128, 384], F32, ctx))
                    ps = self.ps6()
                    for kc in range(8):
                        self.MM(ps[:, 0:384], self.hq(kc, j // 4)[:, (j % 4) * 128:(j % 4) * 128 + 128],
                                wq[:, kc, :], start=(kc == 0), stop=(kc == 7))
                    self.TT("dve", qkv, ps[:, 0:384], brow, ALU.add)
                    yield
                    qr = self.R("qr", 2, lambda i: S.sbuf("qr", [128, 256], BF16, ctx))
                    kd = self.R("kd", 2, lambda i: S.sbuf("kd", [128, 2, 64], BF16, ctx))
                    for (src, dst, nh) in ((qkv[:, 0:256], qr, 4), (qkv[:, 256:320], kd[:, 0, :], 1)):
                        sv = V(src.ap.rearrange("p (h t d) -> p h t d", t=2, d=32), src.trks)
                        dv = V(dst.ap.rearrange("p (h t d) -> p h t d", t=2, d=32), dst.trks)
                        cb = V(self.cos.ap[:, j, :].unsqueeze(1).to_broadcast([128, nh, 32]), self.cos.trks)
                        sb_ = V(self.sin.ap[:, j, :].unsqueeze(1).to_broadcast([128, nh, 32]), self.sin.trks)
                        t1 = self.R("rt1", 2, lambda i: S.sbuf("rt1", [128, 4, 32], F32, ctx))
                        t2 = self.R("rt2", 2, lambda i: S.sbuf("rt2", [128, 4, 32], F32, ctx))
                        t3 = self.R("rt3", 2, lambda i: S.sbuf("rt3", [128, 4, 32], F32, ctx))
                        t4 = self.R("rt4", 2, lambda i: S.sbuf("rt4", [128, 4, 32], F32, ctx))
                        self.TT("dve", t1[:, 0:nh, :], sv[:, :, 0, :], cb, ALU.mult)
                        yield
                        self.TT("dve", t2[:, 0:nh, :], sv[:, :, 1, :], sb_, ALU.mult)
                        yield
                        self.TT("dve", dv[:, :, 0, :], t1[:, 0:nh, :], t2[:, 0:nh, :], ALU.subtract)
                        yield
                        self.TT("pool", t3[:, 0:nh, :], sv[:, :, 1, :], cb, ALU.mult)
                        yield
                        self.TT("pool", t4[:, 0:nh, :], sv[:, :, 0, :], sb_, ALU.mult)
                        yield
                        self.TT("pool", dv[:, :, 1, :], t3[:, 0:nh, :], t4[:, 0:nh, :], ALU.add)
                        yield
                    self.CP("act", kd[:, 1, :], kd[:, 0, :])
                    yield
                    self.CP("act", vtok[j], qkv[:, 320:384])
                    yield
                    pst = self.ps6().bc(BF16)
                    for i in range(2):
                        self.TR(pst[:, i * 128:(i + 1) * 128], qr[:, i * 128:(i + 1) * 128], self.identb)
                    kdf = V(kd.ap.rearrange("p a d -> p (a d)"), kd.trks)
                    self.TR(pst[:, 256:384], kdf, self.identb)
                    src = V(pst.ap[:, 0:256].rearrange("p (i t) -> p i t", t=128), pst.trks)
                    self.CP("act", qTj[j], src)
                    self.CP("dve", kTj[j], pst[:, 256:384])
                    yield
            for j0 in range(0, 16, 2):
                self.lockstep([tile1(j0), tile1(j0 + 1)])
            def blk2(j):
                    nk = 1 if j == 0 else 2
                    SS = nk * 128
                    mask = self.cst[:, C_SWA + 256 - SS:C_SWA + 256]
                    pso = self.PS[6 + (self.psoi % 2)]
                    self.psoi += 1
                    recg = self.R("recg", 2, lambda i: S.sbuf("recg", [128, 4], F32, ctx))
                    ktr = [kTj[j - 1], kTj[j]] if j > 0 else [kTj[0]]
                    sc4 = self.R("sc4", 2, lambda i_: S.sbuf("sc4", [128, 4, 256], F32, ctx))
                    for hl in range(4):
                        i = hl // 2
                        hh = hl % 2
                        ps = self.ps6()
                        for kb in range(nk):
                            self.MM(ps[:, kb * 128:(kb + 1) * 128],
                                    qTj[j][hh * 64:(hh + 1) * 64, i, :], ktr[kb][hh * 64:(hh + 1) * 64, :])
                        self.STT("dve", sc4[:, hl, 0:SS], ps[:, 0:SS], 0.125, mask, ALU.mult, ALU.add)
                        yield
                    sk4 = rows[:, 1536 + 4 * g:1536 + 4 * g + 4]
                    mx4 = self.R("mx4", 2, lambda i_: S.sbuf("mx4", [128, 4], F32, ctx))
                    self.RED("dve", mx4, sc4[:, :, 0:SS], ALU.max)
                    yield
                    self.TT("dve", mx4, mx4, sk4, ALU.max)
                    yield
                    nmx4 = self.R("nmx4", 2, lambda i_: S.sbuf("nmx4", [128, 4], F32, ctx))
                    self.TS("dve", nmx4, mx4, -1.0, ALU.mult)
                    yield
                    pr4 = self.R("pr4", 2, lambda i_: S.sbuf("pr4", [128, 4, 256], BF16, ctx))
                    for hl in range(4):
                        self.ACT(pr4[:, hl, 0:SS], sc4[:, hl, 0:SS], AF.Exp, bias=nmx4[:, hl:hl + 1])
                        yield
                    rs4 = self.R("rs4", 2, lambda i_: S.sbuf("rs4", [128, 4], F32, ctx))
                    self.RED("dve", rs4, pr4[:, :, 0:SS], ALU.add)
                    yield
                    es4 = self.R("es4", 2, lambda i_: S.sbuf("es4", [128, 4], F32, ctx))
                    self.TT("dve", es4, sk4, mx4, ALU.subtract)
                    yield
                    self.ACT(es4, es4, AF.Exp)
                    yield
                    self.TT("dve", rs4, rs4, es4, ALU.add)
                    yield
                    self.RCP(recg, rs4)
                    yield
                    for hl in range(4):
                        pst = self.ps6().bc(BF16)
                        for kb in range(nk):
                            self.TR(pst[:, kb * 128:(kb + 1) * 128], pr4[:, hl, kb * 128:(kb + 1) * 128], self.identb)
                        prT = self.R("prT", 8, lambda i_: S.sbuf("prT", [128, 256], BF16, ctx))
                        self.CP(self.ev(), prT[:, 0:SS], pst[:, 0:SS])
                        yield
                        for kb in range(nk):
                            jk = j - 1 + kb if j > 0 else 0
                            self.MM(pso[:, hl * 64:hl * 64 + 64], prT[:, kb * 128:(kb + 1) * 128],
                                    vtok[jk], start=(kb == 0), stop=(kb == nk - 1))
                    otok = self.R("otok", 2, lambda i: S.sbuf("otok", [128, 256], BF16, ctx))
                    psov = V(pso.ap[:, 0:256].rearrange("p (h d) -> p h d", d=64), pso.trks)
                    recb = V(recg.ap.unsqueeze(2).to_broadcast([128, 4, 64]), recg.trks)
                    ov = V(otok.ap.rearrange("p (h d) -> p h d", d=64), otok.trks)
                    self.TT("dve", ov, psov, recb, ALU.mult)
                    yield
                    pst = self.ps6().bc(BF16)
                    for i in range(2):
                        self.TR(pst[:, i * 128:(i + 1) * 128], otok[:, i * 128:(i + 1) * 128], self.identb)
                    for i in range(2):
                        self.CP(self.ev(), self.mixT[2 * g + i][j // 4][:, (j % 4) * 128:(j % 4) * 128 + 128],
                                pst[:, i * 128:(i + 1) * 128])
                    yield
            for j0 in range(0, 16, 2):
                self.lockstep([blk2(j0), blk2(j0 + 1)])
            S.barrier()

    def mixer_ab(self, b, l, ctx0):
        S = self.S
        e = l // 2
        rab = lambda a, n: self.rows_ab[:, e * RA + a:e * RA + a + n]
        win = self.dr["w_in_ab"][e].rearrange("(kc p) n -> p kc n", p=128)
        sb = lambda name, shape, dt=F32: S.sbuf(name, shape, dt, ctx0)
        identf, onesf, identb = self.identf, self.onesf, self.identb
        triU = self.cst[:, C_TU:C_TU + 128]
        maskL = self.cst[:, C_ML:C_ML + 128]
        negm = self.cst[:, C_NM:C_NM + 128]
        wgate = sb("wgate", [128, 8, 16], BF16)
        self.DMA("pool", wgate[:, :, 0:8], win[:, :, 1536:1544])
        self.DMA("pool", wgate[:, :, 8:16], win[:, :, 3592:3600])
        G = sb("G", [128, 16, 16])
        for j in range(16):
            if j % 4 == 0:
                self.norm_to_hn(l, 0, ctx0, quarters=[j // 4])
            ps = self.ps()
            for kc in range(8):
                self.MM(ps[:, 0:16], self.hq(kc, j // 4)[:, (j % 4) * 128:(j % 4) * 128 + 128], wgate[:, kc, :],
                        start=(kc == 0), stop=(kc == 7))
            self.CP(self.ev(), G[:, j, :], ps[:, 0:16])
        bc4 = lambda v: V(v.ap.unsqueeze(1).to_broadcast([128, 16, 4]), v.trks)
        BETA = sb("BETA", [128, 16, 4])
        self.ACT(BETA, G[:, :, 0:4], AF.Sigmoid)
        X = sb("X", [128, 16, 4])
        T1 = sb("T1", [128, 16, 4])
        T2 = sb("T2", [128, 16, 4])
        SP = sb("SP", [128, 16, 4])

        def softplus(dst, x):
            self.TS("dve", T1, x, -1.0, ALU.mult)
            self.TT("dve", T1, T1, x, ALU.min)
            self.ACT(T1, T1, AF.Exp)
            self.ACT(T1, T1, AF.Ln, bias=1.0)
            self.TS("dve", T2, x, 0.0, ALU.max)
            self.TT("dve", dst, T1, T2, ALU.add)

        GF = sb("GF", [128, 16, 8])
        self.TT("dve", X, G[:, :, 4:8], bc4(rab(0, 4)), ALU.add)
        softplus(SP, X)
        negA = sb("negA", [128, 4])
        self.ACT(negA, rab(4, 4), AF.Exp)
        self.TS("dve", negA, negA, -1.0, ALU.mult)
        self.TT("dve", GF[:, :, 0:4], SP, bc4(negA), ALU.mult)
        ILOG = sb("ILOG", [128, 16, 4])
        self.TT("dve", ILOG, G[:, :, 8:12], bc4(rab(8, 4)), ALU.add)
        self.TT("dve", X, G[:, :, 12:16], bc4(rab(12, 4)), ALU.add)
        self.TS("dve", X, X, -1.0, ALU.mult)
        softplus(SP, X)
        self.TS("dve", GF[:, :, 4:8], SP, -1.0, ALU.mult)
        CUM = sb("CUM", [128, 16, 16])
        for j in range(16):
            ps = self.ps()
            self.MM(ps[:, 0:8], triU, GF[:, j, :])
            self.MM(ps[:, 8:16], onesf, GF[:, j, :])
            self.CP(self.ev(), CUM[:, j, :], ps[:, 0:16])
        gcum, bcum, gtot = CUM[:, :, 0:4], CUM[:, :, 4:8], CUM[:, :, 8:12]
        BG = sb("BG", [128, 16, 4])
        KD = sb("KD", [128, 16, 4])
        EGL = sb("EGL", [128, 16, 4])
        RR = sb("RR", [128, 16, 4])
        self.ACT(BG, gcum, AF.Exp)
        self.TT("dve", BG, BG, BETA, ALU.mult)
        self.TT("dve", KD, gtot, gcum, ALU.subtract)
        self.ACT(KD, KD, AF.Exp)
        self.ACT(EGL, gtot, AF.Exp)
        self.TT("dve", RR, ILOG, bcum, ALU.subtract)
        cols = [i * 128 for i in range(4)]
        vexts = [sb("vext", [128, 129], BF16) for _ in range(4)]
        for v in vexts:
            S.add("pool", lambda e_, v=v: e_.memset(v.ap[:, 128:129], 1.0), writes=[v])
        wh = sb("wh", [128, 8, 1024], BF16)
        Sa = sb("Sa", [128, 128])
        Sab = sb("Sab", [128, 128], BF16)
        Cx = sb("Cx", [128, 129])
        Cb = sb("Cb", [128, 129], BF16)
        mst = sb("mst", [128, 1])
        cwork = sb("cwork", [128, 515])
        ctail = [sb("ctail", [128, 4]) for _ in range(3)]
        qT = sb("qT", [128, 512], BF16)
        kT = sb("kT", [128, 512], BF16)
        vT = sb("vT", [128, 512], BF16)
        qbTs = [sb("qbT", [128, 512], BF16) for _ in range(2)]
        kbT = sb("kbT", [128, 512], BF16)
        acc = sb("acc", [128, 512])
        sl = acc
        t128 = lambda name, dt=F32: self.R(name, 2, lambda i_: S.sbuf(name, [128, 128], dt, ctx0))
        c1 = lambda name, n=1: self.R(name, 2, lambda i_: S.sbuf(name, [128, n], F32, ctx0))

        def finish(key, pf, o, gain, gate, ci, q, tsl):
            tmp = t128(pf + "f_tmp")
            self.ACT(tmp, o, AF.Square)
            yield
            ss = c1(pf + "f_ss")
            self.RED("dve", ss, tmp, ALU.add)
            yield
            self.ACT(ss, ss, AF.Ln, bias=EPS, scale=1.0 / 128.0)
            self.ACT(ss, ss, AF.Exp, scale=-0.5)
            yield
            y = t128(pf + "f_y")
            self.STT("dve", y, o, ss, gain, ALU.mult, ALU.mult)
            yb = t128(pf + "f_yb", BF16)
            self.TT("dve", yb, y, gate, ALU.mult)
            yield
            pY = self.psr(key).bc(BF16)
            self.TR(pY[:, 0:128], yb, identb)
            self.CP(self.ev(), self.mixT[ci][q][:, tsl], pY[:, 0:128])
            yield

        for i in range(4):
            gcols = [i * 128, 512 + i * 128, 1024 + i * 128, 2056 + i * 128, 2568 + i * 128,
                     3080 + i * 128, 3600 + i * 128, 1544 + i * 128]
            for gi, c0 in enumerate(gcols):
                self.DMA("pool", wh[:, :, gi * 128:(gi + 1) * 128], win[:, :, c0:c0 + 128])
            for t_ in (Sa, Sab, Cx, Cb, mst):
                S.add("pool", lambda e_, t_=t_: e_.memset(t_.ap, 0.0), writes=[t_])
            for g in range(3):
                S.add("pool", lambda e_, t_=ctail[g]: e_.memset(t_.ap, 0.0), writes=[ctail[g]])
            def prep(q):
                ps = self.psr("P")
                for c in range(8):
                    sq = self.R("sq", 2, lambda i_: S.sbuf("sq", [128, 512], BF16, ctx0))
                    if c % 2 == 0:
                        self.ACT(sq, self.h[c][q], AF.Square)
                    else:
                        self.TT("dve", sq, self.h[c][q], self.h[c][q], ALU.mult)
                    self.MM(ps, self.onesb, sq, start=(c == 0), stop=(c == 7))
                    yield
                rstd = self.R("rstd", 2, lambda i_: S.sbuf("rstd", [128, 512], F32, ctx0))
                self.ACT(rstd, ps, AF.Ln, bias=EPS, scale=1.0 / D_MODEL)
                self.ACT(rstd, rstd, AF.Exp, scale=-0.5)
                yield
                for c in range(8):
                    self.STT("dve", self.hq(c, q), self.h[c][q], self.gc(l, 0, c), rstd, ALU.mult, ALU.mult)
                    yield
                for gi in range(5):
                    ps = self.psr("P")
                    for kc in range(8):
                        self.MM(ps, wh[:, kc, gi * 128:(gi + 1) * 128], self.hq(kc, q), start=(kc == 0), stop=(kc == 7))
                    if gi < 3:
                        cb_ = cwork
                        self.CP("pool", cb_[:, 0:3], ctail[gi][:, 0:3])
                        yield
                        self.CP(self.ev(), cb_[:, 3:515], ps)
                        yield
                        wc = lambda k: self.convw[:, ((e * 4 + i) * 3 + gi) * 4 + k:((e * 4 + i) * 3 + gi) * 4 + k + 1]
                        self.TS("dve", acc, cb_[:, 0:512], wc(0), ALU.mult)
                        yield
                        for k in range(1, 4):
                            self.STT("dve", acc, cb_[:, k:k + 512], wc(k), acc, ALU.mult, ALU.add)
                            yield
                        self.CP("pool", ctail[gi][:, 0:3], cb_[:, 512:515])
                        yield
                        self.ACT(sl, acc, AF.Silu)
                        yield
                        if gi < 2:
                            sqb = self.R("sq", 2, lambda i_: S.sbuf("sq", [128, 512], BF16, ctx0))
                            self.ACT(sqb, sl, AF.Square)
                            yield
                            p2 = self.psr("P")
                            self.MM(p2, self.onesb, sqb)
                            rn5 = p2
                            self.ACT(rn5, p2, AF.Ln, bias=EPS)
                            yield
                            self.ACT(rn5, rn5, AF.Exp, scale=-0.5)
                            yield
                            if gi == 0:
                                self.STT("dve", qT, sl, 128.0 ** -0.5, rn5, ALU.mult, ALU.mult)
                                yield
                            else:
                                self.TT("dve", kT, sl, rn5, ALU.mult)
                                yield
                        else:
                            self.CP("pool", vT, sl)
                            yield
                    elif gi == 3:
                        self.ACT(qbTs[q % 2], ps, AF.Copy, scale=128.0 ** -0.5)
                        yield
                    else:
                        self.CP("dve", kbT, ps)
                        yield
                yield
            self.lockstep([prep(0)])
            for q in range(4):
                qbT = qbTs[q % 2]
                def mk_tile(jj):
                    j = 4 * q + jj
                    tsl = slice(jj * 128, (jj + 1) * 128)
                    col = lambda T_, off=0: T_[:, j, off + i:off + i + 1]
                    pt = self.ps()
                    for kc in range(8):
                        self.MM(pt, self.hq(kc, q)[:, tsl], wh[:, kc, 512:1024], start=(kc == 0), stop=(kc == 7))
                    h4 = lambda name, dt=F32: self.R(name, 4, lambda i_: S.sbuf(name, [128, 128], dt, ctx0))
                    kbtok = h4("kbtok")
                    self.CP("act", kbtok, pt[:, 0:128])
                    vext = vexts[j % 4]
                    self.CP("dve", vext[:, 0:128], pt[:, 128:256])
                    og = h4("og")
                    self.ACT(og, pt[:, 256:384], AF.Sigmoid)
                    sz = h4("sz")
                    self.ACT(sz, pt[:, 384:512], AF.Silu)
                    beta, gc_, bg, kdc, egl = col(BETA), col(CUM), col(BG), col(KD), col(EGL)
                    rcol, bcc, bl = col(RR), col(CUM, 4), col(CUM, 12)
                    gA = rab(16, 128)
                    gB = rab(144 + i * 128, 128)
                    h4 = lambda name, dt=F32: self.R(name, 4, lambda i_: S.sbuf(name, [128, 128], dt, ctx0))
                    kdec, qdT, ATb, wTn = h4("kdec", BF16), h4("qdT", BF16), h4("ATb", BF16), h4("wTn", BF16)
                    u, P0T, kw0 = h4("u"), h4("P0T", BF16), h4("kw0", BF16)
                    mi = self.R("mi", 4, lambda i_: S.sbuf("mi", [128, 1], F32, ctx0))
                    maxr = self.R("maxr", 4, lambda i_: S.sbuf("maxr", [128, 1], F32, ctx0))
                    kA = "A%d" % (jj % 2)
                    kB = "B%d" % (jj % 2)
                    def bulkA():
                        ptr = self.psr(kA).bc(BF16)
                        self.TR(ptr[:, 0:128], kT[:, tsl], identb)
                        self.TR(ptr[:, 128:256], vT[:, tsl], identb)
                        Rm = self.R("Rm", 2, lambda i_: S.sbuf("Rm", [128, 256], F32, ctx0))
                        self.TS("dve", Rm[:, 0:128], ptr[:, 128:256], beta, ALU.mult)
                        yield
                        self.TS("dve", Rm[:, 128:256], ptr[:, 0:128], bg, ALU.mult)
                        yield
                        self.TS("dve", kdec, ptr[:, 0:128], kdc, ALU.mult)
                        yield
                        dg = t128("dg")
                        self.TS("dve", dg, identf, gc_, ALU.mult)
                        yield
                        pG = self.psr(kA)
                        self.MM(pG[:, 0:128], onesf, dg)
                        D1 = t128("D1")
                        self.STT("dve", D1, pG[:, 0:128], gc_, maskL, ALU.subtract, ALU.mult)
                        yield
                        DT = t128("DT")
                        self.STT("dve", DT, pG[:, 0:128], gc_, triU, ALU.subtract, ALU.mult)
                        yield
                        EgR = pG[:, 0:128]
                        self.ACT(EgR, pG[:, 0:128], AF.Exp)
                        yield
                        self.ACT(D1, D1, AF.Exp, scale=-1.0)
                        yield
                        self.TT("dve", D1, D1, maskL, ALU.mult)
                        yield
                        self.ACT(DT, DT, AF.Exp)
                        yield
                        self.TT("dve", DT, DT, triU, ALU.mult)
                        yield
                        self.TT("dve", qdT, qT[:, tsl], EgR, ALU.mult)
                        yield
                        pK = self.psr(kA)
                        self.MM(pK[:, 0:128], kT[:, tsl], kT[:, tsl])
                        self.MM(pK[:, 128:256], kT[:, tsl], qT[:, tsl])
                        Lf = t128("Lf")
                        self.STT("dve", Lf, pK[:, 0:128], beta, D1, ALU.mult, ALU.mult)
                        yield
                        self.TT("dve", ATb, pK[:, 128:256], DT, ALU.mult)
                        yield
                        pL = self.psr(kA)
                        self.TR(pL[:, 0:128], Lf, identf)
                        LTf = t128("LTf")
                        self.CP("act", LTf, pL[:, 0:128])
                        yield
                        Mf = t128("Mf")
                        MTf = t128("MTf")
                        t128b = lambda name: self.R(name, 2, lambda i_: S.sbuf(name, [128, 128], F32, ctx0))
                        Ck = t128b("Ck")
                        CTk = t128b("CTk")
                        self.TT("pool", Ck, Lf, self.lvlm[:, 0:128], ALU.mult)
                        self.TT("pool", CTk, LTf, self.lvlm[:, 896:1024], ALU.mult)
                        self.TT("pool", Mf, identf, Ck, ALU.subtract)
                        self.TT("pool", MTf, identf, CTk, ALU.subtract)
                        yield
                        for k in range(1, 7):
                            Ck = t128b("Ck")
                            CTk = t128b("CTk")
                            self.TT("pool", Ck, Lf, self.lvlm[:, k * 128:(k + 1) * 128], ALU.mult)
                            self.TT("pool", CTk, LTf, self.lvlm[:, 896 + k * 128:896 + (k + 1) * 128], ALU.mult)
                            pa_ = self.psr(kA)
                            self.MM(pa_[:, 0:128], CTk, Mf)
                            self.MM(pa_[:, 128:256], Ck, MTf)
                            T1f = t128("T1f")
                            T3f = t128("T3f")
                            self.CP("act", T1f, pa_[:, 0:128])
                            self.CP("dve", T3f, pa_[:, 128:256])
                            yield
                            pb_ = self.psr(kA)
                            self.MM(pb_[:, 0:128], MTf, T1f)
                            self.MM(pb_[:, 128:256], Mf, T3f)
                            self.TT("dve", Mf, Mf, pb_[:, 0:128], ALU.subtract)
                            self.TT("dve", MTf, MTf, pb_[:, 128:256], ALU.subtract)
                            yield
                        pX = self.psr(kA)
                        self.MM(pX[:, 0:256], MTf, Rm)
                        wb = t128("wb", BF16)
                        self.CP("act", u, pX[:, 0:128])
                        yield
                        self.CP("dve", wb, pX[:, 128:256])
                        yield
                        pW = self.psr(kA).bc(BF16)
                        self.TR(pW[:, 0:128], wb, identb)
                        self.ACT(wTn, pW[:, 0:128], AF.Copy, scale=-1.0)
                        yield
                    def scanA():
                        pV = self.psr("SA")
                        self.MM(pV[:, 0:128], wTn, Sab)
                        vn = t128("vn", BF16)
                        self.TT("dve", vn, u, pV[:, 0:128], ALU.add)
                        yield
                        pO = self.psr("SA")
                        self.MM(pO[:, 0:128], qdT, Sab, start=True, stop=False)
                        self.MM(pO[:, 0:128], ATb, vn, start=False, stop=True)
                        oA = t128("oA")
                        self.CP("act", oA, pO[:, 0:128])
                        yield
                        pS = self.psr("SA")
                        self.MM(pS[:, 0:128], kdec, vn)
                        self.STT("dve", Sa, Sa, egl, pS[:, 0:128], ALU.mult, ALU.add)
                        yield
                        self.CP("act", Sab, Sa)
                        yield
                        yield from finish("SA", "a", oA, gA, sz, i, q, tsl)
                    def bulkB():
                        dgr = t128("dgr")
                        self.TS("dve", dgr, identf, rcol, ALU.mult)
                        yield
                        pR = self.psr(kB)
                        self.MM(pR[:, 0:128], onesf, dgr)
                        tmpB = t128("tmpB")
                        self.TT("dve", tmpB, pR[:, 0:128], negm, ALU.add)
                        yield
                        self.RED("dve", maxr, pR[:, 0:128], ALU.max)
                        yield
                        mr = c1("mr")
                        self.RED("dve", mr, tmpB, ALU.max)
                        yield
                        nmr = c1("nmr")
                        self.TS("dve", nmr, mr, -1.0, ALU.mult)
                        yield
                        Eb = t128("Eb")
                        self.ACT(Eb, tmpB, AF.Exp, bias=nmr)
                        yield
                        pQ = self.psr(kB)
                        self.MM(pQ[:, 0:128], qbT[:, tsl], kbT[:, tsl])
                        P0 = t128("P0", BF16)
                        self.TT("dve", P0, pQ[:, 0:128], Eb, ALU.mult)
                        yield
                        pP0 = self.psr(kB).bc(BF16)
                        self.TR(pP0[:, 0:128], P0, identb)
                        self.CP("act", P0T, pP0[:, 0:128])
                        yield
                        ew = c1("ew")
                        self.TT("dve", ew, rcol, maxr, ALU.subtract)
                        yield
                        self.ACT(ew, ew, AF.Exp)
                        yield
                        self.TS("dve", kw0, kbtok, ew, ALU.mult)
                        yield
                        self.TT("dve", mi, mr, bcc, ALU.add)
                        yield
                    def scanB():
                        a_ = c1("a_")
                        self.TT("dve", a_, bcc, mst, ALU.add)
                        yield
                        mt = c1("mt")
                        self.TT("dve", mt, a_, mi, ALU.max)
                        yield
                        e3 = c1("e3", 3)
                        self.TT("dve", e3[:, 0:1], a_, mt, ALU.subtract)
                        yield
                        self.TT("dve", e3[:, 1:2], mi, mt, ALU.subtract)
                        yield
                        self.TS("dve", e3[:, 2:3], mt, -1.0, ALU.mult)
                        yield
                        self.ACT(e3, e3, AF.Exp)
                        yield
                        p1 = self.psr("SB")
                        self.MM(p1[:, 0:129], qbT[:, tsl], Cb)
                        p2 = self.psr("SB")
                        self.MM(p2[:, 0:129], P0T, vext)
                        nd = self.R("nd", 2, lambda i_: S.sbuf("nd", [128, 129], F32, ctx0))
                        self.TS("dve", nd, p1[:, 0:129], e3[:, 0:1], ALU.mult)
                        yield
                        self.STT("dve", nd, p2[:, 0:129], e3[:, 1:2], nd, ALU.mult, ALU.add)
                        yield
                        td = c1("td")
                        self.TS("dve", td, nd[:, 128:129], -1.0, ALU.mult)
                        yield
                        self.TT("dve", td, td, nd[:, 128:129], ALU.max)
                        yield
                        self.TT("dve", td, td, e3[:, 2:3], ALU.max)
                        yield
                        self.RCP(td, td)
                        yield
                        hB = t128("hB")
                        self.TS("dve", hB, nd[:, 0:128], td, ALU.mult)
                        yield
                        mm = c1("mm")
                        self.TT("dve", mm, mst, maxr, ALU.max)
                        yield
                        e2 = c1("e2", 2)
                        self.TT("dve", e2[:, 0:1], mst, mm, ALU.subtract)
                        yield
                        self.TT("dve", e2[:, 1:2], maxr, mm, ALU.subtract)
                        yield
                        self.ACT(e2, e2, AF.Exp)
                        yield
                        p3 = self.psr("SB")
                        self.MM(p3[:, 0:129], kw0, vext)
                        self.TS("dve", Cx, Cx, e2[:, 0:1], ALU.mult)
                        yield
                        self.STT("dve", Cx, p3[:, 0:129], e2[:, 1:2], Cx, ALU.mult, ALU.add)
                        yield
                        self.CP("act", Cb, Cx)
                        yield
                        self.TT("dve", mst, bl, mm, ALU.add)
                        yield
                        yield from finish("SB", "b", hB, gB, og, 4 + i, q, tsl)
                    return bulkA, scanA, bulkB, scanB
                TL = [mk_tile(jj) for jj in range(4)]
                def seq(*gs):
                    for g_ in gs:
                        yield from g_
                def par(*gs):
                    gs = list(gs)
                    while gs:
                        al = []
                        for g_ in gs:
                            try:
                                next(g_)
                                al.append(g_)
                            except StopIteration:
                                pass
                        gs = al
                        yield
                self.lockstep([TL[0][0](), TL[0][2](), TL[1][0](), TL[1][2]()])
                self.lockstep([TL[2][0](), TL[2][2](), TL[3][0](), TL[3][2](),
                               seq(par(TL[0][1](), TL[0][3]()), par(TL[1][1](), TL[1][3]()))])
                P2 = [seq(par(TL[2][1](), TL[2][3]()), par(TL[3][1](), TL[3][3]()))]
                if q < 3:
                    P2.append(prep(q + 1))
                self.lockstep(P2)


def make_consts():
    c = np.zeros((128, C_END), np.float32)
    i = np.arange(128)
    P, Fr = i[:, None], i[None, :]
    same = (P // 64) == (Fr // 64)
    c[:, C_ID:C_ID + 128] = np.eye(128)
    s = np.arange(256)[None, :]
    c[:, C_SWA:C_SWA + 256] = np.where((s > P) & (s <= P + 128), 0.0, NEG)
    half = 32
    inv = (10000.0 ** (-np.arange(half, dtype=np.float32) / half)).astype(np.float32)
    c[:, C_INV:C_INV + 32] = inv[None, :]
    c[:, C_ONE:C_ONE + 128] = 1.0
    c[:, C_TU:C_TU + 128] = (Fr >= P)
    c[:, C_ML:C_ML + 128] = (Fr < P)
    c[:, C_NM:C_NM + 128] = np.where(Fr <= P, 0.0, NEG)
    return c


DEBUG = False


def make_lvlmask():
    m = np.zeros((128, 1792), np.float32)
    i = np.arange(128)
    c, e = i[:, None], i[None, :]
    for k in range(7):
        sz = 1 << k
        mk = ((c // (2 * sz)) == (e // (2 * sz))) & ((c % (2 * sz)) >= sz) & ((e % (2 * sz)) < sz)
        m[:, k * 128:(k + 1) * 128] = mk
        m[:, 896 + k * 128:896 + (k + 1) * 128] = mk.T
    return m


def build_nc(NSEQ, LAYERS, mix=("ab", "c")):
    nc = bass.Bass("TRN2", target_bir_lowering=False)

    def di(name, shape, dt=F32):
        return nc.dram_tensor(name, list(shape), dt, kind="ExternalInput").ap()

    dr = {
        "xT": di("xT", [NSEQ, 1024, 2048]),
        "pT": di("pT", [4, NSEQ, 256, 2048]),
        "pos": di("pos", [NSEQ, 128, 16], I32),
        "consts": di("consts", [128, C_END]),
        "lvlmask": di("lvlmask", [128, 1792]),
        "gcols": di("gcols", [128, 128]),
        "convw": di("convw", [128, 96]),
        "rows_ab": di("rows_ab", [128, 2 * RA]),
        "rows_c": di("rows_c", [2, 128, RC]),
        "bo_cols": di("bo_cols", [128, 16]),
        "w_in_ab": di("w_in_ab", [2, 1024, IN_COLS]),
        "w_out_ab": di("w_out_ab", [2, 1024, 1024]),
        "w_qkv_c": di("w_qkv_c", [2, 1024, 1536]),
        "w_o_c": di("w_o_c", [2, 1024, 1024]),
        "w_up": di("w_up", [4, 1024, 4096]),
        "w_down": di("w_down", [4, 4096, 1024]),
        "w_ple": di("w_ple", [4, 256, 1024]),
        "w_ple_gate": di("w_ple_gate", [4, 1024, 1024]),
    }
    dr["outT"] = nc.dram_tensor("outT", [NSEQ, 1024, 2048], F32, kind="ExternalOutput").ap()
    with ExitStack() as ctx:
        kb = KB(nc, ctx, NSEQ, LAYERS, mix)
        kb.debug = DEBUG
        kb.build(dr)
    return nc


def host_params(inp):
    f = lambda a: np.ascontiguousarray(np.asarray(a, dtype=np.float32))
    ng = f(inp["norm_gains"])
    gcols = np.ascontiguousarray(ng.reshape(4, 4, 8, 128).transpose(3, 0, 1, 2).reshape(128, 128))
    cw = f(inp["conv_a"])
    convw = np.ascontiguousarray(cw.reshape(2, 4, 3, 4, 128).transpose(4, 0, 3, 2, 1).reshape(128, 96))
    rows = np.zeros((2, RA), np.float32)
    for e in range(2):
        rows[e, 0:4] = f(inp["dt_bias"])[e]
        rows[e, 4:8] = f(inp["a_log"])[e]
        rows[e, 8:12] = f(inp["i_bias_b"])[e]
        rows[e, 12:16] = f(inp["f_bias_b"])[e]
        rows[e, 16:144] = f(inp["norm_a"])[e]
        rows[e, 144:656] = f(inp["norm_b"])[e].reshape(512)
    rows_ab = np.ascontiguousarray(np.broadcast_to(rows.reshape(1, 2 * RA), (128, 2 * RA)))
    rc = np.zeros((2, RC), np.float32)
    for o in range(2):
        rc[o, 0:1536] = f(inp["b_qkv_c"])[o]
        rc[o, 1536:1552] = f(inp["sinks_c"])[o]
    rows_c = np.ascontiguousarray(np.broadcast_to(rc[:, None, :], (2, 128, RC)))
    bo = f(inp["b_o_c"])
    bo_cols = np.ascontiguousarray(bo.reshape(2, 8, 128).transpose(2, 0, 1).reshape(128, 16))
    return {
        "consts": make_consts(), "lvlmask": make_lvlmask(), "gcols": gcols, "convw": convw, "rows_ab": rows_ab, "rows_c": rows_c,
        "bo_cols": bo_cols,
        "w_in_ab": f(inp["w_in_ab"]), "w_out_ab": f(inp["w_out_ab"]), "w_qkv_c": f(inp["w_qkv_c"]),
        "w_o_c": f(inp["w_o_c"]), "w_up": f(inp["w_up"]), "w_down": f(inp["w_down"]),
        "w_ple": f(inp["w_ple"]), "w_ple_gate": f(inp["w_ple_gate"]),
    }


def run(inp, n_cores=8, LAYERS=DEPTH, mix=("ab", "c"), trace=False):
    x = np.asarray(inp["x"], dtype=np.float32)
    p = np.asarray(inp["p"], dtype=np.float32)
    pos = np.asarray(inp["positions"], dtype=np.int32)
    B = x.shape[0]
    NSEQ = B // n_cores
    shared = host_params(inp)
    nc = build_nc(NSEQ, LAYERS, mix)
    in_maps = []
    for c in range(n_cores):
        sl = slice(c * NSEQ, (c + 1) * NSEQ)
        m = dict(shared)
        m["xT"] = np.ascontiguousarray(x[sl].transpose(0, 2, 1))
        m["pT"] = np.ascontiguousarray(p[:, sl].transpose(0, 1, 3, 2))
        m["pos"] = np.ascontiguousarray(pos[sl].reshape(-1, 16, 128).transpose(0, 2, 1))
        in_maps.append(m)
    res = run_bass_kernel_spmd(nc, in_maps, core_ids=list(range(n_cores)), trace=trace)
    out = np.concatenate([np.asarray(r["outT"]).transpose(0, 2, 1) for r in res.results], axis=0)
    return np.ascontiguousarray(out.astype(np.float32)), res


def kernel(**inputs):
    out, _ = run(inputs)
    return out
```

```python
import numpy as np
import concourse.bass as bass
import concourse.mybir as mybir
from concourse.bass_utils import run_bass_kernel_spmd
from contextlib import ExitStack

F32 = mybir.dt.float32
BF16 = mybir.dt.bfloat16
I32 = mybir.dt.int32
AF = mybir.ActivationFunctionType
ALU = mybir.AluOpType
AX = mybir.AxisListType

ENGS = ("pe", "act", "dve", "pool", "sp")
SEM_EPOCH = 4000
DMA_K = 8
INLINE_WAIT = True

D_MODEL = 1024
SEQ = 2048
DEPTH = 4
D_FF = 4096
PLE = 256
EPS = 1e-6
NEG = -30000.0
A_COLS = 2056
IN_COLS = 4112
C_ID, C_SWA, C_INV, C_ONE, C_TU, C_ML, C_NM, C_END = (0, 128, 384, 416, 544, 672, 800, 928)
RA = 656
RC = 1552


class Trk:
    __slots__ = ("name", "w", "rs", "excl")

    def __init__(self, name, excl=False):
        self.name = name
        self.w = None
        self.rs = []
        self.excl = excl


class V:
    __slots__ = ("ap", "trks")

    def __init__(self, ap, trks):
        self.ap = ap
        self.trks = tuple(trks)

    def __getitem__(self, idx):
        return V(self.ap[idx], self.trks)

    def bc(self, dt):
        return V(self.ap.bitcast(dt), self.trks)


class Ins:
    __slots__ = ("eng", "fn", "deps", "dma", "sig", "ev", "fc", "out")

    def __init__(self, eng, fn, deps, dma, out):
        self.eng = eng
        self.fn = fn
        self.deps = deps
        self.dma = dma
        self.sig = False
        self.ev = None
        self.fc = None
        self.out = out


class Sched:
    def __init__(self, nc, ctx):
        self.nc = nc
        self.ctx = ctx
        self.instrs = []
        self.per_eng = {e: [] for e in ENGS}
        self.uid = 0

    def sbuf(self, name, shape, dtype, ctx=None):
        self.uid += 1
        t = (ctx or self.ctx).enter_context(
            self.nc.sbuf_tensor("%s_%d" % (name, self.uid), list(shape), dtype))
        return V(t[tuple(slice(None) for _ in shape)], [Trk(name)])

    def psum(self, name, shape, dtype=F32):
        t = self.ctx.enter_context(self.nc.psum_tensor(name, list(shape), dtype))
        return V(t[tuple(slice(None) for _ in shape)], [Trk(name, excl=True)])

    def add(self, eng, fn, reads=(), writes=(), dma=False, out=False):
        me = len(self.instrs)
        deps = set()
        for r in reads:
            for t in r.trks:
                if t.excl:
                    if t.w is not None:
                        deps.add(t.w)
                    deps.update(t.rs)
                    t.w = me
                    t.rs = []
                else:
                    if t.w is not None:
                        deps.add(t.w)
                    t.rs.append(me)
        for w in writes:
            for t in w.trks:
                if t.w is not None:
                    deps.add(t.w)
                deps.update(t.rs)
                t.w = me
                t.rs = []
        deps.discard(me)
        self.instrs.append(Ins(eng, fn, deps, dma, out))
        self.per_eng[eng].append(me)
        return me

    def barrier(self):
        last = []
        for e in ENGS:
            lst = self.per_eng[e]
            nd = 0
            seen_c = False
            for idx in reversed(lst):
                ins = self.instrs[idx]
                if ins.fn is None:
                    continue
                if ins.dma:
                    if nd < DMA_K:
                        last.append(idx)
                        nd += 1
                elif not seen_c:
                    last.append(idx)
                    seen_c = True
                if nd >= DMA_K and seen_c:
                    break
        for e in ENGS:
            me = len(self.instrs)
            self.instrs.append(Ins(e, None, set(last), False, False))
            self.per_eng[e].append(me)

    def emit(self):
        nc = self.nc
        instrs = self.instrs
        for ins in instrs:
            for d in ins.deps:
                dd = instrs[d]
                if dd.dma:
                    continue
                if dd.eng == "pe" and ins.eng == "pe" and not ins.dma and ins.fn is not None:
                    continue
                dd.sig = True
        cnt = {e: 0 for e in ENGS}
        dcnt = {e: 0 for e in ENGS}
        for ins in instrs:
            if ins.fn is None:
                continue
            if ins.dma:
                j = dcnt[ins.eng]
                dcnt[ins.eng] += 1
                ins.ev = (("d", ins.eng, j % DMA_K), 16 * (j // DMA_K + 1))
                if j >= DMA_K:
                    ins.fc = (("d", ins.eng, j % DMA_K), 16 * (j // DMA_K))
            elif ins.sig:
                n = cnt[ins.eng]
                cnt[ins.eng] += 1
                ins.ev = (("c", ins.eng, n // SEM_EPOCH), n % SEM_EPOCH + 1)
        sems = {}
        for ins in instrs:
            if ins.ev is not None and ins.ev[0] not in sems:
                k = ins.ev[0]
                sems[k] = self.ctx.enter_context(nc.semaphore("s_%s_%s_%d" % k))
        out_events = [ins.ev for ins in instrs if ins.dma and ins.out]
        self.stats = {e: len(self.per_eng[e]) for e in ENGS}

        def run_engine(ename, eng):
            known = {}
            for idx in self.per_eng[ename]:
                ins = instrs[idx]
                best = {}
                for d in ins.deps:
                    dd = instrs[d]
                    if dd.ev is None:
                        continue
                    if (not dd.dma) and dd.eng == "pe" and ename == "pe" and not ins.dma \
                            and ins.fn is not None:
                        continue
                    k, v = dd.ev
                    if known.get(k, 0) < v and best.get(k, 0) < v:
                        best[k] = v
                if ins.fc is not None:
                    k, v = ins.fc
                    if known.get(k, 0) < v and best.get(k, 0) < v:
                        best[k] = v
                items = list(best.items())
                inline = None
                if INLINE_WAIT and ins.fn is not None and (not ins.dma) and ename in ("act", "dve", "pool") and items:
                    inline = items.pop()
                for k, v in items:
                    eng.wait_ge(sems[k], v)
                    known[k] = v
                if ins.fn is None:
                    continue
                bi = ins.fn(eng)
                if inline is not None:
                    bi._wait_ge(sems[inline[0]], inline[1])
                    known[inline[0]] = inline[1]
                if ins.ev is not None:
                    bi.then_inc(sems[ins.ev[0]], 16 if ins.dma else 1)
            if ename == "sp":
                best = {}
                for (k, v) in out_events:
                    if best.get(k, 0) < v:
                        best[k] = v
                for k, v in best.items():
                    if known.get(k, 0) < v:
                        eng.wait_ge(sems[k], v)

        with nc.Block() as block:
            @block.tensor
            def _(e):
                run_engine("pe", e)

            @block.scalar
            def _(e):
                run_engine("act", e)

            @block.vector
            def _(e):
                run_engine("dve", e)

            @block.gpsimd
            def _(e):
                run_engine("pool", e)

            @block.sync
            def _(e):
                run_engine("sp", e)


class KB:
    def __init__(self, nc, ctx, NSEQ, LAYERS, mix=("ab", "c")):
        self.nc = nc
        self.S = Sched(nc, ctx)
        self.NSEQ = NSEQ
        self.LAYERS = LAYERS
        self.mix = mix
        self.evi = 0
        self.psoi = 0
        self.psrc = {}
        self.psi = 0
        self.rot = {}

    def ev(self):
        self.evi += 1
        return "act" if self.evi % 2 else "dve"

    def ps(self):
        p = self.PS[self.psi % 8]
        self.psi += 1
        return p

    PSR_BANKS = {"A0": (0,), "A1": (1,), "B0": (2,), "B1": (3,), "SA": (4, 5), "SB": (6, 7), "P": (0, 1, 2, 3)}

    def psr(self, key):
        banks = self.PSR_BANKS[key]
        n = self.psrc.get(key, 0)
        self.psrc[key] = n + 1
        return self.PS[banks[n % len(banks)]]

    def ps6(self):
        p = self.PS[self.psi % 6]
        self.psi += 1
        return p

    def ACT(self, out, in_, func, bias=0.0, scale=1.0):
        reads = [in_]
        b, s = bias, scale
        if isinstance(bias, V):
            reads.append(bias)
            b = bias.ap
        if isinstance(scale, V):
            reads.append(scale)
            s = scale.ap
        self.S.add("act", lambda e: e.activation(out=out.ap, in_=in_.ap, func=func, bias=b, scale=s),
                   reads=reads, writes=[out])

    def CP(self, eng, out, in_):
        if eng == "act":
            self.S.add("act", lambda e: e.copy(out=out.ap, in_=in_.ap), reads=[in_], writes=[out])
        else:
            self.S.add(eng, lambda e: e.tensor_copy(out=out.ap, in_=in_.ap), reads=[in_], writes=[out])

    def TT(self, eng, out, a, b, op):
        self.S.add(eng, lambda e: e.tensor_tensor(out=out.ap, in0=a.ap, in1=b.ap, op=op),
                   reads=[a, b], writes=[out])

    def TS(self, eng, out, a, s1, op0, s2=None, op1=None):
        reads = [a]
        x1, x2 = s1, s2
        if isinstance(s1, V):
            reads.append(s1)
            x1 = s1.ap
        if isinstance(s2, V):
            reads.append(s2)
            x2 = s2.ap
        if op1 is None:
            self.S.add(eng, lambda e: e.tensor_scalar(out=out.ap, in0=a.ap, scalar1=x1, scalar2=None, op0=op0),
                       reads=reads, writes=[out])
        else:
            self.S.add(eng, lambda e: e.tensor_scalar(out=out.ap, in0=a.ap, scalar1=x1, scalar2=x2,
                                                      op0=op0, op1=op1), reads=reads, writes=[out])

    def STT(self, eng, out, a, s, b, op0, op1):
        reads = [a, b]
        x = s
        if isinstance(s, V):
            reads.append(s)
            x = s.ap
        self.S.add(eng, lambda e: e.scalar_tensor_tensor(out=out.ap, in0=a.ap, scalar=x, in1=b.ap,
                                                         op0=op0, op1=op1), reads=reads, writes=[out])

    def RED(self, eng, out, in_, op):
        self.S.add(eng, lambda e: e.tensor_reduce(out=out.ap, in_=in_.ap, axis=AX.X, op=op),
                   reads=[in_], writes=[out])

    def RCP(self, out, in_):
        self.S.add("dve", lambda e: e.reciprocal(out=out.ap, in_=in_.ap), reads=[in_], writes=[out])

    def MM(self, ps, lhsT, rhs, start=True, stop=True):
        self.S.add("pe", lambda e: e.matmul(ps.ap, lhsT=lhsT.ap, rhs=rhs.ap, start=start, stop=stop),
                   reads=[lhsT, rhs], writes=[ps])

    def TR(self, ps, in_, ident):
        self.S.add("pe", lambda e: e.transpose(out=ps.ap, in_=in_.ap, identity=ident.ap),
                   reads=[in_, ident], writes=[ps])

    def DMA(self, q, out, in_, is_out=False):
        if is_out:
            self.S.add(q, lambda e: e.dma_start(out=out, in_=in_.ap), reads=[in_], dma=True, out=True)
        else:
            self.S.add(q, lambda e: e.dma_start(out=out.ap, in_=in_), writes=[out], dma=True)

    def dbg(self, name, v, dtype=F32):
        if not getattr(self, "debug", False):
            return
        shp = list(v.ap.shape)
        d = self.nc.dram_tensor(name, shp, dtype, kind="ExternalOutput").ap()
        self.DMA("sp", d, v, is_out=True)

    def lockstep(self, gens):
        gens = list(gens)
        while gens:
            alive = []
            for g in gens:
                try:
                    next(g)
                    alive.append(g)
                except StopIteration:
                    pass
            gens = alive

    def R(self, key, n, mk):
        if key not in self.rot:
            self.rot[key] = [[mk(i) for i in range(n)], 0]
        lst = self.rot[key]
        v = lst[0][lst[1] % n]
        lst[1] += 1
        return v

    def build(self, dr):
        S = self.S
        nc = self.nc
        self.dr = dr
        sb = S.sbuf
        self.PS = [S.psum("ps%d" % i, [128, 512], F32) for i in range(8)]
        self.h = [[sb("h", [128, 512], F32) for q in range(4)] for c in range(8)]
        self.cst = sb("cst", [128, C_END], F32)
        self.gcol = sb("gcol", [128, 128], F32)
        self.convw = sb("convw", [128, 96], F32)
        self.rows_ab = sb("rows_ab", [128, 2 * RA], F32)
        self.bo = sb("bo", [128, 16], F32)
        self.identb = sb("identb", [128, 128], BF16)
        self.onesb = sb("onesb", [128, 128], BF16)
        self.cos = sb("cos", [128, 16, 32], F32)
        self.sin = sb("sin", [128, 16, 32], F32)
        self.lvlm = sb("lvlm", [128, 1792], BF16)
        self.DMA("pool", self.lvlm, dr["lvlmask"])
        self.DMA("sp", self.cst, dr["consts"])
        self.DMA("sp", self.gcol, dr["gcols"])
        self.DMA("sp", self.convw, dr["convw"])
        self.DMA("sp", self.rows_ab, dr["rows_ab"])
        self.DMA("sp", self.bo, dr["bo_cols"])
        self.CP("dve", self.identb, self.cst[:, C_ID:C_ID + 128])
        self.CP("dve", self.onesb, self.cst[:, C_ONE:C_ONE + 128])
        self.negpi = sb("negpi", [128, 1], F32)
        S.add("pool", lambda e: e.memset(self.negpi.ap, -float(np.pi)), writes=[self.negpi])
        self.identf = self.cst[:, C_ID:C_ID + 128]
        self.onesf = self.cst[:, C_ONE:C_ONE + 128]

        for b in range(self.NSEQ):
            for c in range(8):
                for q in range(4):
                    self.DMA("sp", self.h[c][q], dr["xT"][b, c * 128:(c + 1) * 128, q * 512:(q + 1) * 512])
            if self.LAYERS > 1 and "c" in self.mix:
                self.rope_tables(b)
            for l in range(self.LAYERS):
                self.layer(b, l)
            for c in range(8):
                for q in range(4):
                    self.DMA("sp", dr["outT"][b, c * 128:(c + 1) * 128, q * 512:(q + 1) * 512],
                             self.h[c][q], is_out=True)
        S.emit()

    def alloc_hn(self, ctx, nq):
        self.hn = [[self.S.sbuf("hn", [128, 512], BF16, ctx) for q in range(nq)] for c in range(8)]

    def hq(self, c, q):
        row = self.hn[c]
        return row[q % len(row)]

    def gc(self, l, j, c):
        k = (l * 4 + j) * 8 + c
        return self.gcol[:, k:k + 1]

    def rstd_quarter(self, srcs, ctx):
        S = self.S
        ps = self.ps()
        for c in range(8):
            sq = self.R("sq", 2, lambda i: S.sbuf("sq", [128, 512], BF16, ctx))
            if c % 2 == 0:
                self.ACT(sq, srcs[c], AF.Square)
            else:
                self.TT("dve", sq, srcs[c], srcs[c], ALU.mult)
            self.MM(ps, self.onesb, sq, start=(c == 0), stop=(c == 7))
        rstd = self.R("rstd", 2, lambda i: S.sbuf("rstd", [128, 512], F32, ctx))
        self.ACT(rstd, ps, AF.Ln, bias=EPS, scale=1.0 / D_MODEL)
        self.ACT(rstd, rstd, AF.Exp, scale=-0.5)
        return rstd

    def norm_to_hn(self, l, j, ctx, quarters=range(4)):
        for q in quarters:
            rstd = self.rstd_quarter([self.h[c][q] for c in range(8)], ctx)
            for c in range(8):
                self.STT("dve", self.hq(c, q), self.h[c][q], self.gc(l, j, c), rstd, ALU.mult, ALU.mult)

    def add_normed(self, l, j, q, srcs, ctx):
        rstd = self.rstd_quarter(srcs, ctx)
        for c in range(8):
            if c % 4 == 3:
                self.TT("pool", srcs[c], srcs[c], rstd, ALU.mult)
                self.TS("pool", srcs[c], srcs[c], self.gc(l, j, c), ALU.mult)
                self.TT("pool", self.h[c][q], self.h[c][q], srcs[c], ALU.add)
            else:
                self.TT("dve", srcs[c], srcs[c], rstd, ALU.mult)
                self.STT("dve", self.h[c][q], srcs[c], self.gc(l, j, c), self.h[c][q], ALU.mult, ALU.add)

    def layer(self, b, l):
        S = self.S
        with ExitStack() as ctx:
            self.rot = {}
            kind0 = "ab" if l % 2 == 0 else "c"
            if kind0 == "c" and kind0 in self.mix:
                self.alloc_hn(ctx, 4)
                self.norm_to_hn(l, 0, ctx)
            self.mixT = [[S.sbuf("mixT", [128, 512], BF16, ctx) for q in range(4)] for c in range(8)]
            kind = "ab" if l % 2 == 0 else "c"
            if kind in self.mix:
                if kind == "ab":
                    with ExitStack() as c2:
                        self.alloc_hn(c2, 1)
                        self.mixer_ab(b, l, c2)
                        S.barrier()
                    self.rot = {}
                else:
                    self.mixer_c(b, l, ctx)
                self.out_proj(b, l, ctx)
            S.barrier()
        with ExitStack() as ctx:
            self.rot = {}
            self.ffn(b, l, ctx)
            S.barrier()
        with ExitStack() as ctx:
            self.rot = {}
            self.ple(b, l, ctx)
            S.barrier()

    def out_proj(self, b, l, ctx):
        S = self.S
        if l == 1:
            for c in range(8):
                self.dbg("d_mixT%d" % c, self.mixT[c][0], BF16)
        kind = "ab" if l % 2 == 0 else "c"
        wsrc = self.dr["w_out_ab"][l // 2] if kind == "ab" else self.dr["w_o_c"][l // 2]
        wv = wsrc.rearrange("(kc p) n -> p kc n", p=128)
        w = [S.sbuf("wout", [128, 8, 256], BF16, ctx) for i in range(4)]
        for i in range(4):
            self.DMA("pool", w[i], wv[:, :, i * 256:(i + 1) * 256])
        for q in range(4):
            srcs = []
            for m in range(8):
                ps = self.ps()
                for kc in range(8):
                    self.MM(ps, w[m // 2][:, kc, (m % 2) * 128:(m % 2) * 128 + 128], self.mixT[kc][q],
                            start=(kc == 0), stop=(kc == 7))
                t = self.R("mo", 8, lambda i: S.sbuf("mo", [128, 512], F32, ctx))
                if kind == "c":
                    k = (l // 2) * 8 + m
                    self.ACT(t, ps, AF.Identity, bias=self.bo[:, k:k + 1])
                else:
                    self.CP(self.ev(), t, ps)
                srcs.append(t)
            self.add_normed(l, 1, q, srcs, ctx)

    def ffn(self, b, l, ctx):
        S = self.S
        wup = self.dr["w_up"][l].rearrange("(kc p) n -> p kc n", p=128)
        wdn = self.dr["w_down"][l].rearrange("(kc p) n -> p kc n", p=128)
        act = [[S.sbuf("act", [128, 512], BF16, ctx) for n in range(2)] for k in range(16)]
        fft = [[S.sbuf("fft", [128, 512], F32, ctx) for n in range(2)] for m in range(8)]
        wslots = [S.sbuf("wffn", [128, 4096], BF16, ctx) for i in range(3)]
        self.alloc_hn(ctx, 2)
        wi = [0]

        def wslot():
            v = wslots[wi[0] % 3]
            wi[0] += 1
            return v

        for half in range(2):
            qs = [2 * half, 2 * half + 1]
            self.norm_to_hn(l, 2, ctx, quarters=qs)
            for fh in range(2):
                for g in range(4):
                    ws = wslot()
                    wu = V(ws.ap.rearrange("p (kc n) -> p kc n", kc=8), ws.trks)
                    c0 = fh * 2048 + g * 512
                    self.DMA("pool", wu, wup[:, :, c0:c0 + 512])
                    for m in range(4):
                        for n in range(2):
                            ps = self.ps()
                            for kc in range(8):
                                self.MM(ps, wu[:, kc, m * 128:(m + 1) * 128], self.hq(kc, qs[n]),
                                        start=(kc == 0), stop=(kc == 7))
                            r = self.R("relu", 2, lambda i: S.sbuf("relu", [128, 512], F32, ctx))
                            self.ACT(r, ps, AF.Relu)
                            self.TT("dve", act[g * 4 + m][n], r, r, ALU.mult)
                for g in range(4):
                    ws = wslot()
                    wd = V(ws.ap.rearrange("p (kc n) -> p kc n", kc=16), ws.trks)
                    self.DMA("pool", wd, wdn[:, fh * 16:(fh + 1) * 16, g * 256:(g + 1) * 256])
                    pss = [[self.ps() for n in range(2)] for m in range(2)]
                    for kc in range(16):
                        for m in range(2):
                            for n in range(2):
                                self.MM(pss[m][n], wd[:, kc, m * 128:(m + 1) * 128], act[kc][n],
                                        start=(kc == 0), stop=(kc == 15))
                    for m in range(2):
                        for n in range(2):
                            dst = fft[g * 2 + m][n]
                            if fh == 0:
                                self.CP(self.ev(), dst, pss[m][n])
                            else:
                                self.TT("dve", dst, dst, pss[m][n], ALU.add)
            for n in range(2):
                self.add_normed(l, 3, qs[n], [fft[m][n] for m in range(8)], ctx)

    def ple(self, b, l, ctx):
        S = self.S
        self.alloc_hn(ctx, 1)
        wg = S.sbuf("wg", [128, 8, 1024], BF16, ctx)
        wp = S.sbuf("wp", [128, 2, 1024], BF16, ctx)
        pT = [S.sbuf("pT", [128, 2, 512], BF16, ctx) for q in range(4)]
        wgv = self.dr["w_ple_gate"][l].rearrange("(kc p) n -> p kc n", p=128)
        for i in range(2):
            self.DMA("pool", wg[:, :, i * 512:(i + 1) * 512], wgv[:, :, i * 512:(i + 1) * 512])
        self.DMA("pool", wp, self.dr["w_ple"][l].rearrange("(kc p) n -> p kc n", p=128))
        pv = self.dr["pT"][l, b].rearrange("(kc p) t -> p kc t", p=128)
        for q in range(4):
            self.DMA("pool", pT[q], pv[:, :, q * 512:(q + 1) * 512])
        for q in range(4):
            for c in range(8):
                self.CP("act" if c % 2 else "pool", self.hq(c, q), self.h[c][q])
            for m in range(8):
                psg = self.ps()
                for kc in range(8):
                    self.MM(psg, wg[:, kc, m * 128:(m + 1) * 128], self.hq(kc, q), start=(kc == 0), stop=(kc == 7))
                psp = self.ps()
                for kc in range(2):
                    self.MM(psp, wp[:, kc, m * 128:(m + 1) * 128], pT[q][:, kc, :], start=(kc == 0), stop=(kc == 1))
                sg = self.R("sg", 3, lambda i: S.sbuf("sg", [128, 512], F32, ctx))
                self.ACT(sg, psg, AF.Sigmoid)
                self.TT("dve", sg, sg, psp, ALU.mult)
                self.TT("pool", self.h[m][q], self.h[m][q], sg, ALU.add)

    def rope_tables(self, b):
        S = self.S
        with ExitStack() as ctx:
            posi = S.sbuf("posi", [128, 16], I32, ctx)
            posf = S.sbuf("posf", [128, 16], F32, ctx)
            y0 = S.sbuf("y0", [128, 16, 32], F32, ctx)
            yy = S.sbuf("yy", [128, 16, 32], F32, ctx)
            ki = S.sbuf("ki", [128, 16, 32], I32, ctx)
            kf = S.sbuf("kf", [128, 16, 32], F32, ctx)
            mk = S.sbuf("mk", [128, 16, 32], F32, ctx)
            self.DMA("sp", posi, self.dr["pos"][b])
            self.CP("dve", posf, posi)
            inv = self.cst[:, C_INV:C_INV + 32]
            invb = V(inv.ap.unsqueeze(1).to_broadcast([128, 16, 32]), inv.trks)
            posb = V(posf.ap.unsqueeze(2).to_broadcast([128, 16, 32]), posf.trks)
            self.TT("dve", y0, invb, posb, ALU.mult)
            for dst, off in ((self.sin, 0.5), (self.cos, 0.75)):
                self.TS("dve", yy, y0, 1.0 / (2.0 * np.pi), ALU.mult, off, ALU.add)
                self.CP("dve", ki, yy)
                self.CP("dve", kf, ki)
                self.TT("dve", yy, yy, kf, ALU.subtract)
                self.TS("dve", mk, yy, 0.0, ALU.is_lt)
                self.TT("dve", yy, yy, mk, ALU.add)
                self.ACT(dst, yy, AF.Sin, bias=self.negpi, scale=2.0 * np.pi)
            S.barrier()

    def mixer_c(self, b, l, ctx0):
        S = self.S
        o = l // 2
        rows = S.sbuf("rowsc", [128, RC], F32, ctx0)
        self.DMA("sp", rows, self.dr["rows_c"][o])
        wv = self.dr["w_qkv_c"][o].rearrange("(kc p) n -> p kc n", p=128)
        for g in range(4):
          with ExitStack() as ctx:
            self.rot = dict((k, v) for k, v in self.rot.items() if k in ("sq", "rstd", "ntmp", "mo"))
            wq = S.sbuf("wq", [128, 8, 384], BF16, ctx)
            self.DMA("pool", wq[:, :, 0:256], wv[:, :, g * 256:(g + 1) * 256])
            self.DMA("pool", wq[:, :, 256:320], wv[:, :, 1024 + g * 64:1024 + (g + 1) * 64])
            self.DMA("pool", wq[:, :, 320:384], wv[:, :, 1280 + g * 64:1280 + (g + 1) * 64])
            brow = S.sbuf("brow", [128, 384], F32, ctx)
            self.CP("pool", brow[:, 0:256], rows[:, g * 256:(g + 1) * 256])
            self.CP("pool", brow[:, 256:320], rows[:, 1024 + g * 64:1024 + (g + 1) * 64])
            self.CP("pool", brow[:, 320:384], rows[:, 1280 + g * 64:1280 + (g + 1) * 64])
            qTall = S.sbuf("qTall", [128, 2, 2048], BF16, ctx)
            kTall = S.sbuf("kTall", [128, 2048], BF16, ctx)
            qTj = [V(qTall.ap[:, :, j * 128:(j + 1) * 128], [Trk("qTj")]) for j in range(16)]
            kTj = [V(kTall.ap[:, j * 128:(j + 1) * 128], [Trk("kTj")]) for j in range(16)]
            vtok = [S.sbuf("vtok", [128, 64], BF16, ctx) for j in range(16)]
            def tile1(j):
                    qkv = self.R("qkv", 2, lambda i: S.sbuf("qkv", [128, 384], F32, ctx))
                    ps = self.ps6()
                    for kc in range(8):
                        self.MM(ps[:, 0:384], self.hq(kc, j // 4)[:, (j % 4) * 128:(j % 4) * 128 + 128],
                                wq[:, kc, :], start=(kc == 0), stop=(kc == 7))
                    self.TT("dve", qkv, ps[:, 0:384], brow, ALU.add)
                    yield
                    qr = self.R("qr", 2, lambda i: S.sbuf("qr", [128, 256], BF16, ctx))
                    kd = self.R("kd", 2, lambda i: S.sbuf("kd", [128, 2, 64], BF16, ctx))
                    for (src, dst, nh) in ((qkv[:, 0:256], qr, 4), (qkv[:, 256:320], kd[:, 0, :], 1)):
                        sv = V(src.ap.rearrange("p (h t d) -> p h t d", t=2, d=32), src.trks)
                        dv = V(dst.ap.rearrange("p (h t d) -> p h t d", t=2, d=32), dst.trks)
                        cb = V(self.cos.ap[:, j, :].unsqueeze(1).to_broadcast([128, nh, 32]), self.cos.trks)
                        sb_ = V(self.sin.ap[:, j, :].unsqueeze(1).to_broadcast([128, nh, 32]), self.sin.trks)
                        t1 = self.R("rt1", 2, lambda i: S.sbuf("rt1", [128, 4, 32], F32, ctx))
                        t2 = self.R("rt2", 2, lambda i: S.sbuf("rt2", [128, 4, 32], F32, ctx))
                        t3 = self.R("rt3", 2, lambda i: S.sbuf("rt3", [128, 4, 32], F32, ctx))
                        t4 = self.R("rt4", 2, lambda i: S.sbuf("rt4", [128, 4, 32], F32, ctx))
                        self.TT("dve", t1[:, 0:nh, :], sv[:, :, 0, :], cb, ALU.mult)
                        yield
                        self.TT("dve", t2[:, 0:nh, :], sv[:, :, 1, :], sb_, ALU.mult)
                        yield
                        self.TT("dve", dv[:, :, 0, :], t1[:, 0:nh, :], t2[:, 0:nh, :], ALU.subtract)
                        yield
                        self.TT("pool", t3[:, 0:nh, :], sv[:, :, 1, :], cb, ALU.mult)
                        yield
                        self.TT("pool", t4[:, 0:nh, :], sv[:, :, 0, :], sb_, ALU.mult)
                        yield
                        self.TT("pool", dv[:, :, 1, :], t3[:, 0:nh, :], t4[:, 0:nh, :], ALU.add)
                        yield
                    self.CP("act", kd[:, 1, :], kd[:, 0, :])
                    yield
                    self.CP("act", vtok[j], qkv[:, 320:384])
                    yield
                    pst = self.ps6().bc(BF16)
                    for i in range(2):
                        self.TR(pst[:, i * 128:(i + 1) * 128], qr[:, i * 128:(i + 1) * 128], self.identb)
                    kdf = V(kd.ap.rearrange("p a d -> p (a d)"), kd.trks)
                    self.TR(pst[:, 256:384], kdf, self.identb)
                    src = V(pst.ap[:, 0:256].rearrange("p (i t) -> p i t", t=128), pst.trks)
                    self.CP("act", qTj[j], src)
                    self.CP("dve", kTj[j], pst[:, 256:384])
                    yield
            for j0 in range(0, 16, 2):
                self.lockstep([tile1(j0), tile1(j0 + 1)])
            def blk2(j):
                    nk = 1 if j == 0 else 2
                    SS = nk * 128
                    mask = self.cst[:, C_SWA + 256 - SS:C_SWA + 256]
                    pso = self.PS[6 + (self.psoi % 2)]
                    self.psoi += 1
                    recg = self.R("recg", 2, lambda i: S.sbuf("recg", [128, 4], F32, ctx))
                    ktr = [kTj[j - 1], kTj[j]] if j > 0 else [kTj[0]]
                    sc4 = self.R("sc4", 2, lambda i_: S.sbuf("sc4", [128, 4, 256], F32, ctx))
                    for hl in range(4):
                        i = hl // 2
                        hh = hl % 2
                        ps = self.ps6()
                        for kb in range(nk):
                            self.MM(ps[:, kb * 128:(kb + 1) * 128],
                                    qTj[j][hh * 64:(hh + 1) * 64, i, :], ktr[kb][hh * 64:(hh + 1) * 64, :])
                        self.STT("dve", sc4[:, hl, 0:SS], ps[:, 0:SS], 0.125, mask, ALU.mult, ALU.add)
                        yield
                    sk4 = rows[:, 1536 + 4 * g:1536 + 4 * g + 4]
                    mx4 = self.R("mx4", 2, lambda i_: S.sbuf("mx4", [128, 4], F32, ctx))
                    self.RED("dve", mx4, sc4[:, :, 0:SS], ALU.max)
                    yield
                    self.TT("dve", mx4, mx4, sk4, ALU.max)
                    yield
                    nmx4 = self.R("nmx4", 2, lambda i_: S.sbuf("nmx4", [128, 4], F32, ctx))
                    self.TS("dve", nmx4, mx4, -1.0, ALU.mult)
                    yield
                    pr4 = self.R("pr4", 2, lambda i_: S.sbuf("pr4", [128, 4, 256], BF16, ctx))
                    for hl in range(4):
                        self.ACT(pr4[:, hl, 0:SS], sc4[:, hl, 0:SS], AF.Exp, bias=nmx4[:, hl:hl + 1])
                        yield
                    rs4 = self.R("rs4", 2, lambda i_: S.sbuf("rs4", [128, 4], F32, ctx))
                    self.RED("dve", rs4, pr4[:, :, 0:SS], ALU.add)
                    yield
                    es4 = self.R("es4", 2, lambda i_: S.sbuf("es4", [128, 4], F32, ctx))
                    self.TT("dve", es4, sk4, mx4, ALU.subtract)
                    yield
                    self.ACT(es4, es4, AF.Exp)
                    yield
                    self.TT("dve", rs4, rs4, es4, ALU.add)
                    yield
                    self.RCP(recg, rs4)
                    yield
                    for hl in range(4):
                        pst = self.ps6().bc(BF16)
                        for kb in range(nk):
                            self.TR(pst[:, kb * 128:(kb + 1) * 128], pr4[:, hl, kb * 128:(kb + 1) * 128], self.identb)
                        prT = self.R("prT", 8, lambda i_: S.sbuf("prT", [128, 256], BF16, ctx))
                        self.CP(self.ev(), prT[:, 0:SS], pst[:, 0:SS])
                        yield
                        for kb in range(nk):
                            jk = j - 1 + kb if j > 0 else 0
                            self.MM(pso[:, hl * 64:hl * 64 + 64], prT[:, kb * 128:(kb + 1) * 128],
                                    vtok[jk], start=(kb == 0), stop=(kb == nk - 1))
                    otok = self.R("otok", 2, lambda i: S.sbuf("otok", [128, 256], BF16, ctx))
                    psov = V(pso.ap[:, 0:256].rearrange("p (h d) -> p h d", d=64), pso.trks)
                    recb = V(recg.ap.unsqueeze(2).to_broadcast([128, 4, 64]), recg.trks)
                    ov = V(otok.ap.rearrange("p (h d) -> p h d", d=64), otok.trks)
                    self.TT("dve", ov, psov, recb, ALU.mult)
                    yield
                    pst = self.ps6().bc(BF16)
                    for i in range(2):
                        self.TR(pst[:, i * 128:(i + 1) * 128], otok[:, i * 128:(i + 1) * 128], self.identb)
                    for i in range(2):
                        self.CP(self.ev(), self.mixT[2 * g + i][j // 4][:, (j % 4) * 128:(j % 4) * 128 + 128],
                                pst[:, i * 128:(i + 1) * 128])
                    yield
            for j0 in range(0, 16, 2):
                self.lockstep([blk2(j0), blk2(j0 + 1)])
            S.barrier()

    def mixer_ab(self, b, l, ctx0):
        S = self.S
        e = l // 2
        rab = lambda a, n: self.rows_ab[:, e * RA + a:e * RA + a + n]
        win = self.dr["w_in_ab"][e].rearrange("(kc p) n -> p kc n", p=128)
        sb = lambda name, shape, dt=F32: S.sbuf(name, shape, dt, ctx0)
        identf, onesf, identb = self.identf, self.onesf, self.identb
        triU = self.cst[:, C_TU:C_TU + 128]
        maskL = self.cst[:, C_ML:C_ML + 128]
        negm = self.cst[:, C_NM:C_NM + 128]
        wgate = sb("wgate", [128, 8, 16], BF16)
        self.DMA("pool", wgate[:, :, 0:8], win[:, :, 1536:1544])
        self.DMA("pool", wgate[:, :, 8:16], win[:, :, 3592:3600])
        G = sb("G", [128, 16, 16])
        for j in range(16):
            if j % 4 == 0:
                self.norm_to_hn(l, 0, ctx0, quarters=[j // 4])
            ps = self.ps()
            for kc in range(8):
                self.MM(ps[:, 0:16], self.hq(kc, j // 4)[:, (j % 4) * 128:(j % 4) * 128 + 128], wgate[:, kc, :],
                        start=(kc == 0), stop=(kc == 7))
            self.CP(self.ev(), G[:, j, :], ps[:, 0:16])
        bc4 = lambda v: V(v.ap.unsqueeze(1).to_broadcast([128, 16, 4]), v.trks)
        BETA = sb("BETA", [128, 16, 4])
        self.ACT(BETA, G[:, :, 0:4], AF.Sigmoid)
        X = sb("X", [128, 16, 4])
        T1 = sb("T1", [128, 16, 4])
        T2 = sb("T2", [128, 16, 4])
        SP = sb("SP", [128, 16, 4])

        def softplus(dst, x):
            self.TS("dve", T1, x, -1.0, ALU.mult)
            self.TT("dve", T1, T1, x, ALU.min)
            self.ACT(T1, T1, AF.Exp)
            self.ACT(T1, T1, AF.Ln, bias=1.0)
            self.TS("dve", T2, x, 0.0, ALU.max)
            self.TT("dve", dst, T1, T2, ALU.add)

        GF = sb("GF", [128, 16, 8])
        self.TT("dve", X, G[:, :, 4:8], bc4(rab(0, 4)), ALU.add)
        softplus(SP, X)
        negA = sb("negA", [128, 4])
        self.ACT(negA, rab(4, 4), AF.Exp)
        self.TS("dve", negA, negA, -1.0, ALU.mult)
        self.TT("dve", GF[:, :, 0:4], SP, bc4(negA), ALU.mult)
        ILOG = sb("ILOG", [128, 16, 4])
        self.TT("dve", ILOG, G[:, :, 8:12], bc4(rab(8, 4)), ALU.add)
        self.TT("dve", X, G[:, :, 12:16], bc4(rab(12, 4)), ALU.add)
        self.TS("dve", X, X, -1.0, ALU.mult)
        softplus(SP, X)
        self.TS("dve", GF[:, :, 4:8], SP, -1.0, ALU.mult)
        CUM = sb("CUM", [128, 16, 16])
        for j in range(16):
            ps = self.ps()
            self.MM(ps[:, 0:8], triU, GF[:, j, :])
            self.MM(ps[:, 8:16], onesf, GF[:, j, :])
            self.CP(self.ev(), CUM[:, j, :], ps[:, 0:16])
        gcum, bcum, gtot = CUM[:, :, 0:4], CUM[:, :, 4:8], CUM[:, :, 8:12]
        BG = sb("BG", [128, 16, 4])
        KD = sb("KD", [128, 16, 4])
        EGL = sb("EGL", [128, 16, 4])
        RR = sb("RR", [128, 16, 4])
        self.ACT(BG, gcum, AF.Exp)
        self.TT("dve", BG, BG, BETA, ALU.mult)
        self.TT("dve", KD, gtot, gcum, ALU.subtract)
        self.ACT(KD, KD, AF.Exp)
        self.ACT(EGL, gtot, AF.Exp)
        self.TT("dve", RR, ILOG, bcum, ALU.subtract)
        cols = [i * 128 for i in range(4)]
        vexts = [sb("vext", [128, 129], BF16) for _ in range(4)]
        for v in vexts:
            S.add("pool", lambda e_, v=v: e_.memset(v.ap[:, 128:129], 1.0), writes=[v])
        wh = sb("wh", [128, 8, 1024], BF16)
        Sa = sb("Sa", [128, 128])
        Sab = sb("Sab", [128, 128], BF16)
        Cx = sb("Cx", [128, 129])
        Cb = sb("Cb", [128, 129], BF16)
        mst = sb("mst", [128, 1])
        cwork = sb("cwork", [128, 515])
        ctail = [sb("ctail", [128, 4]) for _ in range(3)]
        qT = sb("qT", [128, 512], BF16)
        kT = sb("kT", [128, 512], BF16)
        vT = sb("vT", [128, 512], BF16)
        qbTs = [sb("qbT", [128, 512], BF16) for _ in range(2)]
        kbT = sb("kbT", [128, 512], BF16)
        acc = sb("acc", [128, 512])
        sl = acc
        t128 = lambda name, dt=F32: self.R(name, 2, lambda i_: S.sbuf(name, [128, 128], dt, ctx0))
        c1 = lambda name, n=1: self.R(name, 2, lambda i_: S.sbuf(name, [128, n], F32, ctx0))

        def finish(key, pf, o, gain, gate, ci, q, tsl):
            tmp = t128(pf + "f_tmp")
            self.ACT(tmp, o, AF.Square)
            yield
            ss = c1(pf + "f_ss")
            self.RED("dve", ss, tmp, ALU.add)
            yield
            self.ACT(ss, ss, AF.Ln, bias=EPS, scale=1.0 / 128.0)
            self.ACT(ss, ss, AF.Exp, scale=-0.5)
            yield
            y = t128(pf + "f_y")
            self.STT("dve", y, o, ss, gain, ALU.mult, ALU.mult)
            yb = t128(pf + "f_yb", BF16)
            self.TT("dve", yb, y, gate, ALU.mult)
            yield
            pY = self.psr(key).bc(BF16)
            self.TR(pY[:, 0:128], yb, identb)
            self.CP(self.ev(), self.mixT[ci][q][:, tsl], pY[:, 0:128])
            yield

        for i in range(4):
            gcols = [i * 128, 512 + i * 128, 1024 + i * 128, 2056 + i * 128, 2568 + i * 128,
                     3080 + i * 128, 3600 + i * 128, 1544 + i * 128]
            for gi, c0 in enumerate(gcols):
                self.DMA("pool", wh[:, :, gi * 128:(gi + 1) * 128], win[:, :, c0:c0 + 128])
            for t_ in (Sa, Sab, Cx, Cb, mst):
                S.add("pool", lambda e_, t_=t_: e_.memset(t_.ap, 0.0), writes=[t_])
            for g in range(3):
                S.add("pool", lambda e_, t_=ctail[g]: e_.memset(t_.ap, 0.0), writes=[ctail[g]])
            def prep(q):
                ps = self.psr("P")
                for c in range(8):
                    sq = self.R("sq", 2, lambda i_: S.sbuf("sq", [128, 512], BF16, ctx0))
                    if c % 2 == 0:
                        self.ACT(sq, self.h[c][q], AF.Square)
                    else:
                        self.TT("dve", sq, self.h[c][q], self.h[c][q], ALU.mult)
                    self.MM(ps, self.onesb, sq, start=(c == 0), stop=(c == 7))
                    yield
                rstd = self.R("rstd", 2, lambda i_: S.sbuf("rstd", [128, 512], F32, ctx0))
                self.ACT(rstd, ps, AF.Ln, bias=EPS, scale=1.0 / D_MODEL)
                self.ACT(rstd, rstd, AF.Exp, scale=-0.5)
                yield
                for c in range(8):
                    self.STT("dve", self.hq(c, q), self.h[c][q], self.gc(l, 0, c), rstd, ALU.mult, ALU.mult)
                    yield
                for gi in range(5):
                    ps = self.psr("P")
                    for kc in range(8):
                        self.MM(ps, wh[:, kc, gi * 128:(gi + 1) * 128], self.hq(kc, q), start=(kc == 0), stop=(kc == 7))
                    if gi < 3:
                        cb_ = cwork
                        self.CP("pool", cb_[:, 0:3], ctail[gi][:, 0:3])
                        yield
                        self.CP(self.ev(), cb_[:, 3:515], ps)
                        yield
                        wc = lambda k: self.convw[:, ((e * 4 + i) * 3 + gi) * 4 + k:((e * 4 + i) * 3 + gi) * 4 + k + 1]
                        self.TS("dve", acc, cb_[:, 0:512], wc(0), ALU.mult)
                        yield
                        for k in range(1, 4):
                            self.STT("dve", acc, cb_[:, k:k + 512], wc(k), acc, ALU.mult, ALU.add)
                            yield
                        self.CP("pool", ctail[gi][:, 0:3], cb_[:, 512:515])
                        yield
                        self.ACT(sl, acc, AF.Silu)
                        yield
                        if gi < 2:
                            sqb = self.R("sq", 2, lambda i_: S.sbuf("sq", [128, 512], BF16, ctx0))
                            self.ACT(sqb, sl, AF.Square)
                            yield
                            p2 = self.psr("P")
                            self.MM(p2, self.onesb, sqb)
                            rn5 = p2
                            self.ACT(rn5, p2, AF.Ln, bias=EPS)
                            yield
                            self.ACT(rn5, rn5, AF.Exp, scale=-0.5)
                            yield
                            if gi == 0:
                                self.STT("dve", qT, sl, 128.0 ** -0.5, rn5, ALU.mult, ALU.mult)
                                yield
                            else:
                                self.TT("dve", kT, sl, rn5, ALU.mult)
                                yield
                        else:
                            self.CP("pool", vT, sl)
                            yield
                    elif gi == 3:
                        self.ACT(qbTs[q % 2], ps, AF.Copy, scale=128.0 ** -0.5)
                        yield
                    else:
                        self.CP("dve", kbT, ps)
                        yield
                yield
            self.lockstep([prep(0)])
            for q in range(4):
                qbT = qbTs[q % 2]
                def mk_tile(jj):
                    j = 4 * q + jj
                    tsl = slice(jj * 128, (jj + 1) * 128)
                    col = lambda T_, off=0: T_[:, j, off + i:off + i + 1]
                    pt = self.ps()
                    for kc in range(8):
                        self.MM(pt, self.hq(kc, q)[:, tsl], wh[:, kc, 512:1024], start=(kc == 0), stop=(kc == 7))
                    h4 = lambda name, dt=F32: self.R(name, 4, lambda i_: S.sbuf(name, [128, 128], dt, ctx0))
                    kbtok = h4("kbtok")
                    self.CP("act", kbtok, pt[:, 0:128])
                    vext = vexts[j % 4]
                    self.CP("dve", vext[:, 0:128], pt[:, 128:256])
                    og = h4("og")
                    self.ACT(og, pt[:, 256:384], AF.Sigmoid)
                    sz = h4("sz")
                    self.ACT(sz, pt[:, 384:512], AF.Silu)
                    beta, gc_, bg, kdc, egl = col(BETA), col(CUM), col(BG), col(KD), col(EGL)
                    rcol, bcc, bl = col(RR), col(CUM, 4), col(CUM, 12)
                    gA = rab(16, 128)
                    gB = rab(144 + i * 128, 128)
                    h4 = lambda name, dt=F32: self.R(name, 4, lambda i_: S.sbuf(name, [128, 128], dt, ctx0))
                    kdec, qdT, ATb, wTn = h4("kdec", BF16), h4("qdT", BF16), h4("ATb", BF16), h4("wTn", BF16)
                    u, P0T, kw0 = h4("u"), h4("P0T", BF16), h4("kw0", BF16)
                    mi = self.R("mi", 4, lambda i_: S.sbuf("mi", [128, 1], F32, ctx0))
                    maxr = self.R("maxr", 4, lambda i_: S.sbuf("maxr", [128, 1], F32, ctx0))
                    kA = "A%d" % (jj % 2)
                    kB = "B%d" % (jj % 2)
                    def bulkA():
                        ptr = self.psr(kA).bc(BF16)
                        self.TR(ptr[:, 0:128], kT[:, tsl], identb)
                        self.TR(ptr[:, 128:256], vT[:, tsl], identb)
                        Rm = self.R("Rm", 2, lambda i_: S.sbuf("Rm", [128, 256], F32, ctx0))
                        self.TS("dve", Rm[:, 0:128], ptr[:, 128:256], beta, ALU.mult)
                        yield
                        self.TS("dve", Rm[:, 128:256], ptr[:, 0:128], bg, ALU.mult)
                        yield
                        self.TS("dve", kdec, ptr[:, 0:128], kdc, ALU.mult)
                        yield
                        dg = t128("dg")
                        self.TS("dve", dg, identf, gc_, ALU.mult)
                        yield
                        pG = self.psr(kA)
                        self.MM(pG[:, 0:128], onesf, dg)
                        D1 = t128("D1")
                        self.STT("dve", D1, pG[:, 0:128], gc_, maskL, ALU.subtract, ALU.mult)
                        yield
                        DT = t128("DT")
                        self.STT("dve", DT, pG[:, 0:128], gc_, triU, ALU.subtract, ALU.mult)
                        yield
                        EgR = pG[:, 0:128]
                        self.ACT(EgR, pG[:, 0:128], AF.Exp)
                        yield
                        self.ACT(D1, D1, AF.Exp, scale=-1.0)
                        yield
                        self.TT("dve", D1, D1, maskL, ALU.mult)
                        yield
                        self.ACT(DT, DT, AF.Exp)
                        yield
                        self.TT("dve", DT, DT, triU, ALU.mult)
                        yield
                        self.TT("dve", qdT, qT[:, tsl], EgR, ALU.mult)
                        yield
                        pK = self.psr(kA)
                        self.MM(pK[:, 0:128], kT[:, tsl], kT[:, tsl])
                        self.MM(pK[:, 128:256], kT[:, tsl], qT[:, tsl])
                        Lf = t128("Lf")
                        self.STT("dve", Lf, pK[:, 0:128], beta, D1, ALU.mult, ALU.mult)
                        yield
                        self.TT("dve", ATb, pK[:, 128:256], DT, ALU.mult)
                        yield
                        pL = self.psr(kA)
                        self.TR(pL[:, 0:128], Lf, identf)
                        LTf = t128("LTf")
                        self.CP("act", LTf, pL[:, 0:128])
                        yield
                        Mf = t128("Mf")
                        MTf = t128("MTf")
                        t128b = lambda name: self.R(name, 2, lambda i_: S.sbuf(name, [128, 128], F32, ctx0))
                        Ck = t128b("Ck")
                        CTk = t128b("CTk")
                        self.TT("pool", Ck, Lf, self.lvlm[:, 0:128], ALU.mult)
                        self.TT("pool", CTk, LTf, self.lvlm[:, 896:1024], ALU.mult)
                        self.TT("pool", Mf, identf, Ck, ALU.subtract)
                        self.TT("pool", MTf, identf, CTk, ALU.subtract)
                        yield
                        for k in range(1, 7):
                            Ck = t128b("Ck")
                            CTk = t128b("CTk")
                            self.TT("pool", Ck, Lf, self.lvlm[:, k * 128:(k + 1) * 128], ALU.mult)
                            self.TT("pool", CTk, LTf, self.lvlm[:, 896 + k * 128:896 + (k + 1) * 128], ALU.mult)
                            pa_ = self.psr(kA)
                            self.MM(pa_[:, 0:128], CTk, Mf)
                            self.MM(pa_[:, 128:256], Ck, MTf)
                            T1f = t128("T1f")
                            T3f = t128("T3f")
                            self.CP("act", T1f, pa_[:, 0:128])
                            self.CP("dve", T3f, pa_[:, 128:256])
                            yield
                            pb_ = self.psr(kA)
                            self.MM(pb_[:, 0:128], MTf, T1f)
                            self.MM(pb_[:, 128:256], Mf, T3f)
                            self.TT("dve", Mf, Mf, pb_[:, 0:128], ALU.subtract)
                            self.TT("dve", MTf, MTf, pb_[:, 128:256], ALU.subtract)
                            yield
                        pX = self.psr(kA)
                        self.MM(pX[:, 0:256], MTf, Rm)
                        wb = t128("wb", BF16)
                        self.CP("act", u, pX[:, 0:128])
                        yield
                        self.CP("dve", wb, pX[:, 128:256])
                        yield
                        pW = self.psr(kA).bc(BF16)
                        self.TR(pW[:, 0:128], wb, identb)
                        self.ACT(wTn, pW[:, 0:128], AF.Copy, scale=-1.0)
                        yield
                    def scanA():
                        pV = self.psr("SA")
                        self.MM(pV[:, 0:128], wTn, Sab)
                        vn = t128("vn", BF16)
                        self.TT("dve", vn, u, pV[:, 0:128], ALU.add)
                        yield
                        pO = self.psr("SA")
                        self.MM(pO[:, 0:128], qdT, Sab, start=True, stop=False)
                        self.MM(pO[:, 0:128], ATb, vn, start=False, stop=True)
                        oA = t128("oA")
                        self.CP("act", oA, pO[:, 0:128])
                        yield
                        pS = self.psr("SA")
                        self.MM(pS[:, 0:128], kdec, vn)
                        self.STT("dve", Sa, Sa, egl, pS[:, 0:128], ALU.mult, ALU.add)
                        yield
                        self.CP("act", Sab, Sa)
                        yield
                        yield from finish("SA", "a", oA, gA, sz, i, q, tsl)
                    def bulkB():
                        dgr = t128("dgr")
                        self.TS("dve", dgr, identf, rcol, ALU.mult)
                        yield
                        pR = self.psr(kB)
                        self.MM(pR[:, 0:128], onesf, dgr)
                        tmpB = t128("tmpB")
                        self.TT("dve", tmpB, pR[:, 0:128], negm, ALU.add)
                        yield
                        self.RED("dve", maxr, pR[:, 0:128], ALU.max)
                        yield
                        mr = c1("mr")
                        self.RED("dve", mr, tmpB, ALU.max)
                        yield
                        nmr = c1("nmr")
                        self.TS("dve", nmr, mr, -1.0, ALU.mult)
                        yield
                        Eb = t128("Eb")
                        self.ACT(Eb, tmpB, AF.Exp, bias=nmr)
                        yield
                        pQ = self.psr(kB)
                        self.MM(pQ[:, 0:128], qbT[:, tsl], kbT[:, tsl])
                        P0 = t128("P0", BF16)
                        self.TT("dve", P0, pQ[:, 0:128], Eb, ALU.mult)
                        yield
                        pP0 = self.psr(kB).bc(BF16)
                        self.TR(pP0[:, 0:128], P0, identb)
                        self.CP("act", P0T, pP0[:, 0:128])
                        yield
                        ew = c1("ew")
                        self.TT("dve", ew, rcol, maxr, ALU.subtract)
                        yield
                        self.ACT(ew, ew, AF.Exp)
                        yield
                        self.TS("dve", kw0, kbtok, ew, ALU.mult)
                        yield
                        self.TT("dve", mi, mr, bcc, ALU.add)
                        yield
                    def scanB():
                        a_ = c1("a_")
                        self.TT("dve", a_, bcc, mst, ALU.add)
                        yield
                        mt = c1("mt")
                        self.TT("dve", mt, a_, mi, ALU.max)
                        yield
                        e3 = c1("e3", 3)
                        self.TT("dve", e3[:, 0:1], a_, mt, ALU.subtract)
                        yield
                        self.TT("dve", e3[:, 1:2], mi, mt, ALU.subtract)
                        yield
                        self.TS("dve", e3[:, 2:3], mt, -1.0, ALU.mult)
                        yield
                        self.ACT(e3, e3, AF.Exp)
                        yield
                        p1 = self.psr("SB")
                        self.MM(p1[:, 0:129], qbT[:, tsl], Cb)
                        p2 = self.psr("SB")
                        self.MM(p2[:, 0:129], P0T, vext)
                        nd = self.R("nd", 2, lambda i_: S.sbuf("nd", [128, 129], F32, ctx0))
                        self.TS("dve", nd, p1[:, 0:129], e3[:, 0:1], ALU.mult)
                        yield
                        self.STT("dve", nd, p2[:, 0:129], e3[:, 1:2], nd, ALU.mult, ALU.add)
                        yield
                        td = c1("td")
                        self.TS("dve", td, nd[:, 128:129], -1.0, ALU.mult)
                        yield
                        self.TT("dve", td, td, nd[:, 128:129], ALU.max)
                        yield
                        self.TT("dve", td, td, e3[:, 2:3], ALU.max)
                        yield
                        self.RCP(td, td)
                        yield
                        hB = t128("hB")
                        self.TS("dve", hB, nd[:, 0:128], td, ALU.mult)
                        yield
                        mm = c1("mm")
                        self.TT("dve", mm, mst, maxr, ALU.max)
                        yield
                        e2 = c1("e2", 2)
                        self.TT("dve", e2[:, 0:1], mst, mm, ALU.subtract)
                        yield
                        self.TT("dve", e2[:, 1:2], maxr, mm, ALU.subtract)
                        yield
                        self.ACT(e2, e2, AF.Exp)
                        yield
                        p3 = self.psr("SB")
                        self.MM(p3[:, 0:129], kw0, vext)
                        self.TS("dve", Cx, Cx, e2[:, 0:1], ALU.mult)
                        yield
                        self.STT("dve", Cx, p3[:, 0:129], e2[:, 1:2], Cx, ALU.mult, ALU.add)
                        yield
                        self.CP("act", Cb, Cx)
                        yield
                        self.TT("dve", mst, bl, mm, ALU.add)
                        yield
                        yield from finish("SB", "b", hB, gB, og, 4 + i, q, tsl)
                    return bulkA, scanA, bulkB, scanB
                TL = [mk_tile(jj) for jj in range(4)]
                def seq(*gs):
                    for g_ in gs:
                        yield from g_
                def par(*gs):
                    gs = list(gs)
                    while gs:
                        al = []
                        for g_ in gs:
                            try:
                                next(g_)
                                al.append(g_)
                            except StopIteration:
                                pass
                        gs = al
                        yield
                self.lockstep([TL[0][0](), TL[0][2](), TL[1][0](), TL[1][2]()])
                self.lockstep([TL[2][0](), TL[2][2](), TL[3][0](), TL[3][2](),
                               seq(par(TL[0][1](), TL[0][3]()), par(TL[1][1](), TL[1][3]()))])
                P2 = [seq(par(TL[2][1](), TL[2][3]()), par(TL[3][1](), TL[3][3]()))]
                if q < 3:
                    P2.append(prep(q + 1))
                self.lockstep(P2)


def make_consts():
    c = np.zeros((128, C_END), np.float32)
    i = np.arange(128)
    P, Fr = i[:, None], i[None, :]
    same = (P // 64) == (Fr // 64)
    c[:, C_ID:C_ID + 128] = np.eye(128)
    s = np.arange(256)[None, :]
    c[:, C_SWA:C_SWA + 256] = np.where((s > P) & (s <= P + 128), 0.0, NEG)
    half = 32
    inv = (10000.0 ** (-np.arange(half, dtype=np.float32) / half)).astype(np.float32)
    c[:, C_INV:C_INV + 32] = inv[None, :]
    c[:, C_ONE:C_ONE + 128] = 1.0
    c[:, C_TU:C_TU + 128] = (Fr >= P)
    c[:, C_ML:C_ML + 128] = (Fr < P)
    c[:, C_NM:C_NM + 128] = np.where(Fr <= P, 0.0, NEG)
    return c


DEBUG = False


def make_lvlmask():
    m = np.zeros((128, 1792), np.float32)
    i = np.arange(128)
    c, e = i[:, None], i[None, :]
    for k in range(7):
        sz = 1 << k
        mk = ((c // (2 * sz)) == (e // (2 * sz))) & ((c % (2 * sz)) >= sz) & ((e % (2 * sz)) < sz)
        m[:, k * 128:(k + 1) * 128] = mk
        m[:, 896 + k * 128:896 + (k + 1) * 128] = mk.T
    return m


def build_nc(NSEQ, LAYERS, mix=("ab", "c")):
    nc = bass.Bass("TRN2", target_bir_lowering=False)

    def di(name, shape, dt=F32):
        return nc.dram_tensor(name, list(shape), dt, kind="ExternalInput").ap()

    dr = {
        "xT": di("xT", [NSEQ, 1024, 2048]),
        "pT": di("pT", [4, NSEQ, 256, 2048]),
        "pos": di("pos", [NSEQ, 128, 16], I32),
        "consts": di("consts", [128, C_END]),
        "lvlmask": di("lvlmask", [128, 1792]),
        "gcols": di("gcols", [128, 128]),
        "convw": di("convw", [128, 96]),
        "rows_ab": di("rows_ab", [128, 2 * RA]),
        "rows_c": di("rows_c", [2, 128, RC]),
        "bo_cols": di("bo_cols", [128, 16]),
        "w_in_ab": di("w_in_ab", [2, 1024, IN_COLS]),
        "w_out_ab": di("w_out_ab", [2, 1024, 1024]),
        "w_qkv_c": di("w_qkv_c", [2, 1024, 1536]),
        "w_o_c": di("w_o_c", [2, 1024, 1024]),
        "w_up": di("w_up", [4, 1024, 4096]),
        "w_down": di("w_down", [4, 4096, 1024]),
        "w_ple": di("w_ple", [4, 256, 1024]),
        "w_ple_gate": di("w_ple_gate", [4, 1024, 1024]),
    }
    dr["outT"] = nc.dram_tensor("outT", [NSEQ, 1024, 2048], F32, kind="ExternalOutput").ap()
    with ExitStack() as ctx:
        kb = KB(nc, ctx, NSEQ, LAYERS, mix)
        kb.debug = DEBUG
        kb.build(dr)
    return nc


def host_params(inp):
    f = lambda a: np.ascontiguousarray(np.asarray(a, dtype=np.float32))
    ng = f(inp["norm_gains"])
    gcols = np.ascontiguousarray(ng.reshape(4, 4, 8, 128).transpose(3, 0, 1, 2).reshape(128, 128))
    cw = f(inp["conv_a"])
    convw = np.ascontiguousarray(cw.reshape(2, 4, 3, 4, 128).transpose(4, 0, 3, 2, 1).reshape(128, 96))
    rows = np.zeros((2, RA), np.float32)
    for e in range(2):
        rows[e, 0:4] = f(inp["dt_bias"])[e]
        rows[e, 4:8] = f(inp["a_log"])[e]
        rows[e, 8:12] = f(inp["i_bias_b"])[e]
        rows[e, 12:16] = f(inp["f_bias_b"])[e]
        rows[e, 16:144] = f(inp["norm_a"])[e]
        rows[e, 144:656] = f(inp["norm_b"])[e].reshape(512)
    rows_ab = np.ascontiguousarray(np.broadcast_to(rows.reshape(1, 2 * RA), (128, 2 * RA)))
    rc = np.zeros((2, RC), np.float32)
    for o in range(2):
        rc[o, 0:1536] = f(inp["b_qkv_c"])[o]
        rc[o, 1536:1552] = f(inp["sinks_c"])[o]
    rows_c = np.ascontiguousarray(np.broadcast_to(rc[:, None, :], (2, 128, RC)))
    bo = f(inp["b_o_c"])
    bo_cols = np.ascontiguousarray(bo.reshape(2, 8, 128).transpose(2, 0, 1).reshape(128, 16))
    return {
        "consts": make_consts(), "lvlmask": make_lvlmask(), "gcols": gcols, "convw": convw, "rows_ab": rows_ab, "rows_c": rows_c,
        "bo_cols": bo_cols,
        "w_in_ab": f(inp["w_in_ab"]), "w_out_ab": f(inp["w_out_ab"]), "w_qkv_c": f(inp["w_qkv_c"]),
        "w_o_c": f(inp["w_o_c"]), "w_up": f(inp["w_up"]), "w_down": f(inp["w_down"]),
        "w_ple": f(inp["w_ple"]), "w_ple_gate": f(inp["w_ple_gate"]),
    }


def run(inp, n_cores=8, LAYERS=DEPTH, mix=("ab", "c"), trace=False):
    x = np.asarray(inp["x"], dtype=np.float32)
    p = np.asarray(inp["p"], dtype=np.float32)
    pos = np.asarray(inp["positions"], dtype=np.int32)
    B = x.shape[0]
    NSEQ = B // n_cores
    shared = host_params(inp)
    nc = build_nc(NSEQ, LAYERS, mix)
    in_maps = []
    for c in range(n_cores):
        sl = slice(c * NSEQ, (c + 1) * NSEQ)
        m = dict(shared)
        m["xT"] = np.ascontiguousarray(x[sl].transpose(0, 2, 1))
        m["pT"] = np.ascontiguousarray(p[:, sl].transpose(0, 1, 3, 2))
        m["pos"] = np.ascontiguousarray(pos[sl].reshape(-1, 16, 128).transpose(0, 2, 1))
        in_maps.append(m)
    res = run_bass_kernel_spmd(nc, in_maps, core_ids=list(range(n_cores)), trace=trace)
    out = np.concatenate([np.asarray(r["outT"]).transpose(0, 2, 1) for r in res.results], axis=0)
    return np.ascontiguousarray(out.astype(np.float32)), res


def kernel(**inputs):
    out, _ = run(inputs)
    return out
```

```python
import numpy as np
import concourse.bass as bass
import concourse.mybir as mybir
from concourse.bass_utils import run_bass_kernel_spmd
from contextlib import ExitStack

F32 = mybir.dt.float32
BF16 = mybir.dt.bfloat16
I32 = mybir.dt.int32
AF = mybir.ActivationFunctionType
ALU = mybir.AluOpType
AX = mybir.AxisListType

ENGS = ("pe", "act", "dve", "pool", "sp")
SEM_EPOCH = 4000
DMA_K = 8
INLINE_WAIT = True

D_MODEL = 1024
SEQ = 2048
DEPTH = 4
D_FF = 4096
PLE = 256
EPS = 1e-6
NEG = -30000.0
A_COLS = 2056
IN_COLS = 4112
C_ID, C_SWA, C_INV, C_ONE, C_TU, C_ML, C_NM, C_END = (0, 128, 384, 416, 544, 672, 800, 928)
RA = 656
RC = 1552


class Trk:
    __slots__ = ("name", "w", "rs", "excl")

    def __init__(self, name, excl=False):
        self.name = name
        self.w = None
        self.rs = []
        self.excl = excl


class V:
    __slots__ = ("ap", "trks")

    def __init__(self, ap, trks):
        self.ap = ap
        self.trks = tuple(trks)

    def __getitem__(self, idx):
        return V(self.ap[idx], self.trks)

    def bc(self, dt):
        return V(self.ap.bitcast(dt), self.trks)


class Ins:
    __slots__ = ("eng", "fn", "deps", "dma", "sig", "ev", "fc", "out")

    def __init__(self, eng, fn, deps, dma, out):
        self.eng = eng
        self.fn = fn
        self.deps = deps
        self.dma = dma
        self.sig = False
        self.ev = None
        self.fc = None
        self.out = out


class Sched:
    def __init__(self, nc, ctx):
        self.nc = nc
        self.ctx = ctx
        self.instrs = []
        self.per_eng = {e: [] for e in ENGS}
        self.uid = 0

    def sbuf(self, name, shape, dtype, ctx=None):
        self.uid += 1
        t = (ctx or self.ctx).enter_context(
            self.nc.sbuf_tensor("%s_%d" % (name, self.uid), list(shape), dtype))
        return V(t[tuple(slice(None) for _ in shape)], [Trk(name)])

    def psum(self, name, shape, dtype=F32):
        t = self.ctx.enter_context(self.nc.psum_tensor(name, list(shape), dtype))
        return V(t[tuple(slice(None) for _ in shape)], [Trk(name, excl=True)])

    def add(self, eng, fn, reads=(), writes=(), dma=False, out=False):
        me = len(self.instrs)
        deps = set()
        for r in reads:
            for t in r.trks:
                if t.excl:
                    if t.w is not None:
                        deps.add(t.w)
                    deps.update(t.rs)
                    t.w = me
                    t.rs = []
                else:
                    if t.w is not None:
                        deps.add(t.w)
                    t.rs.append(me)
        for w in writes:
            for t in w.trks:
                if t.w is not None:
                    deps.add(t.w)
                deps.update(t.rs)
                t.w = me
                t.rs = []
        deps.discard(me)
        self.instrs.append(Ins(eng, fn, deps, dma, out))
        self.per_eng[eng].append(me)
        return me

    def barrier(self):
        last = []
        for e in ENGS:
            lst = self.per_eng[e]
            nd = 0
            seen_c = False
            for idx in reversed(lst):
                ins = self.instrs[idx]
                if ins.fn is None:
                    continue
                if ins.dma:
                    if nd < DMA_K:
                        last.append(idx)
                        nd += 1
                elif not seen_c:
                    last.append(idx)
                    seen_c = True
                if nd >= DMA_K and seen_c:
                    break
        for e in ENGS:
            me = len(self.instrs)
            self.instrs.append(Ins(e, None, set(last), False, False))
            self.per_eng[e].append(me)

    def emit(self):
        nc = self.nc
        instrs = self.instrs
        for ins in instrs:
            for d in ins.deps:
                dd = instrs[d]
                if dd.dma:
                    continue
                if dd.eng == "pe" and ins.eng == "pe" and not ins.dma and ins.fn is not None:
                    continue
                dd.sig = True
        cnt = {e: 0 for e in ENGS}
        dcnt = {e: 0 for e in ENGS}
        for ins in instrs:
            if ins.fn is None:
                continue
            if ins.dma:
                j = dcnt[ins.eng]
                dcnt[ins.eng] += 1
                ins.ev = (("d", ins.eng, j % DMA_K), 16 * (j // DMA_K + 1))
                if j >= DMA_K:
                    ins.fc = (("d", ins.eng, j % DMA_K), 16 * (j // DMA_K))
            elif ins.sig:
                n = cnt[ins.eng]
                cnt[ins.eng] += 1
                ins.ev = (("c", ins.eng, n // SEM_EPOCH), n % SEM_EPOCH + 1)
        sems = {}
        for ins in instrs:
            if ins.ev is not None and ins.ev[0] not in sems:
                k = ins.ev[0]
                sems[k] = self.ctx.enter_context(nc.semaphore("s_%s_%s_%d" % k))
        out_events = [ins.ev for ins in instrs if ins.dma and ins.out]
        self.stats = {e: len(self.per_eng[e]) for e in ENGS}

        def run_engine(ename, eng):
            known = {}
            for idx in self.per_eng[ename]:
                ins = instrs[idx]
                best = {}
                for d in ins.deps:
                    dd = instrs[d]
                    if dd.ev is None:
                        continue
                    if (not dd.dma) and dd.eng == "pe" and ename == "pe" and not ins.dma \
                            and ins.fn is not None:
                        continue
                    k, v = dd.ev
                    if known.get(k, 0) < v and best.get(k, 0) < v:
                        best[k] = v
                if ins.fc is not None:
                    k, v = ins.fc
                    if known.get(k, 0) < v and best.get(k, 0) < v:
                        best[k] = v
                items = list(best.items())
                inline = None
                if INLINE_WAIT and ins.fn is not None and (not ins.dma) and ename in ("act", "dve", "pool", "pe") and items:
                    inline = items.pop()
                for k, v in items:
                    eng.wait_ge(sems[k], v)
                    known[k] = v
                if ins.fn is None:
                    continue
                bi = ins.fn(eng)
                if inline is not None:
                    bi._wait_ge(sems[inline[0]], inline[1])
                    known[inline[0]] = inline[1]
                if ins.ev is not None:
                    bi.then_inc(sems[ins.ev[0]], 16 if ins.dma else 1)
            if ename == "sp":
                best = {}
                for (k, v) in out_events:
                    if best.get(k, 0) < v:
                        best[k] = v
                for k, v in best.items():
                    if known.get(k, 0) < v:
                        eng.wait_ge(sems[k], v)

        with nc.Block() as block:
            @block.tensor
            def _(e):
                run_engine("pe", e)

            @block.scalar
            def _(e):
                run_engine("act", e)

            @block.vector
            def _(e):
                run_engine("dve", e)

            @block.gpsimd
            def _(e):
                run_engine("pool", e)

            @block.sync
            def _(e):
                run_engine("sp", e)


class KB:
    def __init__(self, nc, ctx, NSEQ, LAYERS, mix=("ab", "c")):
        self.nc = nc
        self.S = Sched(nc, ctx)
        self.NSEQ = NSEQ
        self.LAYERS = LAYERS
        self.mix = mix
        self.evi = 0
        self.psoi = 0
        self.psrc = {}
        self.psi = 0
        self.rot = {}

    def ev(self):
        self.evi += 1
        return "act" if self.evi % 2 else "dve"

    def ps(self):
        p = self.PS[self.psi % 8]
        self.psi += 1
        return p

    PSR_BANKS = {"A0": (0,), "A1": (1,), "B0": (2,), "B1": (3,), "SA": (4, 5), "SB": (6, 7), "P": (0, 1, 2, 3)}

    def psr(self, key):
        banks = self.PSR_BANKS[key]
        n = self.psrc.get(key, 0)
        self.psrc[key] = n + 1
        return self.PS[banks[n % len(banks)]]

    def ps6(self):
        p = self.PS[self.psi % 6]
        self.psi += 1
        return p

    def ACT(self, out, in_, func, bias=0.0, scale=1.0):
        reads = [in_]
        b, s = bias, scale
        if isinstance(bias, V):
            reads.append(bias)
            b = bias.ap
        if isinstance(scale, V):
            reads.append(scale)
            s = scale.ap
        self.S.add("act", lambda e: e.activation(out=out.ap, in_=in_.ap, func=func, bias=b, scale=s),
                   reads=reads, writes=[out])

    def CP(self, eng, out, in_):
        if eng == "act":
            self.S.add("act", lambda e: e.copy(out=out.ap, in_=in_.ap), reads=[in_], writes=[out])
        else:
            self.S.add(eng, lambda e: e.tensor_copy(out=out.ap, in_=in_.ap), reads=[in_], writes=[out])

    def TT(self, eng, out, a, b, op):
        self.S.add(eng, lambda e: e.tensor_tensor(out=out.ap, in0=a.ap, in1=b.ap, op=op),
                   reads=[a, b], writes=[out])

    def TS(self, eng, out, a, s1, op0, s2=None, op1=None):
        reads = [a]
        x1, x2 = s1, s2
        if isinstance(s1, V):
            reads.append(s1)
            x1 = s1.ap
        if isinstance(s2, V):
            reads.append(s2)
            x2 = s2.ap
        if op1 is None:
            self.S.add(eng, lambda e: e.tensor_scalar(out=out.ap, in0=a.ap, scalar1=x1, scalar2=None, op0=op0),
                       reads=reads, writes=[out])
        else:
            self.S.add(eng, lambda e: e.tensor_scalar(out=out.ap, in0=a.ap, scalar1=x1, scalar2=x2,
                                                      op0=op0, op1=op1), reads=reads, writes=[out])

    def STT(self, eng, out, a, s, b, op0, op1):
        reads = [a, b]
        x = s
        if isinstance(s, V):
            reads.append(s)
            x = s.ap
        self.S.add(eng, lambda e: e.scalar_tensor_tensor(out=out.ap, in0=a.ap, scalar=x, in1=b.ap,
                                                         op0=op0, op1=op1), reads=reads, writes=[out])

    def RED(self, eng, out, in_, op):
        self.S.add(eng, lambda e: e.tensor_reduce(out=out.ap, in_=in_.ap, axis=AX.X, op=op),
                   reads=[in_], writes=[out])

    def RCP(self, out, in_):
        self.S.add("dve", lambda e: e.reciprocal(out=out.ap, in_=in_.ap), reads=[in_], writes=[out])

    def MM(self, ps, lhsT, rhs, start=True, stop=True):
        self.S.add("pe", lambda e: e.matmul(ps.ap, lhsT=lhsT.ap, rhs=rhs.ap, start=start, stop=stop),
                   reads=[lhsT, rhs], writes=[ps])

    def TR(self, ps, in_, ident):
        self.S.add("pe", lambda e: e.transpose(out=ps.ap, in_=in_.ap, identity=ident.ap),
                   reads=[in_, ident], writes=[ps])

    def DMA(self, q, out, in_, is_out=False):
        if is_out:
            self.S.add(q, lambda e: e.dma_start(out=out, in_=in_.ap), reads=[in_], dma=True, out=True)
        else:
            self.S.add(q, lambda e: e.dma_start(out=out.ap, in_=in_), writes=[out], dma=True)

    def dbg(self, name, v, dtype=F32):
        if not getattr(self, "debug", False):
            return
        shp = list(v.ap.shape)
        d = self.nc.dram_tensor(name, shp, dtype, kind="ExternalOutput").ap()
        self.DMA("sp", d, v, is_out=True)

    def lockstep(self, gens):
        gens = list(gens)
        while gens:
            alive = []
            for g in gens:
                try:
                    next(g)
                    alive.append(g)
                except StopIteration:
                    pass
            gens = alive

    def R(self, key, n, mk):
        if key not in self.rot:
            self.rot[key] = [[mk(i) for i in range(n)], 0]
        lst = self.rot[key]
        v = lst[0][lst[1] % n]
        lst[1] += 1
        return v

    def build(self, dr):
        S = self.S
        nc = self.nc
        self.dr = dr
        sb = S.sbuf
        self.PS = [S.psum("ps%d" % i, [128, 512], F32) for i in range(8)]
        self.h = [[sb("h", [128, 512], F32) for q in range(4)] for c in range(8)]
        self.cst = sb("cst", [128, C_END], F32)
        self.gcol = sb("gcol", [128, 128], F32)
        self.convw = sb("convw", [128, 96], F32)
        self.rows_ab = sb("rows_ab", [128, 2 * RA], F32)
        self.bo = sb("bo", [128, 16], F32)
        self.identb = sb("identb", [128, 128], BF16)
        self.onesb = sb("onesb", [128, 128], BF16)
        self.cos = sb("cos", [128, 16, 32], F32)
        self.sin = sb("sin", [128, 16, 32], F32)
        self.lvlm = sb("lvlm", [128, 1792], BF16)
        self.DMA("pool", self.lvlm, dr["lvlmask"])
        self.DMA("sp", self.cst, dr["consts"])
        self.DMA("sp", self.gcol, dr["gcols"])
        self.DMA("sp", self.convw, dr["convw"])
        self.DMA("sp", self.rows_ab, dr["rows_ab"])
        self.DMA("sp", self.bo, dr["bo_cols"])
        self.CP("dve", self.identb, self.cst[:, C_ID:C_ID + 128])
        self.CP("dve", self.onesb, self.cst[:, C_ONE:C_ONE + 128])
        self.negpi = sb("negpi", [128, 1], F32)
        S.add("pool", lambda e: e.memset(self.negpi.ap, -float(np.pi)), writes=[self.negpi])
        self.identf = self.cst[:, C_ID:C_ID + 128]
        self.onesf = self.cst[:, C_ONE:C_ONE + 128]

        for b in range(self.NSEQ):
            for c in range(8):
                for q in range(4):
                    self.DMA("sp", self.h[c][q], dr["xT"][b, c * 128:(c + 1) * 128, q * 512:(q + 1) * 512])
            if self.LAYERS > 1 and "c" in self.mix:
                self.rope_tables(b)
            for l in range(self.LAYERS):
                self.layer(b, l)
            for c in range(8):
                for q in range(4):
                    self.DMA("sp", dr["outT"][b, c * 128:(c + 1) * 128, q * 512:(q + 1) * 512],
                             self.h[c][q], is_out=True)
        S.emit()

    def alloc_hn(self, ctx, nq):
        self.hn = [[self.S.sbuf("hn", [128, 512], BF16, ctx) for q in range(nq)] for c in range(8)]

    def hq(self, c, q):
        row = self.hn[c]
        return row[q % len(row)]

    def gc(self, l, j, c):
        k = (l * 4 + j) * 8 + c
        return self.gcol[:, k:k + 1]

    def rstd_quarter(self, srcs, ctx):
        S = self.S
        ps = self.ps()
        for c in range(8):
            sq = self.R("sq", 2, lambda i: S.sbuf("sq", [128, 512], BF16, ctx))
            if c % 2 == 0:
                self.ACT(sq, srcs[c], AF.Square)
            else:
                self.TT("dve", sq, srcs[c], srcs[c], ALU.mult)
            self.MM(ps, self.onesb, sq, start=(c == 0), stop=(c == 7))
        rstd = self.R("rstd", 2, lambda i: S.sbuf("rstd", [128, 512], F32, ctx))
        self.ACT(rstd, ps, AF.Ln, bias=EPS, scale=1.0 / D_MODEL)
        self.ACT(rstd, rstd, AF.Exp, scale=-0.5)
        return rstd

    def norm_to_hn(self, l, j, ctx, quarters=range(4)):
        for q in quarters:
            rstd = self.rstd_quarter([self.h[c][q] for c in range(8)], ctx)
            for c in range(8):
                self.STT("dve", self.hq(c, q), self.h[c][q], self.gc(l, j, c), rstd, ALU.mult, ALU.mult)

    def add_normed(self, l, j, q, srcs, ctx):
        rstd = self.rstd_quarter(srcs, ctx)
        for c in range(8):
            if c % 4 == 3:
                self.TT("pool", srcs[c], srcs[c], rstd, ALU.mult)
                self.TS("pool", srcs[c], srcs[c], self.gc(l, j, c), ALU.mult)
                self.TT("pool", self.h[c][q], self.h[c][q], srcs[c], ALU.add)
            else:
                self.TT("dve", srcs[c], srcs[c], rstd, ALU.mult)
                self.STT("dve", self.h[c][q], srcs[c], self.gc(l, j, c), self.h[c][q], ALU.mult, ALU.add)

    def layer(self, b, l):
        S = self.S
        with ExitStack() as ctx:
            self.rot = {}
            kind0 = "ab" if l % 2 == 0 else "c"
            if kind0 == "c" and kind0 in self.mix:
                self.alloc_hn(ctx, 4)
                self.norm_to_hn(l, 0, ctx)
            self.mixT = [[S.sbuf("mixT", [128, 512], BF16, ctx) for q in range(4)] for c in range(8)]
            kind = "ab" if l % 2 == 0 else "c"
            if kind in self.mix:
                if kind == "ab":
                    with ExitStack() as c2:
                        self.alloc_hn(c2, 1)
                        self.mixer_ab(b, l, c2)
                        S.barrier()
                    self.rot = {}
                else:
                    self.mixer_c(b, l, ctx)
                self.out_proj(b, l, ctx)
            S.barrier()
        with ExitStack() as ctx:
            self.rot = {}
            self.ffn(b, l, ctx)
            S.barrier()
        with ExitStack() as ctx:
            self.rot = {}
            self.ple(b, l, ctx)
            S.barrier()

    def out_proj(self, b, l, ctx):
        S = self.S
        if l == 1:
            for c in range(8):
                self.dbg("d_mixT%d" % c, self.mixT[c][0], BF16)
        kind = "ab" if l % 2 == 0 else "c"
        wsrc = self.dr["w_out_ab"][l // 2] if kind == "ab" else self.dr["w_o_c"][l // 2]
        wv = wsrc.rearrange("(kc p) n -> p kc n", p=128)
        w = [S.sbuf("wout", [128, 8, 256], BF16, ctx) for i in range(4)]
        for i in range(4):
            self.DMA("pool", w[i], wv[:, :, i * 256:(i + 1) * 256])
        for q in range(4):
            srcs = []
            for m in range(8):
                ps = self.ps()
                for kc in range(8):
                    self.MM(ps, w[m // 2][:, kc, (m % 2) * 128:(m % 2) * 128 + 128], self.mixT[kc][q],
                            start=(kc == 0), stop=(kc == 7))
                t = self.R("mo", 8, lambda i: S.sbuf("mo", [128, 512], F32, ctx))
                if kind == "c":
                    k = (l // 2) * 8 + m
                    self.ACT(t, ps, AF.Identity, bias=self.bo[:, k:k + 1])
                else:
                    self.CP(self.ev(), t, ps)
                srcs.append(t)
            self.add_normed(l, 1, q, srcs, ctx)

    def ffn(self, b, l, ctx):
        S = self.S
        wup = self.dr["w_up"][l].rearrange("(kc p) n -> p kc n", p=128)
        wdn = self.dr["w_down"][l].rearrange("(kc p) n -> p kc n", p=128)
        act = [[S.sbuf("act", [128, 512], BF16, ctx) for n in range(2)] for k in range(16)]
        fft = [[S.sbuf("fft", [128, 512], F32, ctx) for n in range(2)] for m in range(8)]
        wslots = [S.sbuf("wffn", [128, 4096], BF16, ctx) for i in range(3)]
        self.alloc_hn(ctx, 2)
        wi = [0]

        def wslot():
            v = wslots[wi[0] % 3]
            wi[0] += 1
            return v

        for half in range(2):
            qs = [2 * half, 2 * half + 1]
            self.norm_to_hn(l, 2, ctx, quarters=qs)
            for fh in range(2):
                for g in range(4):
                    ws = wslot()
                    wu = V(ws.ap.rearrange("p (kc n) -> p kc n", kc=8), ws.trks)
                    c0 = fh * 2048 + g * 512
                    self.DMA("pool", wu, wup[:, :, c0:c0 + 512])
                    for m in range(4):
                        for n in range(2):
                            ps = self.ps()
                            for kc in range(8):
                                self.MM(ps, wu[:, kc, m * 128:(m + 1) * 128], self.hq(kc, qs[n]),
                                        start=(kc == 0), stop=(kc == 7))
                            r = self.R("relu", 2, lambda i: S.sbuf("relu", [128, 512], F32, ctx))
                            self.ACT(r, ps, AF.Relu)
                            self.TT("dve", act[g * 4 + m][n], r, r, ALU.mult)
                for g in range(4):
                    ws = wslot()
                    wd = V(ws.ap.rearrange("p (kc n) -> p kc n", kc=16), ws.trks)
                    self.DMA("pool", wd, wdn[:, fh * 16:(fh + 1) * 16, g * 256:(g + 1) * 256])
                    pss = [[self.ps() for n in range(2)] for m in range(2)]
                    for kc in range(16):
                        for m in range(2):
                            for n in range(2):
                                self.MM(pss[m][n], wd[:, kc, m * 128:(m + 1) * 128], act[kc][n],
                                        start=(kc == 0), stop=(kc == 15))
                    for m in range(2):
                        for n in range(2):
                            dst = fft[g * 2 + m][n]
                            if fh == 0:
                                self.CP(self.ev(), dst, pss[m][n])
                            else:
                                self.TT("dve", dst, dst, pss[m][n], ALU.add)
            for n in range(2):
                self.add_normed(l, 3, qs[n], [fft[m][n] for m in range(8)], ctx)

    def ple(self, b, l, ctx):
        S = self.S
        self.alloc_hn(ctx, 1)
        wg = S.sbuf("wg", [128, 8, 1024], BF16, ctx)
        wp = S.sbuf("wp", [128, 2, 1024], BF16, ctx)
        pT = [S.sbuf("pT", [128, 2, 512], BF16, ctx) for q in range(4)]
        wgv = self.dr["w_ple_gate"][l].rearrange("(kc p) n -> p kc n", p=128)
        for i in range(2):
            self.DMA("pool", wg[:, :, i * 512:(i + 1) * 512], wgv[:, :, i * 512:(i + 1) * 512])
        self.DMA("pool", wp, self.dr["w_ple"][l].rearrange("(kc p) n -> p kc n", p=128))
        pv = self.dr["pT"][l, b].rearrange("(kc p) t -> p kc t", p=128)
        for q in range(4):
            self.DMA("pool", pT[q], pv[:, :, q * 512:(q + 1) * 512])
        for q in range(4):
            for c in range(8):
                self.CP("act" if c % 2 else "pool", self.hq(c, q), self.h[c][q])
            for m in range(8):
                psg = self.ps()
                for kc in range(8):
                    self.MM(psg, wg[:, kc, m * 128:(m + 1) * 128], self.hq(kc, q), start=(kc == 0), stop=(kc == 7))
                psp = self.ps()
                for kc in range(2):
                    self.MM(psp, wp[:, kc, m * 128:(m + 1) * 128], pT[q][:, kc, :], start=(kc == 0), stop=(kc == 1))
                sg = self.R("sg", 3, lambda i: S.sbuf("sg", [128, 512], F32, ctx))
                self.ACT(sg, psg, AF.Sigmoid)
                self.TT("dve", sg, sg, psp, ALU.mult)
                self.TT("pool", self.h[m][q], self.h[m][q], sg, ALU.add)

    def rope_tables(self, b):
        S = self.S
        with ExitStack() as ctx:
            posi = S.sbuf("posi", [128, 16], I32, ctx)
            posf = S.sbuf("posf", [128, 16], F32, ctx)
            y0 = S.sbuf("y0", [128, 16, 32], F32, ctx)
            yy = S.sbuf("yy", [128, 16, 32], F32, ctx)
            ki = S.sbuf("ki", [128, 16, 32], I32, ctx)
            kf = S.sbuf("kf", [128, 16, 32], F32, ctx)
            mk = S.sbuf("mk", [128, 16, 32], F32, ctx)
            self.DMA("sp", posi, self.dr["pos"][b])
            self.CP("dve", posf, posi)
            inv = self.cst[:, C_INV:C_INV + 32]
            invb = V(inv.ap.unsqueeze(1).to_broadcast([128, 16, 32]), inv.trks)
            posb = V(posf.ap.unsqueeze(2).to_broadcast([128, 16, 32]), posf.trks)
            self.TT("dve", y0, invb, posb, ALU.mult)
            for dst, off in ((self.sin, 0.5), (self.cos, 0.75)):
                self.TS("dve", yy, y0, 1.0 / (2.0 * np.pi), ALU.mult, off, ALU.add)
                self.CP("dve", ki, yy)
                self.CP("dve", kf, ki)
                self.TT("dve", yy, yy, kf, ALU.subtract)
                self.TS("dve", mk, yy, 0.0, ALU.is_lt)
                self.TT("dve", yy, yy, mk, ALU.add)
                self.ACT(dst, yy, AF.Sin, bias=self.negpi, scale=2.0 * np.pi)
            S.barrier()

    def mixer_c(self, b, l, ctx0):
        S = self.S
        o = l // 2
        rows = S.sbuf("rowsc", [128, RC], F32, ctx0)
        self.DMA("sp", rows, self.dr["rows_c"][o])
        wv = self.dr["w_qkv_c"][o].rearrange("(kc p) n -> p kc n", p=128)
        for g in range(4):
          with ExitStack() as ctx:
            self.rot = dict((k, v) for k, v in self.rot.items() if k in ("sq", "rstd", "ntmp", "mo"))
            wq = S.sbuf("wq", [128, 8, 384], BF16, ctx)
            self.DMA("pool", wq[:, :, 0:256], wv[:, :, g * 256:(g + 1) * 256])
            self.DMA("pool", wq[:, :, 256:320], wv[:, :, 1024 + g * 64:1024 + (g + 1) * 64])
            self.DMA("pool", wq[:, :, 320:384], wv[:, :, 1280 + g * 64:1280 + (g + 1) * 64])
            brow = S.sbuf("brow", [128, 384], F32, ctx)
            self.CP("pool", brow[:, 0:256], rows[:, g * 256:(g + 1) * 256])
            self.CP("pool", brow[:, 256:320], rows[:, 1024 + g * 64:1024 + (g + 1) * 64])
            self.CP("pool", brow[:, 320:384], rows[:, 1280 + g * 64:1280 + (g + 1) * 64])
            qTall = S.sbuf("qTall", [128, 2, 2048], BF16, ctx)
            kTall = S.sbuf("kTall", [128, 2048], BF16, ctx)
            qTj = [V(qTall.ap[:, :, j * 128:(j + 1) * 128], [Trk("qTj")]) for j in range(16)]
            kTj = [V(kTall.ap[:, j * 128:(j + 1) * 128], [Trk("kTj")]) for j in range(16)]
            vtok = [S.sbuf("vtok", [128, 64], BF16, ctx) for j in range(16)]
            def tile1(j):
                    qkv = self.R("qkv", 2, lambda i: S.sbuf("qkv", [128, 384], F32, ctx))
                    ps = self.ps6()
                    for kc in range(8):
                        self.MM(ps[:, 0:384], self.hq(kc, j // 4)[:, (j % 4) * 128:(j % 4) * 128 + 128],
                                wq[:, kc, :], start=(kc == 0), stop=(kc == 7))
                    self.TT("dve", qkv, ps[:, 0:384], brow, ALU.add)
                    yield
                    qr = self.R("qr", 2, lambda i: S.sbuf("qr", [128, 256], BF16, ctx))
                    kd = self.R("kd", 2, lambda i: S.sbuf("kd", [128, 2, 64], BF16, ctx))
                    for (src, dst, nh) in ((qkv[:, 0:256], qr, 4), (qkv[:, 256:320], kd[:, 0, :], 1)):
                        sv = V(src.ap.rearrange("p (h t d) -> p h t d", t=2, d=32), src.trks)
                        dv = V(dst.ap.rearrange("p (h t d) -> p h t d", t=2, d=32), dst.trks)
                        cb = V(self.cos.ap[:, j, :].unsqueeze(1).to_broadcast([128, nh, 32]), self.cos.trks)
                        sb_ = V(self.sin.ap[:, j, :].unsqueeze(1).to_broadcast([128, nh, 32]), self.sin.trks)
                        t1 = self.R("rt1", 2, lambda i: S.sbuf("rt1", [128, 4, 32], F32, ctx))
                        t2 = self.R("rt2", 2, lambda i: S.sbuf("rt2", [128, 4, 32], F32, ctx))
                        t3 = self.R("rt3", 2, lambda i: S.sbuf("rt3", [128, 4, 32], F32, ctx))
                        t4 = self.R("rt4", 2, lambda i: S.sbuf("rt4", [128, 4, 32], F32, ctx))
                        self.TT("dve", t1[:, 0:nh, :], sv[:, :, 0, :], cb, ALU.mult)
                        yield
                        self.TT("dve", t2[:, 0:nh, :], sv[:, :, 1, :], sb_, ALU.mult)
                        yield
                        self.TT("dve", dv[:, :, 0, :], t1[:, 0:nh, :], t2[:, 0:nh, :], ALU.subtract)
                        yield
                        self.TT("pool", t3[:, 0:nh, :], sv[:, :, 1, :], cb, ALU.mult)
                        yield
                        self.TT("pool", t4[:, 0:nh, :], sv[:, :, 0, :], sb_, ALU.mult)
                        yield
                        self.TT("pool", dv[:, :, 1, :], t3[:, 0:nh, :], t4[:, 0:nh, :], ALU.add)
                        yield
                    self.CP("act", kd[:, 1, :], kd[:, 0, :])
                    yield
                    self.CP("act", vtok[j], qkv[:, 320:384])
                    yield
                    pst = self.ps6().bc(BF16)
                    for i in range(2):
                        self.TR(pst[:, i * 128:(i + 1) * 128], qr[:, i * 128:(i + 1) * 128], self.identb)
                    kdf = V(kd.ap.rearrange("p a d -> p (a d)"), kd.trks)
                    self.TR(pst[:, 256:384], kdf, self.identb)
                    src = V(pst.ap[:, 0:256].rearrange("p (i t) -> p i t", t=128), pst.trks)
                    self.CP("act", qTj[j], src)
                    self.CP("dve", kTj[j], pst[:, 256:384])
                    yield
            for j0 in range(0, 16, 2):
                self.lockstep([tile1(j0), tile1(j0 + 1)])
            def blk2(j):
                    nk = 1 if j == 0 else 2
                    SS = nk * 128
                    mask = self.cst[:, C_SWA + 256 - SS:C_SWA + 256]
                    pso = self.PS[6 + (self.psoi % 2)]
                    self.psoi += 1
                    recg = self.R("recg", 2, lambda i: S.sbuf("recg", [128, 4], F32, ctx))
                    ktr = [kTj[j - 1], kTj[j]] if j > 0 else [kTj[0]]
                    sc4 = self.R("sc4", 2, lambda i_: S.sbuf("sc4", [128, 4, 256], F32, ctx))
                    for hl in range(4):
                        i = hl // 2
                        hh = hl % 2
                        ps = self.ps6()
                        for kb in range(nk):
                            self.MM(ps[:, kb * 128:(kb + 1) * 128],
                                    qTj[j][hh * 64:(hh + 1) * 64, i, :], ktr[kb][hh * 64:(hh + 1) * 64, :])
                        self.STT("dve", sc4[:, hl, 0:SS], ps[:, 0:SS], 0.125, mask, ALU.mult, ALU.add)
                        yield
                    sk4 = rows[:, 1536 + 4 * g:1536 + 4 * g + 4]
                    mx4 = self.R("mx4", 2, lambda i_: S.sbuf("mx4", [128, 4], F32, ctx))
                    self.RED("dve", mx4, sc4[:, :, 0:SS], ALU.max)
                    yield
                    self.TT("dve", mx4, mx4, sk4, ALU.max)
                    yield
                    nmx4 = self.R("nmx4", 2, lambda i_: S.sbuf("nmx4", [128, 4], F32, ctx))
                    self.TS("dve", nmx4, mx4, -1.0, ALU.mult)
                    yield
                    pr4 = self.R("pr4", 2, lambda i_: S.sbuf("pr4", [128, 4, 256], BF16, ctx))
                    for hl in range(4):
                        self.ACT(pr4[:, hl, 0:SS], sc4[:, hl, 0:SS], AF.Exp, bias=nmx4[:, hl:hl + 1])
                        yield
                    rs4 = self.R("rs4", 2, lambda i_: S.sbuf("rs4", [128, 4], F32, ctx))
                    self.RED("dve", rs4, pr4[:, :, 0:SS], ALU.add)
                    yield
                    es4 = self.R("es4", 2, lambda i_: S.sbuf("es4", [128, 4], F32, ctx))
                    self.TT("dve", es4, sk4, mx4, ALU.subtract)
                    yield
                    self.ACT(es4, es4, AF.Exp)
                    yield
                    self.TT("dve", rs4, rs4, es4, ALU.add)
                    yield
                    self.RCP(recg, rs4)
                    yield
                    for hl in range(4):
                        pst = self.ps6().bc(BF16)
                        for kb in range(nk):
                            self.TR(pst[:, kb * 128:(kb + 1) * 128], pr4[:, hl, kb * 128:(kb + 1) * 128], self.identb)
                        prT = self.R("prT", 8, lambda i_: S.sbuf("prT", [128, 256], BF16, ctx))
                        self.CP(self.ev(), prT[:, 0:SS], pst[:, 0:SS])
                        yield
                        for kb in range(nk):
                            jk = j - 1 + kb if j > 0 else 0
                            self.MM(pso[:, hl * 64:hl * 64 + 64], prT[:, kb * 128:(kb + 1) * 128],
                                    vtok[jk], start=(kb == 0), stop=(kb == nk - 1))
                    otok = self.R("otok", 2, lambda i: S.sbuf("otok", [128, 256], BF16, ctx))
                    psov = V(pso.ap[:, 0:256].rearrange("p (h d) -> p h d", d=64), pso.trks)
                    recb = V(recg.ap.unsqueeze(2).to_broadcast([128, 4, 64]), recg.trks)
                    ov = V(otok.ap.rearrange("p (h d) -> p h d", d=64), otok.trks)
                    self.TT("dve", ov, psov, recb, ALU.mult)
                    yield
                    pst = self.ps6().bc(BF16)
                    for i in range(2):
                        self.TR(pst[:, i * 128:(i + 1) * 128], otok[:, i * 128:(i + 1) * 128], self.identb)
                    for i in range(2):
                        self.CP(self.ev(), self.mixT[2 * g + i][j // 4][:, (j % 4) * 128:(j % 4) * 128 + 128],
                                pst[:, i * 128:(i + 1) * 128])
                    yield
            for j0 in range(0, 16, 2):
                self.lockstep([blk2(j0), blk2(j0 + 1)])
            S.barrier()

    def mixer_ab(self, b, l, ctx0):
        S = self.S
        e = l // 2
        rab = lambda a, n: self.rows_ab[:, e * RA + a:e * RA + a + n]
        win = self.dr["w_in_ab"][e].rearrange("(kc p) n -> p kc n", p=128)
        sb = lambda name, shape, dt=F32: S.sbuf(name, shape, dt, ctx0)
        identf, onesf, identb = self.identf, self.onesf, self.identb
        triU = self.cst[:, C_TU:C_TU + 128]
        maskL = self.cst[:, C_ML:C_ML + 128]
        negm = self.cst[:, C_NM:C_NM + 128]
        wgate = sb("wgate", [128, 8, 16], BF16)
        self.DMA("pool", wgate[:, :, 0:8], win[:, :, 1536:1544])
        self.DMA("pool", wgate[:, :, 8:16], win[:, :, 3592:3600])
        G = sb("G", [128, 16, 16])
        for j in range(16):
            if j % 4 == 0:
                self.norm_to_hn(l, 0, ctx0, quarters=[j // 4])
            ps = self.ps()
            for kc in range(8):
                self.MM(ps[:, 0:16], self.hq(kc, j // 4)[:, (j % 4) * 128:(j % 4) * 128 + 128], wgate[:, kc, :],
                        start=(kc == 0), stop=(kc == 7))
            self.CP(self.ev(), G[:, j, :], ps[:, 0:16])
        bc4 = lambda v: V(v.ap.unsqueeze(1).to_broadcast([128, 16, 4]), v.trks)
        BETA = sb("BETA", [128, 16, 4])
        self.ACT(BETA, G[:, :, 0:4], AF.Sigmoid)
        X = sb("X", [128, 16, 4])
        T1 = sb("T1", [128, 16, 4])
        T2 = sb("T2", [128, 16, 4])
        SP = sb("SP", [128, 16, 4])

        def softplus(dst, x):
            self.TS("dve", T1, x, -1.0, ALU.mult)
            self.TT("dve", T1, T1, x, ALU.min)
            self.ACT(T1, T1, AF.Exp)
            self.ACT(T1, T1, AF.Ln, bias=1.0)
            self.TS("dve", T2, x, 0.0, ALU.max)
            self.TT("dve", dst, T1, T2, ALU.add)

        GF = sb("GF", [128, 16, 8])
        self.TT("dve", X, G[:, :, 4:8], bc4(rab(0, 4)), ALU.add)
        softplus(SP, X)
        negA = sb("negA", [128, 4])
        self.ACT(negA, rab(4, 4), AF.Exp)
        self.TS("dve", negA, negA, -1.0, ALU.mult)
        self.TT("dve", GF[:, :, 0:4], SP, bc4(negA), ALU.mult)
        ILOG = sb("ILOG", [128, 16, 4])
        self.TT("dve", ILOG, G[:, :, 8:12], bc4(rab(8, 4)), ALU.add)
        self.TT("dve", X, G[:, :, 12:16], bc4(rab(12, 4)), ALU.add)
        self.TS("dve", X, X, -1.0, ALU.mult)
        softplus(SP, X)
        self.TS("dve", GF[:, :, 4:8], SP, -1.0, ALU.mult)
        CUM = sb("CUM", [128, 16, 16])
        for j in range(16):
            ps = self.ps()
            self.MM(ps[:, 0:8], triU, GF[:, j, :])
            self.MM(ps[:, 8:16], onesf, GF[:, j, :])
            self.CP(self.ev(), CUM[:, j, :], ps[:, 0:16])
        gcum, bcum, gtot = CUM[:, :, 0:4], CUM[:, :, 4:8], CUM[:, :, 8:12]
        BG = sb("BG", [128, 16, 4])
        KD = sb("KD", [128, 16, 4])
        EGL = sb("EGL", [128, 16, 4])
        RR = sb("RR", [128, 16, 4])
        self.ACT(BG, gcum, AF.Exp)
        self.TT("dve", BG, BG, BETA, ALU.mult)
        self.TT("dve", KD, gtot, gcum, ALU.subtract)
        self.ACT(KD, KD, AF.Exp)
        self.ACT(EGL, gtot, AF.Exp)
        self.TT("dve", RR, ILOG, bcum, ALU.subtract)
        cols = [i * 128 for i in range(4)]
        vexts = [sb("vext", [128, 129], BF16) for _ in range(4)]
        for v in vexts:
            S.add("pool", lambda e_, v=v: e_.memset(v.ap[:, 128:129], 1.0), writes=[v])
        wh = sb("wh", [128, 8, 1024], BF16)
        Sa = sb("Sa", [128, 128])
        Sab = sb("Sab", [128, 128], BF16)
        Cx = sb("Cx", [128, 129])
        Cb = sb("Cb", [128, 129], BF16)
        mst = sb("mst", [128, 1])
        cwork = sb("cwork", [128, 515])
        ctail = [sb("ctail", [128, 4]) for _ in range(3)]
        qT = sb("qT", [128, 512], BF16)
        kT = sb("kT", [128, 512], BF16)
        vT = sb("vT", [128, 512], BF16)
        qbTs = [sb("qbT", [128, 512], BF16) for _ in range(2)]
        kbT = sb("kbT", [128, 512], BF16)
        acc = sb("acc", [128, 512])
        sl = acc
        t128 = lambda name, dt=F32: self.R(name, 2, lambda i_: S.sbuf(name, [128, 128], dt, ctx0))
        c1 = lambda name, n=1: self.R(name, 2, lambda i_: S.sbuf(name, [128, n], F32, ctx0))

        def finish(key, pf, o, gain, gate, ci, q, tsl):
            tmp = t128(pf + "f_tmp")
            self.ACT(tmp, o, AF.Square)
            yield
            ss = c1(pf + "f_ss")
            self.RED("dve", ss, tmp, ALU.add)
            yield
            self.ACT(ss, ss, AF.Ln, bias=EPS, scale=1.0 / 128.0)
            self.ACT(ss, ss, AF.Exp, scale=-0.5)
            yield
            y = t128(pf + "f_y")
            self.STT("dve", y, o, ss, gain, ALU.mult, ALU.mult)
            yb = t128(pf + "f_yb", BF16)
            self.TT("dve", yb, y, gate, ALU.mult)
            yield
            pY = self.psr(key).bc(BF16)
            self.TR(pY[:, 0:128], yb, identb)
            self.CP(self.ev(), self.mixT[ci][q][:, tsl], pY[:, 0:128])
            yield

        for i in range(4):
            gcols = [i * 128, 512 + i * 128, 1024 + i * 128, 2056 + i * 128, 2568 + i * 128,
                     3080 + i * 128, 3600 + i * 128, 1544 + i * 128]
            for gi, c0 in enumerate(gcols):
                self.DMA("pool", wh[:, :, gi * 128:(gi + 1) * 128], win[:, :, c0:c0 + 128])
            for t_ in (Sa, Sab, Cx, Cb, mst):
                S.add("pool", lambda e_, t_=t_: e_.memset(t_.ap, 0.0), writes=[t_])
            for g in range(3):
                S.add("pool", lambda e_, t_=ctail[g]: e_.memset(t_.ap, 0.0), writes=[ctail[g]])
            def prep(q):
                ps = self.psr("P")
                for c in range(8):
                    sq = self.R("sq", 2, lambda i_: S.sbuf("sq", [128, 512], BF16, ctx0))
                    if c % 2 == 0:
                        self.ACT(sq, self.h[c][q], AF.Square)
                    else:
                        self.TT("dve", sq, self.h[c][q], self.h[c][q], ALU.mult)
                    self.MM(ps, self.onesb, sq, start=(c == 0), stop=(c == 7))
                    yield
                rstd = self.R("rstd", 2, lambda i_: S.sbuf("rstd", [128, 512], F32, ctx0))
                self.ACT(rstd, ps, AF.Ln, bias=EPS, scale=1.0 / D_MODEL)
                self.ACT(rstd, rstd, AF.Exp, scale=-0.5)
                yield
                for c in range(8):
                    self.STT("dve", self.hq(c, q), self.h[c][q], self.gc(l, 0, c), rstd, ALU.mult, ALU.mult)
                    yield
                for gi in range(5):
                    ps = self.psr("P")
                    for kc in range(8):
                        self.MM(ps, wh[:, kc, gi * 128:(gi + 1) * 128], self.hq(kc, q), start=(kc == 0), stop=(kc == 7))
                    if gi < 3:
                        cb_ = cwork
                        self.CP("pool", cb_[:, 0:3], ctail[gi][:, 0:3])
                        yield
                        self.CP(self.ev(), cb_[:, 3:515], ps)
                        yield
                        wc = lambda k: self.convw[:, ((e * 4 + i) * 3 + gi) * 4 + k:((e * 4 + i) * 3 + gi) * 4 + k + 1]
                        self.TS("dve", acc, cb_[:, 0:512], wc(0), ALU.mult)
                        yield
                        for k in range(1, 4):
                            self.STT("dve", acc, cb_[:, k:k + 512], wc(k), acc, ALU.mult, ALU.add)
                            yield
                        self.CP("pool", ctail[gi][:, 0:3], cb_[:, 512:515])
                        yield
                        self.ACT(sl, acc, AF.Silu)
                        yield
                        if gi < 2:
                            sqb = self.R("sq", 2, lambda i_: S.sbuf("sq", [128, 512], BF16, ctx0))
                            self.ACT(sqb, sl, AF.Square)
                            yield
                            p2 = self.psr("P")
                            self.MM(p2, self.onesb, sqb)
                            rn5 = p2
                            self.ACT(rn5, p2, AF.Ln, bias=EPS)
                            yield
                            self.ACT(rn5, rn5, AF.Exp, scale=-0.5)
                            yield
                            if gi == 0:
                                self.STT("dve", qT, sl, 128.0 ** -0.5, rn5, ALU.mult, ALU.mult)
                                yield
                            else:
                                self.TT("dve", kT, sl, rn5, ALU.mult)
                                yield
                        else:
                            self.CP("pool", vT, sl)
                            yield
                    elif gi == 3:
                        self.ACT(qbTs[q % 2], ps, AF.Copy, scale=128.0 ** -0.5)
                        yield
                    else:
                        self.CP("dve", kbT, ps)
                        yield
                yield
            self.lockstep([prep(0)])
            for q in range(4):
                qbT = qbTs[q % 2]
                def mk_tile(jj):
                    j = 4 * q + jj
                    tsl = slice(jj * 128, (jj + 1) * 128)
                    col = lambda T_, off=0: T_[:, j, off + i:off + i + 1]
                    pt = self.ps()
                    for kc in range(8):
                        self.MM(pt, self.hq(kc, q)[:, tsl], wh[:, kc, 512:1024], start=(kc == 0), stop=(kc == 7))
                    h4 = lambda name, dt=F32: self.R(name, 4, lambda i_: S.sbuf(name, [128, 128], dt, ctx0))
                    kbtok = h4("kbtok")
                    self.CP("act", kbtok, pt[:, 0:128])
                    vext = vexts[j % 4]
                    self.CP("dve", vext[:, 0:128], pt[:, 128:256])
                    og = h4("og")
                    self.ACT(og, pt[:, 256:384], AF.Sigmoid)
                    sz = h4("sz")
                    self.ACT(sz, pt[:, 384:512], AF.Silu)
                    beta, gc_, bg, kdc, egl = col(BETA), col(CUM), col(BG), col(KD), col(EGL)
                    rcol, bcc, bl = col(RR), col(CUM, 4), col(CUM, 12)
                    gA = rab(16, 128)
                    gB = rab(144 + i * 128, 128)
                    h4 = lambda name, dt=F32: self.R(name, 4, lambda i_: S.sbuf(name, [128, 128], dt, ctx0))
                    kdec, qdT, ATb, wTn = h4("kdec", BF16), h4("qdT", BF16), h4("ATb", BF16), h4("wTn", BF16)
                    u, P0T, kw0 = h4("u"), h4("P0T", BF16), h4("kw0", BF16)
                    mi = self.R("mi", 4, lambda i_: S.sbuf("mi", [128, 1], F32, ctx0))
                    maxr = self.R("maxr", 4, lambda i_: S.sbuf("maxr", [128, 1], F32, ctx0))
                    kA = "A%d" % (jj % 2)
                    kB = "B%d" % (jj % 2)
                    def bulkA():
                        ptr = self.psr(kA).bc(BF16)
                        self.TR(ptr[:, 0:128], kT[:, tsl], identb)
                        self.TR(ptr[:, 128:256], vT[:, tsl], identb)
                        Rm = self.R("Rm", 2, lambda i_: S.sbuf("Rm", [128, 256], F32, ctx0))
                        self.TS("dve", Rm[:, 0:128], ptr[:, 128:256], beta, ALU.mult)
                        yield
                        self.TS("dve", Rm[:, 128:256], ptr[:, 0:128], bg, ALU.mult)
                        yield
                        self.TS("dve", kdec, ptr[:, 0:128], kdc, ALU.mult)
                        yield
                        dg = t128("dg")
                        self.TS("dve", dg, identf, gc_, ALU.mult)
                        yield
                        pG = self.psr(kA)
                        self.MM(pG[:, 0:128], onesf, dg)
                        D1 = t128("D1")
                        self.STT("dve", D1, pG[:, 0:128], gc_, maskL, ALU.subtract, ALU.mult)
                        yield
                        DT = t128("DT")
                        self.STT("dve", DT, pG[:, 0:128], gc_, triU, ALU.subtract, ALU.mult)
                        yield
                        EgR = pG[:, 0:128]
                        self.ACT(EgR, pG[:, 0:128], AF.Exp)
                        yield
                        self.ACT(D1, D1, AF.Exp, scale=-1.0)
                        yield
                        self.TT("dve", D1, D1, maskL, ALU.mult)
                        yield
                        self.ACT(DT, DT, AF.Exp)
                        yield
                        self.TT("dve", DT, DT, triU, ALU.mult)
                        yield
                        self.TT("dve", qdT, qT[:, tsl], EgR, ALU.mult)
                        yield
                        pK = self.psr(kA)
                        self.MM(pK[:, 0:128], kT[:, tsl], kT[:, tsl])
                        self.MM(pK[:, 128:256], kT[:, tsl], qT[:, tsl])
                        Lf = t128("Lf")
                        self.STT("dve", Lf, pK[:, 0:128], beta, D1, ALU.mult, ALU.mult)
                        yield
                        self.TT("dve", ATb, pK[:, 128:256], DT, ALU.mult)
                        yield
                        pL = self.psr(kA)
                        self.TR(pL[:, 0:128], Lf, identf)
                        LTf = t128("LTf")
                        self.CP("act", LTf, pL[:, 0:128])
                        yield
                        Mf = t128("Mf")
                        MTf = t128("MTf")
                        t128b = lambda name: self.R(name, 2, lambda i_: S.sbuf(name, [128, 128], F32, ctx0))
                        Ck = t128b("Ck")
                        CTk = t128b("CTk")
                        self.TT("pool", Ck, Lf, self.lvlm[:, 0:128], ALU.mult)
                        self.TT("pool", CTk, LTf, self.lvlm[:, 896:1024], ALU.mult)
                        self.TT("pool", Mf, identf, Ck, ALU.subtract)
                        self.TT("pool", MTf, identf, CTk, ALU.subtract)
                        yield
                        for k in range(1, 7):
                            Ck = t128b("Ck")
                            CTk = t128b("CTk")
                            self.TT("pool", Ck, Lf, self.lvlm[:, k * 128:(k + 1) * 128], ALU.mult)
                            self.TT("pool", CTk, LTf, self.lvlm[:, 896 + k * 128:896 + (k + 1) * 128], ALU.mult)
                            pa_ = self.psr(kA)
                            self.MM(pa_[:, 0:128], CTk, Mf)
                            self.MM(pa_[:, 128:256], Ck, MTf)
                            T1f = t128("T1f")
                            T3f = t128("T3f")
                            self.CP("act", T1f, pa_[:, 0:128])
                            self.CP("dve", T3f, pa_[:, 128:256])
                            yield
                            pb_ = self.psr(kA)
                            self.MM(pb_[:, 0:128], MTf, T1f)
                            self.MM(pb_[:, 128:256], Mf, T3f)
                            self.TT("dve", Mf, Mf, pb_[:, 0:128], ALU.subtract)
                            self.TT("dve", MTf, MTf, pb_[:, 128:256], ALU.subtract)
                            yield
                        pX = self.psr(kA)
                        self.MM(pX[:, 0:256], MTf, Rm)
                        wb = t128("wb", BF16)
                        self.CP("act", u, pX[:, 0:128])
                        yield
                        self.CP("dve", wb, pX[:, 128:256])
                        yield
                        pW = self.psr(kA).bc(BF16)
                        self.TR(pW[:, 0:128], wb, identb)
                        self.ACT(wTn, pW[:, 0:128], AF.Copy, scale=-1.0)
                        yield
                    def scanA():
                        pV = self.psr("SA")
                        self.MM(pV[:, 0:128], wTn, Sab)
                        vn = t128("vn", BF16)
                        self.TT("dve", vn, u, pV[:, 0:128], ALU.add)
                        yield
                        pO = self.psr("SA")
                        self.MM(pO[:, 0:128], qdT, Sab, start=True, stop=False)
                        self.MM(pO[:, 0:128], ATb, vn, start=False, stop=True)
                        oA = t128("oA")
                        self.CP("act", oA, pO[:, 0:128])
                        yield
                        pS = self.psr("SA")
                        self.MM(pS[:, 0:128], kdec, vn)
                        self.STT("dve", Sa, Sa, egl, pS[:, 0:128], ALU.mult, ALU.add)
                        yield
                        self.CP("act", Sab, Sa)
                        yield
                        yield from finish("SA", "a", oA, gA, sz, i, q, tsl)
                    def bulkB():
                        dgr = t128("dgr")
                        self.TS("dve", dgr, identf, rcol, ALU.mult)
                        yield
                        pR = self.psr(kB)
                        self.MM(pR[:, 0:128], onesf, dgr)
                        tmpB = t128("tmpB")
                        self.TT("dve", tmpB, pR[:, 0:128], negm, ALU.add)
                        yield
                        self.RED("dve", maxr, pR[:, 0:128], ALU.max)
                        yield
                        mr = c1("mr")
                        self.RED("dve", mr, tmpB, ALU.max)
                        yield
                        nmr = c1("nmr")
                        self.TS("dve", nmr, mr, -1.0, ALU.mult)
                        yield
                        Eb = t128("Eb")
                        self.ACT(Eb, tmpB, AF.Exp, bias=nmr)
                        yield
                        pQ = self.psr(kB)
                        self.MM(pQ[:, 0:128], qbT[:, tsl], kbT[:, tsl])
                        P0 = t128("P0", BF16)
                        self.TT("dve", P0, pQ[:, 0:128], Eb, ALU.mult)
                        yield
                        pP0 = self.psr(kB).bc(BF16)
                        self.TR(pP0[:, 0:128], P0, identb)
                        self.CP("act", P0T, pP0[:, 0:128])
                        yield
                        ew = c1("ew")
                        self.TT("dve", ew, rcol, maxr, ALU.subtract)
                        yield
                        self.ACT(ew, ew, AF.Exp)
                        yield
                        self.TS("dve", kw0, kbtok, ew, ALU.mult)
                        yield
                        self.TT("dve", mi, mr, bcc, ALU.add)
                        yield
                    def scanB():
                        a_ = c1("a_")
                        self.TT("dve", a_, bcc, mst, ALU.add)
                        yield
                        mt = c1("mt")
                        self.TT("dve", mt, a_, mi, ALU.max)
                        yield
                        e3 = c1("e3", 3)
                        self.TT("dve", e3[:, 0:1], a_, mt, ALU.subtract)
                        yield
                        self.TT("dve", e3[:, 1:2], mi, mt, ALU.subtract)
                        yield
                        self.TS("dve", e3[:, 2:3], mt, -1.0, ALU.mult)
                        yield
                        self.ACT(e3, e3, AF.Exp)
                        yield
                        p1 = self.psr("SB")
                        self.MM(p1[:, 0:129], qbT[:, tsl], Cb)
                        p2 = self.psr("SB")
                        self.MM(p2[:, 0:129], P0T, vext)
                        nd = self.R("nd", 2, lambda i_: S.sbuf("nd", [128, 129], F32, ctx0))
                        self.TS("dve", nd, p1[:, 0:129], e3[:, 0:1], ALU.mult)
                        yield
                        self.STT("dve", nd, p2[:, 0:129], e3[:, 1:2], nd, ALU.mult, ALU.add)
                        yield
                        td = c1("td")
                        self.TS("dve", td, nd[:, 128:129], -1.0, ALU.mult)
                        yield
                        self.TT("dve", td, td, nd[:, 128:129], ALU.max)
                        yield
                        self.TT("dve", td, td, e3[:, 2:3], ALU.max)
                        yield
                        self.RCP(td, td)
                        yield
                        hB = t128("hB")
                        self.TS("dve", hB, nd[:, 0:128], td, ALU.mult)
                        yield
                        mm = c1("mm")
                        self.TT("dve", mm, mst, maxr, ALU.max)
                        yield
                        e2 = c1("e2", 2)
                        self.TT("dve", e2[:, 0:1], mst, mm, ALU.subtract)
                        yield
                        self.TT("dve", e2[:, 1:2], maxr, mm, ALU.subtract)
                        yield
                        self.ACT(e2, e2, AF.Exp)
                        yield
                        p3 = self.psr("SB")
                        self.MM(p3[:, 0:129], kw0, vext)
                        self.TS("dve", Cx, Cx, e2[:, 0:1], ALU.mult)
                        yield
                        self.STT("dve", Cx, p3[:, 0:129], e2[:, 1:2], Cx, ALU.mult, ALU.add)
                        yield
                        self.CP("act", Cb, Cx)
                        yield
                        self.TT("dve", mst, bl, mm, ALU.add)
                        yield
                        yield from finish("SB", "b", hB, gB, og, 4 + i, q, tsl)
                    return bulkA, scanA, bulkB, scanB
                TL = [mk_tile(jj) for jj in range(4)]
                def seq(*gs):
                    for g_ in gs:
                        yield from g_
                def par(*gs):
                    gs = list(gs)
                    while gs:
                        al = []
                        for g_ in gs:
                            try:
                                next(g_)
                                al.append(g_)
                            except StopIteration:
                                pass
                        gs = al
                        yield
                self.lockstep([TL[0][0](), TL[0][2](), TL[1][0](), TL[1][2]()])
                self.lockstep([TL[2][0](), TL[2][2](), TL[3][0](), TL[3][2](),
                               seq(par(TL[0][1](), TL[0][3]()), par(TL[1][1](), TL[1][3]()))])
                P2 = [seq(par(TL[2][1](), TL[2][3]()), par(TL[3][1](), TL[3][3]()))]
                if q < 3:
                    P2.append(prep(q + 1))
                self.lockstep(P2)


def make_consts():
    c = np.zeros((128, C_END), np.float32)
    i = np.arange(128)
    P, Fr = i[:, None], i[None, :]
    same = (P // 64) == (Fr // 64)
    c[:, C_ID:C_ID + 128] = np.eye(128)
    s = np.arange(256)[None, :]
    c[:, C_SWA:C_SWA + 256] = np.where((s > P) & (s <= P + 128), 0.0, NEG)
    half = 32
    inv = (10000.0 ** (-np.arange(half, dtype=np.float32) / half)).astype(np.float32)
    c[:, C_INV:C_INV + 32] = inv[None, :]
    c[:, C_ONE:C_ONE + 128] = 1.0
    c[:, C_TU:C_TU + 128] = (Fr >= P)
    c[:, C_ML:C_ML + 128] = (Fr < P)
    c[:, C_NM:C_NM + 128] = np.where(Fr <= P, 0.0, NEG)
    return c


DEBUG = False


def make_lvlmask():
    m = np.zeros((128, 1792), np.float32)
    i = np.arange(128)
    c, e = i[:, None], i[None, :]
    for k in range(7):
        sz = 1 << k
        mk = ((c // (2 * sz)) == (e // (2 * sz))) & ((c % (2 * sz)) >= sz) & ((e % (2 * sz)) < sz)
        m[:, k * 128:(k + 1) * 128] = mk
        m[:, 896 + k * 128:896 + (k + 1) * 128] = mk.T
    return m


def build_nc(NSEQ, LAYERS, mix=("ab", "c")):
    nc = bass.Bass("TRN2", target_bir_lowering=False)

    def di(name, shape, dt=F32):
        return nc.dram_tensor(name, list(shape), dt, kind="ExternalInput").ap()

    dr = {
        "xT": di("xT", [NSEQ, 1024, 2048]),
        "pT": di("pT", [4, NSEQ, 256, 2048]),
        "pos": di("pos", [NSEQ, 128, 16], I32),
        "consts": di("consts", [128, C_END]),
        "lvlmask": di("lvlmask", [128, 1792]),
        "gcols": di("gcols", [128, 128]),
        "convw": di("convw", [128, 96]),
        "rows_ab": di("rows_ab", [128, 2 * RA]),
        "rows_c": di("rows_c", [2, 128, RC]),
        "bo_cols": di("bo_cols", [128, 16]),
        "w_in_ab": di("w_in_ab", [2, 1024, IN_COLS]),
        "w_out_ab": di("w_out_ab", [2, 1024, 1024]),
        "w_qkv_c": di("w_qkv_c", [2, 1024, 1536]),
        "w_o_c": di("w_o_c", [2, 1024, 1024]),
        "w_up": di("w_up", [4, 1024, 4096]),
        "w_down": di("w_down", [4, 4096, 1024]),
        "w_ple": di("w_ple", [4, 256, 1024]),
        "w_ple_gate": di("w_ple_gate", [4, 1024, 1024]),
    }
    dr["outT"] = nc.dram_tensor("outT", [NSEQ, 1024, 2048], F32, kind="ExternalOutput").ap()
    with ExitStack() as ctx:
        kb = KB(nc, ctx, NSEQ, LAYERS, mix)
        kb.debug = DEBUG
        kb.build(dr)
    return nc


def host_params(inp):
    f = lambda a: np.ascontiguousarray(np.asarray(a, dtype=np.float32))
    ng = f(inp["norm_gains"])
    gcols = np.ascontiguousarray(ng.reshape(4, 4, 8, 128).transpose(3, 0, 1, 2).reshape(128, 128))
    cw = f(inp["conv_a"])
    convw = np.ascontiguousarray(cw.reshape(2, 4, 3, 4, 128).transpose(4, 0, 3, 2, 1).reshape(128, 96))
    rows = np.zeros((2, RA), np.float32)
    for e in range(2):
        rows[e, 0:4] = f(inp["dt_bias"])[e]
        rows[e, 4:8] = f(inp["a_log"])[e]
        rows[e, 8:12] = f(inp["i_bias_b"])[e]
        rows[e, 12:16] = f(inp["f_bias_b"])[e]
        rows[e, 16:144] = f(inp["norm_a"])[e]
        rows[e, 144:656] = f(inp["norm_b"])[e].reshape(512)
    rows_ab = np.ascontiguousarray(np.broadcast_to(rows.reshape(1, 2 * RA), (128, 2 * RA)))
    rc = np.zeros((2, RC), np.float32)
    for o in range(2):
        rc[o, 0:1536] = f(inp["b_qkv_c"])[o]
        rc[o, 1536:1552] = f(inp["sinks_c"])[o]
    rows_c = np.ascontiguousarray(np.broadcast_to(rc[:, None, :], (2, 128, RC)))
    bo = f(inp["b_o_c"])
    bo_cols = np.ascontiguousarray(bo.reshape(2, 8, 128).transpose(2, 0, 1).reshape(128, 16))
    return {
        "consts": make_consts(), "lvlmask": make_lvlmask(), "gcols": gcols, "convw": convw, "rows_ab": rows_ab, "rows_c": rows_c,
        "bo_cols": bo_cols,
        "w_in_ab": f(inp["w_in_ab"]), "w_out_ab": f(inp["w_out_ab"]), "w_qkv_c": f(inp["w_qkv_c"]),
        "w_o_c": f(inp["w_o_c"]), "w_up": f(inp["w_up"]), "w_down": f(inp["w_down"]),
        "w_ple": f(inp["w_ple"]), "w_ple_gate": f(inp["w_ple_gate"]),
    }


def run(inp, n_cores=8, LAYERS=DEPTH, mix=("ab", "c"), trace=False):
    x = np.asarray(inp["x"], dtype=np.float32)
    p = np.asarray(inp["p"], dtype=np.float32)
    pos = np.asarray(inp["positions"], dtype=np.int32)
    B = x.shape[0]
    NSEQ = B // n_cores
    shared = host_params(inp)
    nc = build_nc(NSEQ, LAYERS, mix)
    in_maps = []
    for c in range(n_cores):
        sl = slice(c * NSEQ, (c + 1) * NSEQ)
        m = dict(shared)
        m["xT"] = np.ascontiguousarray(x[sl].transpose(0, 2, 1))
        m["pT"] = np.ascontiguousarray(p[:, sl].transpose(0, 1, 3, 2))
        m["pos"] = np.ascontiguousarray(pos[sl].reshape(-1, 16, 128).transpose(0, 2, 1))
        in_maps.append(m)
    res = run_bass_kernel_spmd(nc, in_maps, core_ids=list(range(n_cores)), trace=trace)
    out = np.concatenate([np.asarray(r["outT"]).transpose(0, 2, 1) for r in res.results], axis=0)
    return np.ascontiguousarray(out.astype(np.float32)), res


def kernel(**inputs):
    out, _ = run(inputs)
    return out
```
